# Optimizing a Trainium2 kernel written in Bass

```python
import math
import jax
import jax.numpy as jnp
from jax import lax
import numpy as np

D_MODEL = 2048
BATCH = 8
SEQ = 4096
DEPTH = 4

CHUNK = 64
N_EVEN = (DEPTH + 1) // 2
N_ODD = DEPTH // 2
D_FF = ((8 * D_MODEL // 3 + 255) // 256) * 256
RMS_EPS = 1e-6
LN_EPS = 1e-5
NEG_INF = -1e30

CONV_W = 3
A_WIDTH = D_MODEL // 2
POOL_WINDOWS = (2, 4, 8, 16)
N_POOL = len(POOL_WINDOWS)
B_WIDTH = D_MODEL // 2
B_GROUP = B_WIDTH // N_POOL
AB_IN = 3 * A_WIDTH + B_WIDTH

C_HEADS = D_MODEL // 256
C_HEAD_DIM = 128
C_WIDTH = C_HEADS * C_HEAD_DIM
IDX_HEADS = 16
IDX_DIM = 64
IDX_W_SCALE = IDX_HEADS ** -0.5
TOPK_MAX = 256
Q_BLOCK = 128
NUM_BUCKETS = 32
MAX_DISTANCE = 128

R_HEADS = D_MODEL // 256
R_QK_DIM = 64
R_V_DIM = 128
R_WIDTH = R_HEADS * R_V_DIM
ROPE_BASE = 10000.0

CD_SIZES = (C_WIDTH, C_WIDTH, C_WIDTH,
            IDX_HEADS * IDX_DIM, IDX_DIM, IDX_HEADS,
            R_HEADS * R_QK_DIM, R_HEADS * R_QK_DIM,
            R_WIDTH, R_WIDTH)
CD_IN = sum(CD_SIZES)

kernel_name = 'hybrid_chunk_causal_conv_pool_dsa_retention'


def _split(a, sizes):
    outs, off = [], 0
    for s in sizes:
        outs.append(a[..., off:off + s])
        off += s
    return outs


def rmsnorm(x, g):
    xf = x.astype(jnp.float32)
    y = xf * lax.rsqrt(jnp.mean(xf * xf, axis=-1, keepdims=True) + RMS_EPS)
    return (y * g.astype(jnp.float32)).astype(x.dtype)


def swiglu_ffn(h, w_gate_up, w_down):
    gu = h @ w_gate_up
    gate, up = gu[..., :D_FF], gu[..., D_FF:]
    return (jax.nn.silu(gate) * up) @ w_down


def short_gated_conv(a_in, conv_w):
    b, c, h = _split(a_in, (A_WIDTH, A_WIDTH, A_WIDTH))
    u = c * h
    T = u.shape[1]
    up = jnp.pad(u, ((0, 0), (CONV_W - 1, 0), (0, 0)))
    y = sum(conv_w[j] * up[:, j:j + T] for j in range(CONV_W))
    return b * y


def multiscale_pool(b_in, pool_w, pool_scale):
    Bsz, T, _ = b_in.shape
    ug = b_in.reshape(Bsz, T, N_POOL, B_GROUP).astype(jnp.float32)
    cs = jnp.pad(jnp.cumsum(ug, axis=1), ((0, 0), (1, 0), (0, 0), (0, 0)))
    t = jnp.arange(T)
    win = jnp.array(POOL_WINDOWS, dtype=jnp.int32)
    start = jnp.maximum(t[:, None] + 1 - win[None, :], 0)
    lower = cs[:, start, jnp.arange(N_POOL)[None, :], :]
    count = (t[:, None] + 1 - start).astype(jnp.float32)
    pooled = (cs[:, 1:] - lower) / count[None, :, :, None] - ug
    mixed = jnp.einsum('btgc,gcd->btgd', pooled.astype(b_in.dtype), pool_w)
    return mixed.reshape(Bsz, T, B_WIDTH) * pool_scale


def t5_bucket(rel):
    nb = NUM_BUCKETS // 2
    max_exact = nb // 2
    ret = jnp.where(rel > 0, nb, 0)
    n = jnp.abs(rel)
    large = max_exact + (jnp.log(jnp.maximum(n, 1).astype(jnp.float32) / max_exact)
                         / math.log(MAX_DISTANCE / max_exact) * (nb - max_exact)).astype(jnp.int32)
    large = jnp.minimum(large, nb - 1)
    return ret + jnp.where(n < max_exact, n, large)


def dsa_attention(q, k, v, q_idx, k_idx, w_idx, rel_bias_table):
    Bsz, T, H, Dh = q.shape
    topk = min(TOPK_MAX, T // 4)
    n_blocks = T // Q_BLOCK
    key_pos = jnp.arange(T)
    k_idx32 = k_idx.astype(jnp.float32)
    table = rel_bias_table.astype(jnp.float32)
    gather = jax.vmap(lambda a, i: a[i])

    def one_block(blk):
        t0 = blk * Q_BLOCK
        qb = lax.dynamic_slice_in_dim(q, t0, Q_BLOCK, axis=1)
        qib = lax.dynamic_slice_in_dim(q_idx, t0, Q_BLOCK, axis=1).astype(jnp.float32)
        wib = lax.dynamic_slice_in_dim(w_idx, t0, Q_BLOCK, axis=1).astype(jnp.float32) * IDX_W_SCALE
        q_pos = t0 + jnp.arange(Q_BLOCK)
        vis_end = (q_pos // CHUNK + 1) * CHUNK
        dots = jnp.einsum('bqhd,bsd->bqhs', qib, k_idx32) * (IDX_DIM ** -0.5)
        score = jnp.einsum('bqh,bqhs->bqs', wib, jax.nn.relu(dots))
        admissible = key_pos[None, :] < vis_end[:, None]
        score = jnp.where(admissible[None], score, NEG_INF)
        _, sel = lax.top_k(score, topk)
        valid = sel < vis_end[None, :, None]
        kg = gather(k, sel)
        vg = gather(v, sel)
        logits = jnp.einsum('bqhd,bqkhd->bqhk', qb, kg).astype(jnp.float32) * (Dh ** -0.5)
        bias = table[t5_bucket(sel - q_pos[None, :, None])]
        logits = logits + jnp.moveaxis(bias, -1, 2)
        logits = jnp.where(valid[:, :, None, :], logits, NEG_INF)
        p = jax.nn.softmax(logits, axis=-1).astype(v.dtype)
        return jnp.einsum('bqhk,bqkhd->bqhd', p, vg)

    out = lax.map(one_block, jnp.arange(n_blocks))
    return jnp.moveaxis(out, 0, 1).reshape(Bsz, T, H * Dh)


def rope(x, pos):
    half = x.shape[-1] // 2
    freqs = ROPE_BASE ** (-jnp.arange(half, dtype=jnp.float32) / half)
    ang = pos.astype(jnp.float32)[:, None] * freqs[None, :]
    cos = jnp.cos(ang)[None, :, None, :]
    sin = jnp.sin(ang)[None, :, None, :]
    x1, x2 = x[..., :half], x[..., half:]
    return jnp.concatenate([x1 * cos - x2 * sin, x1 * sin + x2 * cos], axis=-1)


def retention(q, k, v, g, ret_gain):
    Bsz, T, H, dk = q.shape
    dv = v.shape[-1]
    nc = T // CHUNK
    pos = jnp.arange(T)
    qf = rope(q.astype(jnp.float32), pos)
    kf = rope(k.astype(jnp.float32), pos) * (dk ** -0.5)
    vf = v.astype(jnp.float32)
    log_g = jnp.log(1.0 - 2.0 ** (-5.0 - jnp.arange(H, dtype=jnp.float32)))
    qc = qf.reshape(Bsz, nc, CHUNK, H, dk)
    kc = kf.reshape(Bsz, nc, CHUNK, H, dk)
    vc = vf.reshape(Bsz, nc, CHUNK, H, dv)
    i = jnp.arange(CHUNK)
    intra = jnp.exp(jnp.abs(i[:, None] - i[None, :]).astype(jnp.float32)[None] * log_g[:, None, None])
    a = jnp.einsum('bnihd,bnjhd->bnhij', qc, kc) * intra[None, None]
    o_intra = jnp.einsum('bnhij,bnjhe->bnihe', a, vc)
    k_dec = jnp.exp((CHUNK - 1 - i).astype(jnp.float32)[:, None] * log_g[None, :])
    kv = jnp.einsum('bnjhd,jh,bnjhe->nbhde', kc, k_dec, vc)
    chunk_dec = jnp.exp(CHUNK * log_g)[None, :, None, None]

    def step(state, kv_n):
        return state * chunk_dec + kv_n, state

    _, prev = lax.scan(step, jnp.zeros((Bsz, H, dk, dv), jnp.float32), kv)
    q_dec = jnp.exp((i + 1).astype(jnp.float32)[:, None] * log_g[None, :])
    o_inter = jnp.einsum('bnihd,ih,nbhde->bnihe', qc, q_dec, prev)
    o = (o_intra + o_inter).reshape(Bsz, T, H, dv)
    mu = jnp.mean(o, axis=-1, keepdims=True)
    var = jnp.mean(jnp.square(o - mu), axis=-1, keepdims=True)
    o = ((o - mu) * lax.rsqrt(var + LN_EPS)).reshape(Bsz, T, H * dv) * ret_gain.astype(jnp.float32)
    return (jax.nn.silu(g.astype(jnp.float32)) * o).astype(g.dtype)


def conv_pool_mixer(u, w_in, conv_w, pool_w, pool_scale, w_out):
    proj = u @ w_in
    ya = short_gated_conv(proj[..., :3 * A_WIDTH], conv_w)
    yb = multiscale_pool(proj[..., 3 * A_WIDTH:], pool_w, pool_scale)
    return jnp.concatenate([ya, yb], axis=-1) @ w_out


def sparse_retention_mixer(u, w_in, ret_gain, rel_bias_table, w_out):
    Bsz, T, _ = u.shape
    proj = u @ w_in
    cq, ck, cv, iq, ik, iw, rq, rk, rv, rg = _split(proj, CD_SIZES)
    heads = lambda a, h: a.reshape(Bsz, T, h, -1)
    yc = dsa_attention(heads(cq, C_HEADS), heads(ck, C_HEADS), heads(cv, C_HEADS),
                       heads(iq, IDX_HEADS), ik, iw, rel_bias_table)
    yd = retention(heads(rq, R_HEADS), heads(rk, R_HEADS), heads(rv, R_HEADS), rg, ret_gain)
    return jnp.concatenate([yc, yd], axis=-1) @ w_out


def setup_inputs(seed: int = 0) -> dict:
    key = jax.random.key(seed)
    ks = jax.random.split(key, 16)
    f32 = jnp.float32
    nrm = lambda k, shape, scale: jax.random.normal(k, shape, f32) * scale
    return {
        'x': nrm(ks[0], (BATCH, SEQ, D_MODEL), 1.0),
        'norm_gains': 1.0 + nrm(ks[1], (DEPTH, 3, D_MODEL), 0.05),
        'ffn_w_gate_up': nrm(ks[2], (DEPTH, 2, D_MODEL, 2 * D_FF), D_MODEL ** -0.5),
        'ffn_w_down': nrm(ks[3], (DEPTH, 2, D_FF, D_MODEL), D_FF ** -0.5),
        'ab_w_in': nrm(ks[4], (N_EVEN, D_MODEL, AB_IN), D_MODEL ** -0.5),
        'ab_conv_w': nrm(ks[5], (N_EVEN, CONV_W, A_WIDTH), CONV_W ** -0.5),
        'ab_pool_w': nrm(ks[6], (N_EVEN, N_POOL, B_GROUP, B_GROUP), B_GROUP ** -0.5),
        'ab_pool_scale': 1.0 + nrm(ks[7], (N_EVEN, B_WIDTH), 0.1),
        'ab_w_out': nrm(ks[8], (N_EVEN, A_WIDTH + B_WIDTH, D_MODEL), (A_WIDTH + B_WIDTH) ** -0.5),
        'cd_w_in': nrm(ks[9], (N_ODD, D_MODEL, CD_IN), D_MODEL ** -0.5),
        'ret_norm_gain': 1.0 + nrm(ks[10], (N_ODD, R_WIDTH), 0.05),
        'cd_w_out': nrm(ks[11], (N_ODD, C_WIDTH + R_WIDTH, D_MODEL), (C_WIDTH + R_WIDTH) ** -0.5),
        'rel_bias_table': nrm(ks[12], (NUM_BUCKETS, C_HEADS), 0.5),
        'final_norm': 1.0 + nrm(ks[13], (D_MODEL,), 0.05),
    }


def reference(x, norm_gains, ffn_w_gate_up, ffn_w_down, ab_w_in, ab_conv_w, ab_pool_w,
              ab_pool_scale, ab_w_out, cd_w_in, ret_norm_gain, cd_w_out, rel_bias_table,
              final_norm):
    h = x
    for layer in range(DEPTH):
        j = layer // 2
        h = h + 0.5 * swiglu_ffn(rmsnorm(h, norm_gains[layer, 0]),
                                 ffn_w_gate_up[layer, 0], ffn_w_down[layer, 0])
        u = rmsnorm(h, norm_gains[layer, 1])
        if layer % 2 == 0:
            mix = conv_pool_mixer(u, ab_w_in[j], ab_conv_w[j], ab_pool_w[j],
                                  ab_pool_scale[j], ab_w_out[j])
        else:
            mix = sparse_retention_mixer(u, cd_w_in[j], ret_norm_gain[j],
                                         rel_bias_table, cd_w_out[j])
        h = h + mix
        h = h + 0.5 * swiglu_ffn(rmsnorm(h, norm_gains[layer, 2]),
                                 ffn_w_gate_up[layer, 1], ffn_w_down[layer, 1])
    return rmsnorm(h, final_norm)
```

```python
import numpy as np
import concourse.bass as bass
import concourse.mybir as mybir
from concourse.bass_utils import run_bass_kernel_spmd

F32 = mybir.dt.float32
BF16 = mybir.dt.bfloat16
AF = mybir.ActivationFunctionType
ALU = mybir.AluOpType
AX = mybir.AxisListType

D = 2048
T = 4096
DFF = 5632
DEPTH = 4
NFC = D // 128
TT = 512
NT = T // TT
RMS_EPS = 1e-6
N_CORES = 8

CENGS = ("pe", "act", "dve", "pool")
ENGS = CENGS + ("sp",)


class Op:
    __slots__ = ("eng", "fn", "deps", "xdeps", "is_dma", "dsem", "dval", "val", "waited")

    def __init__(self, eng, fn, is_dma=False):
        self.eng = eng
        self.fn = fn
        self.deps = []
        self.xdeps = []
        self.is_dma = is_dma
        self.dsem = None
        self.dval = 0
        self.val = None
        self.waited = False


class Tracker:
    def __init__(self, nc):
        self.nc = nc
        self.esem = {e: nc.alloc_semaphore(name="es_" + e) for e in CENGS}
        self.ecount = {e: 0 for e in CENGS}
        self.dma_sems = {}
        self.dma_cnt = {}
        self.last_w = {}
        self.readers = {}
        self.byeng = {e: [] for e in ENGS}
        self.nops = 0

    def _add(self, op, reads, writes):
        deps = []
        for r in reads:
            w = self.last_w.get(r)
            if w is not None:
                deps.append(w)
        for w_ in writes:
            w = self.last_w.get(w_)
            if w is not None:
                deps.append(w)
            deps.extend(self.readers.get(w_, ()))
        for r in reads:
            self.readers.setdefault(r, []).append(op)
        for w_ in writes:
            self.last_w[w_] = op
            self.readers[w_] = []
        seen = set()
        for d in deps:
            if d is op or id(d) in seen:
                continue
            if op.eng == "pe" and d.eng == "pe" and not d.is_dma and not op.is_dma:
                continue
            seen.add(id(d))
            op.deps.append(d)
            d.waited = True
        self.byeng[op.eng].append(op)
        self.nops += 1
        return op

    def op(self, eng, fn, reads=(), writes=()):
        return self._add(Op(eng, fn), reads, writes)

    def dma(self, eng, fn, sem, reads=(), writes=()):
        op = Op(eng, fn, is_dma=True)
        if sem not in self.dma_sems:
            self.dma_sems[sem] = self.nc.alloc_semaphore(name="ds_" + sem)
            self.dma_cnt[sem] = 0
        self.dma_cnt[sem] += 16
        op.dsem = sem
        op.dval = self.dma_cnt[sem]
        return self._add(op, reads, writes)

    def barrier(self):
        bs = []
        for e in CENGS:
            b = Op(e, None)
            b.waited = True
            for f in CENGS:
                if f != e:
                    for o in reversed(self.byeng[f]):
                        if not o.is_dma:
                            b.deps.append(o)
                            o.waited = True
                            break
            for s, c in self.dma_cnt.items():
                if c:
                    b.xdeps.append((s, c))
            bs.append(b)
        for b in bs:
            self.byeng[b.eng].append(b)
        for e in ENGS:
            c = Op(e, None)
            c.deps = list(bs)
            self.byeng[e].append(c)
        self.last_w = {}
        self.readers = {}

    def emit(self, block):
        for e in CENGS:
            for op in self.byeng[e]:
                if not op.is_dma and op.waited:
                    self.ecount[e] += 1
                    op.val = self.ecount[e]
        seen = {}

        def run(en, eng):
            def w(key, sem, v):
                if seen.get((en, key), 0) >= v:
                    return
                seen[(en, key)] = v
                eng.wait_ge(sem, v)

            for op in self.byeng[en]:
                for d in op.deps:
                    if d.is_dma:
                        w(("d", d.dsem), self.dma_sems[d.dsem], d.dval)
                    elif d.eng != "sp":
                        w(("e", d.eng), self.esem[d.eng], d.val)
                for (s, v) in op.xdeps:
                    w(("d", s), self.dma_sems[s], v)
                if op.fn is None:
                    if op.val is not None:
                        eng.nop().then_inc(self.esem[op.eng], 1)
                    continue
                ins = op.fn(eng)
                if op.is_dma:
                    ins.then_inc(self.dma_sems[op.dsem], 16)
                elif op.val is not None:
                    ins.then_inc(self.esem[op.eng], 1)

        @block.tensor
        def _(e):
            run("pe", e)

        @block.scalar
        def _(e):
            run("act", e)

        @block.vector
        def _(e):
            run("dve", e)

        @block.gpsimd
        def _(e):
            run("pool", e)

        @block.sync
        def _(e):
            run("sp", e)


class Arena:
    def __init__(self, big, n32):
        self.big = big
        self.n32 = n32
        self.off = 0
        self.mark = 0

    def set_mark(self):
        self.mark = self.off

    def reset(self):
        self.off = self.mark

    def alloc(self, n_elems, dtype):
        sz = 2 if dtype == BF16 else 4
        n32 = (n_elems * sz + 3) // 4
        n32 = (n32 + 7) // 8 * 8
        assert self.off + n32 <= self.n32, f"SBUF arena overflow {self.off}+{n32}>{self.n32}"
        ap = self.big[:, self.off:self.off + n32]
        self.off += n32
        if dtype != F32:
            ap = ap.bitcast(dtype)
        return ap[:, :n_elems]


class Ctx:
    pass


def build_program(phases=None, debug_out=False, odd_stages="abcd"):
    nc = bass.Bass("TRN2", target_bir_lowering=False)
    C = Ctx()
    C.nc = nc
    dt = nc.dram_tensor
    x = dt("x", [T, D], F32, kind="ExternalInput").ap()
    norm_gains = dt("norm_gains", [DEPTH * 3, D], F32, kind="ExternalInput").ap()
    w_gu = dt("ffn_w_gate_up", [DEPTH * 2, D, 2 * DFF], F32, kind="ExternalInput").ap()
    w_dn = dt("ffn_w_down", [DEPTH * 2, DFF, D], F32, kind="ExternalInput").ap()
    final_norm = dt("final_norm", [1, D], F32, kind="ExternalInput").ap()
    ident_d = dt("ident", [128, 128], F32, kind="ExternalInput").ap()
    ab_w_in = dt("ab_w_in", [2, D, 4096], F32, kind="ExternalInput").ap()
    ab_conv_w = dt("ab_conv_w", [2, 3, 1024], F32, kind="ExternalInput").ap()
    ab_pool_w = dt("ab_pool_w", [2, 4, 256, 256], F32, kind="ExternalInput").ap()
    ab_pool_scale = dt("ab_pool_scale", [2, 1024], F32, kind="ExternalInput").ap()
    ab_w_out = dt("ab_w_out", [2, D, D], F32, kind="ExternalInput").ap()
    invc0_d = dt("invc0", [128, 4 * TT], F32, kind="ExternalInput").ap()
    cd_w_in = dt("cd_w_in", [2, D, 7248], F32, kind="ExternalInput").ap()
    ret_norm_gain = dt("ret_norm_gain", [2, 1024], F32, kind="ExternalInput").ap()
    cd_w_out = dt("cd_w_out", [2, D, D], F32, kind="ExternalInput").ap()
    rope_d = dt("rope_tab", [2, 4, 2, 128, T], F32, kind="ExternalInput").ap()
    dd_d = dt("ret_diag", [128, 8 * 128], F32, kind="ExternalInput").ap()
    biasg_d = dt("bias_g", [128, 8 * 2 * 128], F32, kind="ExternalInput").ap()
    t15_d = dt("bias_t15", [128, 8], F32, kind="ExternalInput").ap()
    tab_d = dt("bias_tab", [128, 256], F32, kind="ExternalInput").ap()
    out = dt("out", [T, D], F32, kind="ExternalOutput").ap()
    hbuf = [dt("hA", [D, T], F32, kind="Internal").ap(), dt("hB", [D, T], F32, kind="Internal").ap()]
    NGC = DFF // 256
    NDC = D // 256
    NKF = DFF // 128
    wg_b = dt("wg_b", [DEPTH * 2, NGC, 128, NFC, 256], BF16, kind="Internal").ap()
    wu_b = dt("wu_b", [DEPTH * 2, NGC, 128, NFC, 256], BF16, kind="Internal").ap()
    wd_b = dt("wd_b", [DEPTH * 2, NDC, 128, NKF, 256], BF16, kind="Internal").ap()
    abc_b = dt("abc_b", [2, 8, 128, NFC, 384], BF16, kind="Internal").ap()
    abp_b = dt("abp_b", [2, 4, 128, NFC, 256], BF16, kind="Internal").ap()
    abo_b = dt("abo_b", [2, 8, 128, NFC, 256], BF16, kind="Internal").ap()
    cdF_b = dt("cdF_b", [2, 16, 128, NFC, 256], BF16, kind="Internal").ap()
    cdI_b = dt("cdI_b", [2, 128, NFC, 128], BF16, kind="Internal").ap()
    cdR_b = dt("cdR_b", [2, 16, 128, NFC, 128], BF16, kind="Internal").ap()
    cdT_b = dt("cdT_b", [2, 4, 128, NFC, 512], BF16, kind="Internal").ap()
    cdW_b = dt("cdW_b", [2, 128, NFC, 16], BF16, kind="Internal").ap()
    cdo_b = dt("cdo_b", [2, 8, 128, NFC, 256], BF16, kind="Internal").ap()
    qT_d = dt("qT_d", [1024, T], BF16, kind="Internal").ap()
    kT_d = dt("kT_d", [1024, T], BF16, kind="Internal").ap()
    iqT_d = dt("iqT_d", [1024, T], BF16, kind="Internal").ap()
    ikT_d = dt("ikT_d", [128, T], BF16, kind="Internal").ap()
    iw_d = dt("iw_d", [T, 16], F32, kind="Internal").ap()
    rqT_d = dt("rqT_d", [512, T], BF16, kind="Internal").ap()
    rkT_d = dt("rkT_d", [512, T], BF16, kind="Internal").ap()
    vc_d = dt("vc_d", [T, 1024], BF16, kind="Internal").ap()
    vr_d = dt("vr_d", [T, 1024], BF16, kind="Internal").ap()
    sg_d = dt("sg_d", [1024, T], F32, kind="Internal").ap()
    catT_d = dt("catT_d", [D, T], BF16, kind="Internal").ap()
    nmT_d = dt("nmT_d", [32, 128, 32 * 128], BF16, kind="Internal").ap()

    N32 = 51 * 1024 + 512
    es = nc.sbuf_tensor("big", [128, N32], F32)
    ps_cm = nc.psum_tensor("ps", [128, 8, 512], F32)
    with es as big, ps_cm as ps, nc.Block() as block:
        TR = Tracker(nc)
        A = Arena(big, N32)
        C.TR, C.A, C.ps = TR, A, ps
        C.psn = 0

        C.rot = list(range(8))

        def psbank():
            b = C.rot[C.psn % len(C.rot)]
            C.psn += 1
            return b
        C.psbank = psbank

        ident = A.alloc(128, F32)
        ones = A.alloc(128, F32)
        gains = A.alloc(13 * NFC, F32).rearrange("p (s f) -> p s f", f=NFC)
        TR.dma("sp", lambda e: e.dma_start(out=ident, in_=ident_d), "cid", writes=["ident"])
        TR.op("dve", lambda e: e.memset(ones, 1.0), writes=["ones"])
        for s in range(13):
            src = norm_gains[s] if s < 12 else final_norm[0]
            TR.dma("sp", lambda e, s=s, src=src: e.dma_start(
                out=gains[:, s, :], in_=src.rearrange("(f p) -> p f", p=128), allow_slow_non_contiguous=True),
                "const", writes=["gains"])
        C.ident, C.ones, C.gains = ident, ones, gains
        A.set_mark()

        def cast_ffn(fi):
            wv = w_gu[fi].rearrange("(kc p) n -> p kc n", p=128)
            for c in range(NGC):
                TR.dma("pool", lambda e, c=c: e.dma_start(out=wg_b[fi, c], in_=wv[:, :, c * 256:(c + 1) * 256]),
                       "cast", writes=[("wg", fi, c)])
                TR.dma("pool", lambda e, c=c: e.dma_start(out=wu_b[fi, c], in_=wv[:, :, DFF + c * 256:DFF + (c + 1) * 256]),
                       "cast", writes=[("wu", fi, c)])
            dv = w_dn[fi].rearrange("(kc p) n -> p kc n", p=128)
            for c in range(NDC):
                for k0 in range(0, NKF, 11):
                    TR.dma("pool", lambda e, c=c, k0=k0: e.dma_start(out=wd_b[fi, c, :, k0:k0 + 11, :],
                                                                      in_=dv[:, k0:k0 + 11, c * 256:(c + 1) * 256]),
                           "cast", writes=[("wd", fi, c, k0)])

        def rms_norm_tile(hsrc, ti, gslot, hT, xn, sq, rstd, out_dtype_bf16=True):
            t0 = ti * TT
            hv = hsrc.rearrange("(f p) t -> p f t", p=128)
            for q in range(4):
                TR.dma("sp", lambda e, q=q: e.dma_start(out=hT[:, q * 4:(q + 1) * 4, :], in_=hv[:, q * 4:(q + 1) * 4, t0:t0 + TT]),
                       "hT%d" % q, writes=[("hT", q)])
            b = psbank()
            for f in range(NFC):
                s = sq[f % 2]
                TR.op("act", lambda e, f=f, s=s: e.activation(out=s, in_=hT[:, f, :], func=AF.Square),
                      reads=[("hT", f // 4)], writes=[("sq", f % 2)])
                TR.op("pe", lambda e, f=f, s=s: e.matmul(ps[:, b, :], lhsT=ones, rhs=s, start=(f == 0), stop=(f == NFC - 1)),
                      reads=[("sq", f % 2), "ones"], writes=[("ps", b)])
            TR.op("dve", lambda e: e.tensor_scalar(out=rstd, in0=ps[:, b, :], scalar1=1.0 / D, scalar2=RMS_EPS,
                                                   op0=ALU.mult, op1=ALU.add), reads=[("ps", b)], writes=["rstd"])
            TR.op("act", lambda e: e.activation(out=rstd, in_=rstd, func=AF.Sqrt), reads=["rstd"], writes=["rstd"])
            TR.op("dve", lambda e: e.reciprocal(out=rstd, in_=rstd), reads=["rstd"], writes=["rstd"])
            for f in range(NFC):
                TR.op("dve", lambda e, f=f: e.scalar_tensor_tensor(out=xn[:, f, :], in0=hT[:, f, :], scalar=gains[:, gslot, f:f + 1],
                                                                 in1=rstd, op0=ALU.mult, op1=ALU.mult),
                      reads=[("hT", f // 4), "rstd", "gains"], writes=[("xn", f)])

        def residual_epilogue(hsrc, hdst, ti, fo, b, scale, epi, epi_n):
            t0 = ti * TT
            slot = epi_n[0] % len(epi)
            epi_n[0] += 1
            et = epi[slot]
            TR.dma("sp", lambda e: e.dma_start(out=et, in_=hsrc[fo * 128:(fo + 1) * 128, t0:t0 + TT]),
                   "epi%d" % slot, writes=[("epi", slot)])
            TR.op("dve", lambda e: e.scalar_tensor_tensor(out=et, in0=ps[:, b, :], scalar=float(scale), in1=et,
                                                          op0=ALU.mult, op1=ALU.add),
                  reads=[("ps", b)], writes=[("epi", slot)])
            TR.dma("sp", lambda e: e.dma_start(out=hdst[fo * 128:(fo + 1) * 128, t0:t0 + TT], in_=et),
                   "epi%d" % slot, reads=[("epi", slot)], writes=[("hdst", fo, ti)])

        def phase_prologue(hdst):
            A.reset()
            xt = [A.alloc(D, F32) for _ in range(2)]
            st = [A.alloc(TT, F32) for _ in range(3)]
            n = 0
            for tb in range(T // 128):
                xs = xt[tb % 2]
                TR.dma("sp", lambda e, xs=xs, tb=tb: e.dma_start(out=xs, in_=x[tb * 128:(tb + 1) * 128, :]),
                       "xt%d" % (tb % 2), writes=[("xt", tb % 2)])
                for f4 in range(NFC // 4):
                    b = psbank()
                    for k in range(4):
                        f = f4 * 4 + k
                        TR.op("pe", lambda e, xs=xs, f=f, k=k, b=b: e.transpose(out=ps[:, b, k * 128:(k + 1) * 128],
                                                                              in_=xs[:, f * 128:(f + 1) * 128], identity=ident),
                              reads=[("xt", tb % 2), "ident"], writes=[("ps", b)])
                    s = n % 3
                    n += 1
                    sb = st[s]
                    eng = "act" if n % 2 == 0 else "dve"
                    if eng == "act":
                        TR.op("act", lambda e, sb=sb, b=b: e.activation(out=sb, in_=ps[:, b, :], func=AF.Copy),
                              reads=[("ps", b)], writes=[("st", s)])
                    else:
                        TR.op("dve", lambda e, sb=sb, b=b: e.tensor_copy(out=sb, in_=ps[:, b, :]),
                              reads=[("ps", b)], writes=[("st", s)])
                    dst = hdst.rearrange("(f p) t -> p f t", p=128)[:, f4 * 4:(f4 + 1) * 4, tb * 128:(tb + 1) * 128]
                    TR.dma("sp", lambda e, sb=sb, dst=dst: e.dma_start(out=dst, in_=sb.rearrange("p (k t) -> p k t", t=128)),
                           "st%d" % s, reads=[("st", s)], writes=[("hdst", f4, tb)])

        def phase_ffn(fi, gslot, hsrc, hdst, next_cast=None):
            A.reset()
            hT = A.alloc(NFC * TT, F32).rearrange("p (f t) -> p f t", t=TT)
            xn = A.alloc(NFC * TT, BF16).rearrange("p (f t) -> p f t", t=TT)
            act = A.alloc(NKF * TT, BF16).rearrange("p (f t) -> p f t", t=TT)
            wg = [A.alloc(NFC * 256, BF16).rearrange("p (k n) -> p k n", n=256) for _ in range(2)]
            wu = [A.alloc(NFC * 256, BF16).rearrange("p (k n) -> p k n", n=256) for _ in range(2)]
            wd = [A.alloc(NKF * 256, BF16).rearrange("p (k n) -> p k n", n=256) for _ in range(2)]
            sq = [A.alloc(TT, F32) for _ in range(2)]
            rstd = A.alloc(TT, F32)
            sg = [A.alloc(TT, F32) for _ in range(2)]
            epi = [A.alloc(TT, F32) for _ in range(3)]
            epi_n = [0]
            wn = [0, 0]
            if next_cast is not None:
                next_cast()
            for ti in range(NT):
                rms_norm_tile(hsrc, ti, gslot, hT, xn, sq, rstd)
                for c in range(NGC):
                    s = wn[0] % 2
                    wn[0] += 1
                    TR.dma("sp", lambda e, c=c, s=s: e.dma_start(out=wg[s], in_=wg_b[fi, c]), "wg%d" % s,
                           reads=[("wg", fi, c)], writes=[("wgs", s)])
                    TR.dma("sp", lambda e, c=c, s=s: e.dma_start(out=wu[s], in_=wu_b[fi, c]), "wu%d" % s,
                           reads=[("wu", fi, c)], writes=[("wus", s)])
                    for hf in range(2):
                        bg = psbank()
                        bu = psbank()
                        for k in range(NFC):
                            TR.op("pe", lambda e, s=s, hf=hf, k=k, bg=bg: e.matmul(
                                ps[:, bg, :], lhsT=wg[s][:, k, hf * 128:(hf + 1) * 128], rhs=xn[:, k, :],
                                start=(k == 0), stop=(k == NFC - 1)),
                                reads=[("wgs", s), ("xn", k)], writes=[("ps", bg)])
                        for k in range(NFC):
                            TR.op("pe", lambda e, s=s, hf=hf, k=k, bu=bu: e.matmul(
                                ps[:, bu, :], lhsT=wu[s][:, k, hf * 128:(hf + 1) * 128], rhs=xn[:, k, :],
                                start=(k == 0), stop=(k == NFC - 1)),
                                reads=[("wus", s), ("xn", k)], writes=[("ps", bu)])
                        j = c * 2 + hf
                        sgt = sg[j % 2]
                        TR.op("act", lambda e, sgt=sgt, bg=bg: e.activation(out=sgt, in_=ps[:, bg, :], func=AF.Silu),
                              reads=[("ps", bg)], writes=[("sg", j % 2)])
                        TR.op("dve", lambda e, sgt=sgt, bu=bu, j=j: e.tensor_tensor(out=act[:, j, :], in0=ps[:, bu, :], in1=sgt,
                                                                                     op=ALU.mult),
                              reads=[("ps", bu), ("sg", j % 2)], writes=[("act", j)])
                for c in range(NDC):
                    s = wn[1] % 2
                    wn[1] += 1
                    for k0 in range(0, NKF, 11):
                        TR.dma("sp", lambda e, c=c, s=s, k0=k0: e.dma_start(out=wd[s][:, k0:k0 + 11, :], in_=wd_b[fi, c, :, k0:k0 + 11, :]),
                               "wd%d_%d" % (s, k0 // 11), reads=[("wd", fi, c, k0)], writes=[("wds", s, k0)])
                    for hf in range(2):
                        b = psbank()
                        for k in range(NKF):
                            TR.op("pe", lambda e, s=s, hf=hf, k=k, b=b: e.matmul(
                                ps[:, b, :], lhsT=wd[s][:, k, hf * 128:(hf + 1) * 128], rhs=act[:, k, :],
                                start=(k == 0), stop=(k == NKF - 1)),
                                reads=[("wds", s, (k // 11) * 11), ("act", k)], writes=[("ps", b)])
                        residual_epilogue(hsrc, hdst, ti, c * 2 + hf, b, 0.5, epi, epi_n)

        def cast_even(j):
            wv = ab_w_in[j].rearrange("(kc p) n -> p kc n", p=128)
            for jj in range(8):
                for wh in range(3):
                    c0 = wh * 1024 + jj * 128
                    TR.dma("pool", lambda e, jj=jj, wh=wh, c0=c0: e.dma_start(out=abc_b[j, jj, :, :, wh * 128:(wh + 1) * 128],
                                                                             in_=wv[:, :, c0:c0 + 128]),
                           "cast", writes=[("abc", j, jj, wh)])
            for g in range(4):
                c0 = 3072 + g * 256
                TR.dma("pool", lambda e, g=g, c0=c0: e.dma_start(out=abp_b[j, g], in_=wv[:, :, c0:c0 + 256]),
                       "cast", writes=[("abp", j, g)])
            ov = ab_w_out[j].rearrange("(kc p) n -> p kc n", p=128)
            for c in range(8):
                TR.dma("pool", lambda e, c=c: e.dma_start(out=abo_b[j, c], in_=ov[:, :, c * 256:(c + 1) * 256]),
                       "cast", writes=[("abo", j, c)])

        def phase_mix_even(layer, hsrc, hdst, next_cast=None):
            j = layer // 2
            gslot = layer * 3 + 1
            A.reset()
            hT = A.alloc(NFC * TT, F32).rearrange("p (f t) -> p f t", t=TT)
            xn = A.alloc(NFC * TT, BF16).rearrange("p (f t) -> p f t", t=TT)
            cat = A.alloc(NFC * TT, BF16).rearrange("p (f t) -> p f t", t=TT)
            ucat = A.alloc(8 * 514, F32).rearrange("p (f t) -> p f t", t=514)
            pcat = A.alloc(8 * 527, F32).rearrange("p (f t) -> p f t", t=527)
            wc = [A.alloc(NFC * 384, BF16).rearrange("p (k n) -> p k n", n=384) for _ in range(2)]
            wp = [A.alloc(NFC * 256, BF16).rearrange("p (k n) -> p k n", n=256) for _ in range(2)]
            wo = [A.alloc(NFC * 256, BF16).rearrange("p (k n) -> p k n", n=256) for _ in range(2)]
            pw = A.alloc(8 * 256, BF16).rearrange("p (g n) -> p g n", n=256)
            cw = A.alloc(24, F32).rearrange("p (k j) -> p k j", j=8)
            psc = A.alloc(8, F32)
            invc0 = A.alloc(4 * TT, F32).rearrange("p (g t) -> p g t", t=TT)
            sq = [A.alloc(TT, F32) for _ in range(2)]
            rstd = A.alloc(TT, F32)
            tmp1 = A.alloc(TT, F32)
            yv = A.alloc(TT, F32)
            tA = A.alloc(528, F32)
            tB = A.alloc(528, F32)
            pl = A.alloc(2 * TT, BF16).rearrange("p (i t) -> p i t", t=TT)
            epi = [A.alloc(TT, F32) for _ in range(3)]
            epi_n = [0]
            wn = [0, 0, 0]
            TR.dma("pool", lambda e: e.dma_start(out=pw, in_=ab_pool_w[j].rearrange("g (i p) n -> p (g i) n", p=128)),
                   "oc4", writes=["pw"])
            TR.dma("sp", lambda e: e.dma_start(out=cw, in_=ab_conv_w[j].rearrange("k (j p) -> p k j", p=128),
                                               allow_slow_non_contiguous=True), "oc5", writes=["cw"])
            TR.dma("sp", lambda e: e.dma_start(out=psc, in_=ab_pool_scale[j].rearrange("(m p) -> p m", p=128),
                                               allow_slow_non_contiguous=True), "oc0", writes=["psc"])
            TR.dma("sp", lambda e: e.dma_start(out=invc0.rearrange("p g t -> p (g t)"), in_=invc0_d), "oc1", writes=["invc0"])
            TR.op("dve", lambda e: e.memset(ucat.rearrange("p f t -> p (f t)"), 0.0), writes=[("ucat", m) for m in range(8)])
            TR.op("dve", lambda e: e.memset(pcat.rearrange("p f t -> p (f t)"), 0.0), writes=[("pcat", m) for m in range(8)])
            if next_cast is not None:
                next_cast()
            for ti in range(NT):
                rms_norm_tile(hsrc, ti, gslot, hT, xn, sq, rstd)
                for jj in range(8):
                    s = wn[0] % 2
                    wn[0] += 1
                    TR.dma("sp", lambda e, jj=jj, s=s: e.dma_start(out=wc[s], in_=abc_b[j, jj]), "wc%d" % s,
                           reads=[("abc", j, jj, wh) for wh in range(3)], writes=[("wcs", s)])
                    banks = []
                    for wh in range(3):
                        b = psbank()
                        banks.append(b)
                        for k in range(NFC):
                            TR.op("pe", lambda e, s=s, wh=wh, k=k, b=b: e.matmul(
                                ps[:, b, :], lhsT=wc[s][:, k, wh * 128:(wh + 1) * 128], rhs=xn[:, k, :],
                                start=(k == 0), stop=(k == NFC - 1)),
                                reads=[("wcs", s), ("xn", k)], writes=[("ps", b)])
                    bb, bc, bh = banks
                    uk = ("ucat", jj)
                    TR.op("act", lambda e, bc=bc: e.activation(out=tmp1, in_=ps[:, bc, :], func=AF.Copy),
                          reads=[("ps", bc)], writes=["tmp1"])
                    TR.op("dve", lambda e, jj=jj, bh=bh: e.tensor_tensor(out=ucat[:, jj, 2:514], in0=ps[:, bh, :], in1=tmp1, op=ALU.mult),
                          reads=[("ps", bh), "tmp1"], writes=[uk])
                    TR.op("dve", lambda e, jj=jj: e.tensor_scalar(out=yv, in0=ucat[:, jj, 0:512], scalar1=cw[:, 0, jj:jj + 1], scalar2=None,
                                                                  op0=ALU.mult), reads=[uk, "cw"], writes=["yv"])
                    TR.op("dve", lambda e, jj=jj: e.scalar_tensor_tensor(out=yv, in0=ucat[:, jj, 1:513], scalar=cw[:, 1, jj:jj + 1], in1=yv,
                                                                         op0=ALU.mult, op1=ALU.add), reads=[uk, "cw", "yv"], writes=["yv"])
                    TR.op("dve", lambda e, jj=jj: e.scalar_tensor_tensor(out=yv, in0=ucat[:, jj, 2:514], scalar=cw[:, 2, jj:jj + 1], in1=yv,
                                                                         op0=ALU.mult, op1=ALU.add), reads=[uk, "cw", "yv"], writes=["yv"])
                    TR.op("dve", lambda e, jj=jj, bb=bb: e.tensor_tensor(out=cat[:, jj, :], in0=ps[:, bb, :], in1=yv, op=ALU.mult),
                          reads=[("ps", bb), "yv"], writes=[("cat", jj)])
                    TR.op("act", lambda e, jj=jj: e.activation(out=ucat[:, jj, 0:2], in_=ucat[:, jj, 512:514], func=AF.Copy),
                          reads=[uk], writes=[uk])
                for g in range(4):
                    W = (2, 4, 8, 16)[g]
                    s = wn[1] % 2
                    wn[1] += 1
                    TR.dma("sp", lambda e, g=g, s=s: e.dma_start(out=wp[s], in_=abp_b[j, g]), "wp%d" % s,
                           reads=[("abp", j, g)], writes=[("wps", s)])
                    for ic in range(2):
                        m = 2 * g + ic
                        pk = ("pcat", m)
                        b = psbank()
                        for k in range(NFC):
                            TR.op("pe", lambda e, s=s, ic=ic, k=k, b=b: e.matmul(
                                ps[:, b, :], lhsT=wp[s][:, k, ic * 128:(ic + 1) * 128], rhs=xn[:, k, :],
                                start=(k == 0), stop=(k == NFC - 1)),
                                reads=[("wps", s), ("xn", k)], writes=[("ps", b)])
                        TR.op("act", lambda e, m=m, b=b: e.activation(out=pcat[:, m, 15:527], in_=ps[:, b, :], func=AF.Copy),
                              reads=[("ps", b)], writes=[pk])
                        lo = 15 - (W - 1)
                        ln = 512 + W - 1
                        cur = pcat[:, m, lo:lo + ln]
                        curkey = pk
                        st = 1
                        bufs = [(tA, "tA"), (tB, "tB")]
                        bi = 0
                        while st < W:
                            nb, nk = bufs[bi]
                            bi ^= 1
                            nl = ln - st
                            TR.op("dve", lambda e, cur=cur, nb=nb, st=st, nl=nl: e.tensor_tensor(
                                out=nb[:, 0:nl], in0=cur[:, st:st + nl], in1=cur[:, 0:nl], op=ALU.add),
                                reads=[curkey], writes=[nk])
                            cur, curkey, ln = nb[:, 0:nl], nk, nl
                            st *= 2
                        assert ln == 512
                        if ti == 0:
                            TR.op("dve", lambda e, cur=cur, g=g: e.tensor_tensor(out=yv, in0=cur, in1=invc0[:, g, :], op=ALU.mult),
                                  reads=[curkey, "invc0"], writes=["yv"])
                            TR.op("dve", lambda e, m=m, ic=ic: e.tensor_tensor(out=pl[:, ic, :], in0=yv, in1=pcat[:, m, 15:527], op=ALU.subtract),
                                  reads=["yv", pk], writes=[("pl", ic)])
                        else:
                            TR.op("dve", lambda e, cur=cur, m=m, ic=ic, W=W: e.scalar_tensor_tensor(
                                out=pl[:, ic, :], in0=cur, scalar=1.0 / W, in1=pcat[:, m, 15:527], op0=ALU.mult, op1=ALU.subtract),
                                reads=[curkey, pk], writes=[("pl", ic)])
                        TR.op("act", lambda e, m=m: e.activation(out=pcat[:, m, 0:15], in_=pcat[:, m, 512:527], func=AF.Copy),
                              reads=[pk], writes=[pk])
                    for oc in range(2):
                        b = psbank()
                        for ic in range(2):
                            TR.op("pe", lambda e, g=g, ic=ic, oc=oc, b=b: e.matmul(
                                ps[:, b, :], lhsT=pw[:, 2 * g + ic, oc * 128:(oc + 1) * 128], rhs=pl[:, ic, :],
                                start=(ic == 0), stop=(ic == 1)),
                                reads=["pw", ("pl", ic)], writes=[("ps", b)])
                        mo = 2 * g + oc
                        TR.op("act", lambda e, mo=mo, b=b: e.activation(out=cat[:, 8 + mo, :], in_=ps[:, b, :], func=AF.Copy,
                                                                        scale=psc[:, mo:mo + 1]),
                              reads=[("ps", b), "psc"], writes=[("cat", 8 + mo)])
                for c in range(8):
                    s = wn[2] % 2
                    wn[2] += 1
                    TR.dma("sp", lambda e, c=c, s=s: e.dma_start(out=wo[s], in_=abo_b[j, c]), "wo%d" % s,
                           reads=[("abo", j, c)], writes=[("wos", s)])
                    for hf in range(2):
                        b = psbank()
                        for k in range(NFC):
                            TR.op("pe", lambda e, s=s, hf=hf, k=k, b=b: e.matmul(
                                ps[:, b, :], lhsT=wo[s][:, k, hf * 128:(hf + 1) * 128], rhs=cat[:, k, :],
                                start=(k == 0), stop=(k == NFC - 1)),
                                reads=[("wos", s), ("cat", k)], writes=[("ps", b)])
                        residual_epilogue(hsrc, hdst, ti, c * 2 + hf, b, 1.0, epi, epi_n)

        NQB = T // 128
        LN_EPS = 1e-5
        ASCALE = 128 ** -0.5
        GAM = [1.0 - 2.0 ** (-5.0 - h) for h in range(8)]

        def cast_odd(j):
            wv = cd_w_in[j].rearrange("(kc p) n -> p kc n", p=128)
            fcols = [0, 256, 512, 768, 1024, 1280, 1536, 1792, 3072, 3328, 3584, 3840, 6224, 6480, 6736, 6992]
            for g, c0 in enumerate(fcols):
                TR.dma("pool", lambda e, g=g, c0=c0: e.dma_start(out=cdF_b[j, g], in_=wv[:, :, c0:c0 + 256]),
                       "cast", writes=[("cdF", j, g)])
            for hh in range(2):
                TR.dma("pool", lambda e, hh=hh: e.dma_start(out=cdI_b[j, :, :, hh * 64:(hh + 1) * 64], in_=wv[:, :, 4096:4160]),
                       "cast", writes=[("cdI", j, hh)])
            for qk in range(2):
                base = 4176 + qk * 512
                for c in range(4):
                    TR.dma("pool", lambda e, qk=qk, c=c, base=base: e.dma_start(out=cdR_b[j, qk * 8 + c], in_=wv[:, :, base + c * 128:base + (c + 1) * 128]),
                           "cast", writes=[("cdR", j, qk * 8 + c, 0)])
                    for hh in range(2):
                        for half in range(2):
                            d0 = hh * 64 + half * 32
                            s0 = base + c * 128 + hh * 64 + (1 - half) * 32
                            TR.dma("pool", lambda e, qk=qk, c=c, d0=d0, s0=s0: e.dma_start(
                                out=cdR_b[j, qk * 8 + 4 + c, :, :, d0:d0 + 32], in_=wv[:, :, s0:s0 + 32]),
                                "cast", writes=[("cdR", j, qk * 8 + 4 + c, hh * 2 + half)])
            for g, c0 in enumerate([2048, 2560, 5200, 5712]):
                TR.dma("pool", lambda e, g=g, c0=c0: e.dma_start(out=cdT_b[j, g], in_=wv[:, :, c0:c0 + 512]),
                       "cast", writes=[("cdT", j, g)])
            TR.dma("pool", lambda e: e.dma_start(out=cdW_b[j], in_=wv[:, :, 4160:4176]), "cast", writes=[("cdW", j)])
            ov = cd_w_out[j].rearrange("(kc p) n -> p kc n", p=128)
            for c in range(8):
                TR.dma("pool", lambda e, c=c: e.dma_start(out=cdo_b[j, c], in_=ov[:, :, c * 256:(c + 1) * 256]),
                       "cast", writes=[("cdo", j, c)])

        def odd_stage_a(layer, hsrc):
            j = layer // 2
            gslot = layer * 3 + 1
            A.reset()
            hT = A.alloc(NFC * TT, F32).rearrange("p (f t) -> p f t", t=TT)
            xn = A.alloc(NFC * TT, BF16).rearrange("p (f t) -> p f t", t=TT)
            sq = [A.alloc(TT, F32) for _ in range(2)]
            rstd = A.alloc(TT, F32)
            wF = [A.alloc(NFC * 256, BF16).rearrange("p (k n) -> p k n", n=256) for _ in range(2)]
            wR = [A.alloc(NFC * 256, BF16).rearrange("p (a k n) -> p a k n", a=2, n=128) for _ in range(2)]
            wT = [A.alloc(NFC * 512, BF16).rearrange("p (k n) -> p k n", n=512) for _ in range(2)]
            wI = A.alloc(NFC * 128, BF16).rearrange("p (k n) -> p k n", n=128)
            wW = A.alloc(NFC * 16, BF16).rearrange("p (k n) -> p k n", n=16)
            stg = [A.alloc(TT, BF16) for _ in range(4)]
            stf = [A.alloc(TT, F32) for _ in range(3)]
            rtab = [A.alloc(2 * TT, F32).rearrange("p (a t) -> p a t", t=TT) for _ in range(2)]
            t1 = A.alloc(TT, F32)
            t2 = A.alloc(TT, F32)
            iwt = A.alloc(16, F32)
            cnt = {"F": 0, "R": 0, "T": 0, "stg": 0, "stf": 0, "rt": 0}
            TR.dma("sp", lambda e: e.dma_start(out=wI, in_=cdI_b[j]), "oc1", reads=[("cdI", j, 0), ("cdI", j, 1)], writes=["wI"])
            TR.dma("sp", lambda e: e.dma_start(out=wW, in_=cdW_b[j]), "oc2", reads=[("cdW", j)], writes=["wW"])

            def fm_group(ti, lhs_fn, wkeys, evac):
                b = psbank()
                for k in range(NFC):
                    TR.op("pe", lambda e, k=k, b=b: e.matmul(ps[:, b, :], lhsT=lhs_fn(k), rhs=xn[:, k, :],
                                                             start=(k == 0), stop=(k == NFC - 1)),
                          reads=list(wkeys) + [("xn", k)], writes=[("ps", b)])
                evac(b)

            def store_bf(ti, b, dst_rows, func=AF.Copy, use_act=True):
                sl = cnt["stg"] % 4
                cnt["stg"] += 1
                sb = stg[sl]
                if use_act:
                    TR.op("act", lambda e: e.activation(out=sb, in_=ps[:, b, :], func=func), reads=[("ps", b)], writes=[("stg", sl)])
                else:
                    TR.op("dve", lambda e: e.tensor_copy(out=sb, in_=ps[:, b, :]), reads=[("ps", b)], writes=[("stg", sl)])
                TR.dma("sp", lambda e: e.dma_start(out=dst_rows[:, ti * TT:(ti + 1) * TT], in_=sb), "stg%d" % sl,
                       reads=[("stg", sl)], writes=[("scr", id(dst_rows), ti)])

            for ti in range(NT):
                rms_norm_tile(hsrc, ti, gslot, hT, xn, sq, rstd)
                for g in range(16):
                    s_ = cnt["F"] % 2
                    cnt["F"] += 1
                    TR.dma("sp", lambda e, g=g, s_=s_: e.dma_start(out=wF[s_], in_=cdF_b[j, g]), "wF%d" % s_,
                           reads=[("cdF", j, g)], writes=[("wFs", s_)])
                    for hf in range(2):
                        ch = (g % 4) * 2 + hf
                        kind = g // 4
                        if kind < 3:
                            dst = (qT_d, kT_d, iqT_d)[kind][ch * 128:(ch + 1) * 128, :]
                            fm_group(ti, lambda k, s_=s_, hf=hf: wF[s_][:, k, hf * 128:(hf + 1) * 128], [("wFs", s_)],
                                     lambda b, dst=dst, ch=ch: store_bf(ti, b, dst, use_act=(ch % 2 == 0)))
                        else:
                            def ev(b, ch=ch, ti=ti):
                                sl = cnt["stf"] % 3
                                cnt["stf"] += 1
                                sb = stf[sl]
                                TR.op("act", lambda e: e.activation(out=sb, in_=ps[:, b, :], func=AF.Silu), reads=[("ps", b)], writes=[("stf", sl)])
                                TR.dma("sp", lambda e: e.dma_start(out=sg_d[ch * 128:(ch + 1) * 128, ti * TT:(ti + 1) * TT], in_=sb),
                                       "stf%d" % sl, reads=[("stf", sl)], writes=[("sg_d", ch, ti)])
                            fm_group(ti, lambda k, s_=s_, hf=hf: wF[s_][:, k, hf * 128:(hf + 1) * 128], [("wFs", s_)], ev)
                fm_group(ti, lambda k: wI[:, k, :], ["wI"], lambda b: store_bf(ti, b, ikT_d))
                for qk in range(2):
                    for c in range(4):
                        s_ = cnt["R"] % 2
                        cnt["R"] += 1
                        TR.dma("sp", lambda e, qk=qk, c=c, s_=s_: e.dma_start(out=wR[s_][:, 0], in_=cdR_b[j, qk * 8 + c]), "wR%d_0" % s_,
                               reads=[("cdR", j, qk * 8 + c, 0)], writes=[("wRs", s_, 0)])
                        TR.dma("sp", lambda e, qk=qk, c=c, s_=s_: e.dma_start(out=wR[s_][:, 1], in_=cdR_b[j, qk * 8 + 4 + c]), "wR%d_1" % s_,
                               reads=[("cdR", j, qk * 8 + 4 + c, q) for q in range(4)], writes=[("wRs", s_, 1)])
                        r_ = cnt["rt"] % 2
                        cnt["rt"] += 1
                        TR.dma("sp", lambda e, qk=qk, c=c, r_=r_, ti=ti: e.dma_start(out=rtab[r_], in_=rope_d[qk, c].rearrange("a p t -> p a t")[:, :, ti * TT:(ti + 1) * TT]),
                               "rt%d" % r_, writes=[("rtab", r_)])
                        bA = psbank()
                        bB = psbank()
                        for ab, b in ((0, bA), (1, bB)):
                            for k in range(NFC):
                                TR.op("pe", lambda e, k=k, b=b, ab=ab, s_=s_: e.matmul(ps[:, b, :], lhsT=wR[s_][:, ab, k, :], rhs=xn[:, k, :],
                                                                                        start=(k == 0), stop=(k == NFC - 1)),
                                      reads=[("wRs", s_, ab), ("xn", k)], writes=[("ps", b)])
                        TR.op("dve", lambda e, bA=bA, r_=r_: e.tensor_tensor(out=t1, in0=ps[:, bA, :], in1=rtab[r_][:, 0, :], op=ALU.mult),
                              reads=[("ps", bA), ("rtab", r_)], writes=["t1"])
                        TR.op("dve", lambda e, bB=bB, r_=r_: e.tensor_tensor(out=t2, in0=ps[:, bB, :], in1=rtab[r_][:, 1, :], op=ALU.mult),
                              reads=[("ps", bB), ("rtab", r_)], writes=["t2"])
                        sl = cnt["stg"] % 4
                        cnt["stg"] += 1
                        sb = stg[sl]
                        TR.op("dve", lambda e, sb=sb: e.tensor_tensor(out=sb, in0=t1, in1=t2, op=ALU.add), reads=["t1", "t2"], writes=[("stg", sl)])
                        dst = (rqT_d, rkT_d)[qk]
                        TR.dma("sp", lambda e, sb=sb, dst=dst, c=c, ti=ti: e.dma_start(out=dst[c * 128:(c + 1) * 128, ti * TT:(ti + 1) * TT], in_=sb),
                               "stg%d" % sl, reads=[("stg", sl)], writes=[("rqk", qk, c, ti)])
                for g in range(4):
                    s_ = cnt["T"] % 2
                    cnt["T"] += 1
                    TR.dma("sp", lambda e, g=g, s_=s_: e.dma_start(out=wT[s_], in_=cdT_b[j, g]), "wT%d" % s_,
                           reads=[("cdT", j, g)], writes=[("wTs", s_)])
                    dst = (vc_d, vr_d)[g // 2]
                    for tb in range(4):
                        b = psbank()
                        for k in range(NFC):
                            TR.op("pe", lambda e, k=k, b=b, tb=tb, s_=s_: e.matmul(ps[:, b, :], lhsT=xn[:, k, tb * 128:(tb + 1) * 128], rhs=wT[s_][:, k, :],
                                                                                    start=(k == 0), stop=(k == NFC - 1)),
                                  reads=[("wTs", s_), ("xn", k)], writes=[("ps", b)])
                        sl = cnt["stg"] % 4
                        cnt["stg"] += 1
                        sb = stg[sl]
                        if tb % 2 == 0:
                            TR.op("act", lambda e, sb=sb, b=b: e.activation(out=sb, in_=ps[:, b, :], func=AF.Copy), reads=[("ps", b)], writes=[("stg", sl)])
                        else:
                            TR.op("dve", lambda e, sb=sb, b=b: e.tensor_copy(out=sb, in_=ps[:, b, :]), reads=[("ps", b)], writes=[("stg", sl)])
                        r0 = ti * TT + tb * 128
                        c0 = (g % 2) * 512
                        TR.dma("sp", lambda e, sb=sb, dst=dst, r0=r0, c0=c0: e.dma_start(out=dst[r0:r0 + 128, c0:c0 + 512], in_=sb),
                               "stg%d" % sl, reads=[("stg", sl)], writes=[("v_d", g, r0)])
                for tb in range(4):
                    b = psbank()
                    for k in range(NFC):
                        TR.op("pe", lambda e, k=k, b=b, tb=tb: e.matmul(ps[:, b, 0:16], lhsT=xn[:, k, tb * 128:(tb + 1) * 128], rhs=wW[:, k, :],
                                                                         start=(k == 0), stop=(k == NFC - 1)),
                              reads=["wW", ("xn", k)], writes=[("ps", b)])
                    TR.op("act", lambda e, b=b: e.activation(out=iwt, in_=ps[:, b, 0:16], func=AF.Copy, scale=0.25 * 0.125),
                          reads=[("ps", b)], writes=["iwt"])
                    r0 = ti * TT + tb * 128
                    TR.dma("sp", lambda e, r0=r0: e.dma_start(out=iw_d[r0:r0 + 128, :], in_=iwt), "iwt", reads=["iwt"], writes=[("iw_d", r0)])

        def odd_stage_b(layer):
            j = layer // 2
            A.reset()
            qh = [A.alloc(T, BF16) for _ in range(2)]
            kh = [A.alloc(T, BF16) for _ in range(2)]
            vh = [A.alloc(NQB * 128, BF16).rearrange("p (j e) -> p j e", e=128) for _ in range(2)]
            dd = A.alloc(8 * 128, F32).rearrange("p (h i) -> p h i", i=128)
            rgain = A.alloc(8, F32)
            at = [A.alloc(128, BF16) for _ in range(4)]
            oT = [A.alloc(TT, F32) for _ in range(2)]
            cen = A.alloc(TT, F32)
            sqv = A.alloc(TT, F32)
            rs = A.alloc(TT, F32)
            sgt = [A.alloc(TT, F32) for _ in range(2)]
            yb = [A.alloc(TT, BF16) for _ in range(2)]
            for s0_ in range(2):
                TR.op("dve", lambda e, s0_=s0_: e.memset(qh[s0_], 0.0), writes=[("qh", s0_)])
                TR.op("dve", lambda e, s0_=s0_: e.memset(kh[s0_], 0.0), writes=[("kh", s0_)])
            TR.dma("sp", lambda e: e.dma_start(out=dd.rearrange("p h i -> p (h i)"), in_=dd_d), "oc3", writes=["dd"])
            TR.dma("sp", lambda e: e.dma_start(out=rgain, in_=ret_norm_gain[j].rearrange("(h p) -> p h", p=128), allow_slow_non_contiguous=True),
                   "oc4", writes=["rgain"])
            n_at = 0
            n_o = 0
            C.rot = [0, 1, 2, 3, 4, 5]
            npo = [0]
            import os as _os
            for h in range(int(_os.environ.get("ODD_B_HEADS", "8"))):
                s_ = h % 2
                TR.dma("sp", lambda e, h=h, s_=s_: e.dma_start(out=qh[s_][0:64, :], in_=rqT_d[h * 64:(h + 1) * 64, :]), "rq%d" % s_, writes=[("qh", s_)])
                TR.dma("sp", lambda e, h=h, s_=s_: e.dma_start(out=kh[s_][0:64, :], in_=rkT_d[h * 64:(h + 1) * 64, :]), "rk%d" % s_, writes=[("kh", s_)])
                for q4 in range(4):
                    TR.dma("sp", lambda e, h=h, s_=s_, q4=q4: e.dma_start(out=vh[s_][:, q4 * 8:(q4 + 1) * 8, :],
                                                                        in_=vr_d.rearrange("(j p) c -> p j c", p=128)[:, q4 * 8:(q4 + 1) * 8, h * 128:(h + 1) * 128]),
                           "vv%d_%d" % (s_, q4), writes=[("vh", s_, q4)])
                for tg in range(int(_os.environ.get("ODD_B_TG", str(NT)))):
                    os_ = n_o % 2
                    n_o += 1
                    _skip = _os.environ.get("ODD_B_SKIP", "")
                    if "qk" in _skip:
                        TR.op("dve", lambda e, os_=os_: e.memset(oT[os_], 1.0), writes=[("oT", os_)])
                    for ib in range(int(_os.environ.get("ODD_B_IB", "4")) if "qk" not in _skip else 0):
                        I = tg * 4 + ib
                        po = 6 + npo[0] % 2
                        npo[0] += 1
                        for J in range(I + 1):
                            b = psbank()
                            jj = 0
                            TR.op("pe", lambda e, b=b, J=J, I=I, s_=s_: e.matmul(
                                ps[:, b, 0:128], lhsT=kh[s_][:, J * 128:(J + 1) * 128], rhs=qh[s_][:, I * 128:(I + 1) * 128],
                                start=True, stop=True), reads=[("kh", s_), ("qh", s_)], writes=[("ps", b)])
                            a_ = n_at % 4
                            n_at += 1
                            ab = at[a_]
                            if J < I:
                                sc = float(GAM[h] ** (128 * (I - J)))
                                TR.op("act", lambda e, ab=ab, b=b, sc=sc: e.activation(out=ab, in_=ps[:, b, 0:128], func=AF.Copy, scale=sc),
                                      reads=[("ps", b)], writes=[("at", a_)])
                            else:
                                TR.op("dve", lambda e, ab=ab, b=b, h=h: e.tensor_tensor(out=ab, in0=ps[:, b, 0:128], in1=dd[:, h, :], op=ALU.mult),
                                      reads=[("ps", b), "dd"], writes=[("at", a_)])
                            TR.op("pe", lambda e, ab=ab, po=po, J=J, I=I, s_=s_: e.matmul(ps[:, po, 0:128], lhsT=vh[s_][:, J, :], rhs=ab,
                                                                                           start=(J == 0), stop=(J == I)),
                                  reads=[("vh", s_, J // 8), ("at", a_)], writes=[("ps", po)])
                        TR.op("act", lambda e, po=po, ib=ib, os_=os_: e.activation(out=oT[os_][:, ib * 128:(ib + 1) * 128], in_=ps[:, po, 0:128], func=AF.Copy),
                              reads=[("ps", po)], writes=[("oT", os_)])
                    o_ = oT[os_]
                    if "gn" in _skip:
                        TR.dma("sp", lambda e, o_=o_, h=h, tg=tg: e.dma_start(out=sg_d[h * 128:(h + 1) * 128, tg * TT:(tg + 1) * TT], in_=o_),
                               "yb%d" % os_, reads=[("oT", os_)], writes=[("catT", 8 + h, tg)])
                        continue
                    b1 = psbank()
                    TR.op("pe", lambda e, b1=b1, o_=o_: e.matmul(ps[:, b1, :], lhsT=ones, rhs=o_, start=True, stop=True),
                          reads=[("oT", os_), "ones"], writes=[("ps", b1)])
                    TR.op("dve", lambda e, b1=b1, o_=o_: e.scalar_tensor_tensor(out=cen, in0=ps[:, b1, :], scalar=-1.0 / 128, in1=o_, op0=ALU.mult, op1=ALU.add),
                          reads=[("ps", b1), ("oT", os_)], writes=["cen"])
                    TR.op("act", lambda e: e.activation(out=sqv, in_=cen, func=AF.Square), reads=["cen"], writes=["sqv"])
                    b2 = psbank()
                    TR.op("pe", lambda e, b2=b2: e.matmul(ps[:, b2, :], lhsT=ones, rhs=sqv, start=True, stop=True),
                          reads=["sqv", "ones"], writes=[("ps", b2)])
                    TR.op("dve", lambda e, b2=b2: e.tensor_scalar(out=rs, in0=ps[:, b2, :], scalar1=1.0 / 128, scalar2=LN_EPS, op0=ALU.mult, op1=ALU.add),
                          reads=[("ps", b2)], writes=["rs"])
                    TR.op("act", lambda e: e.activation(out=rs, in_=rs, func=AF.Sqrt), reads=["rs"], writes=["rs"])
                    TR.op("dve", lambda e: e.reciprocal(out=rs, in_=rs), reads=["rs"], writes=["rs"])
                    TR.op("dve", lambda e: e.tensor_tensor(out=cen, in0=cen, in1=rs, op=ALU.mult), reads=["cen", "rs"], writes=["cen"])
                    g_ = sgt[os_]
                    TR.dma("sp", lambda e, g_=g_, h=h, tg=tg: e.dma_start(out=g_, in_=sg_d[h * 128:(h + 1) * 128, tg * TT:(tg + 1) * TT]), "sgt%d" % os_,
                           writes=[("sgt", os_)])
                    y_ = yb[os_]
                    TR.op("dve", lambda e, g_=g_, y_=y_, h=h: e.scalar_tensor_tensor(out=y_, in0=cen, scalar=rgain[:, h:h + 1], in1=g_, op0=ALU.mult, op1=ALU.mult),
                          reads=["cen", "rgain", ("sgt", os_)], writes=[("yb", os_)])
                    TR.dma("sp", lambda e, y_=y_, h=h, tg=tg: e.dma_start(out=catT_d[1024 + h * 128:1024 + (h + 1) * 128, tg * TT:(tg + 1) * TT], in_=y_),
                           "yb%d" % os_, reads=[("yb", os_)], writes=[("catT", 8 + h, tg)])

        def odd_stage_c1(layer):
            A.reset()
            ikE = A.alloc(T, BF16)
            ikO = A.alloc(T, BF16)
            iq = [A.alloc(8 * 128, BF16).rearrange("p (c t) -> p c t", t=128) for _ in range(2)]
            iw = [A.alloc(16, F32) for _ in range(2)]
            acc = A.alloc(T, F32)
            work = A.alloc(T, F32)
            rl = [A.alloc(512, BF16) for _ in range(3)]
            m8 = A.alloc(8, F32)
            thrc = A.alloc(1, F32)
            nm = A.alloc(T, BF16)
            nmT = [A.alloc(NQB * 128, BF16).rearrange("p (j t) -> p j t", t=128) for _ in range(2)]
            identb = A.alloc(128, BF16)
            TR.op("dve", lambda e: e.tensor_copy(out=identb, in_=ident), reads=["ident"], writes=["identb"])
            TR.op("dve", lambda e: e.memset(thrc, -1e29), writes=["thrc"])
            TR.op("dve", lambda e: e.memset(ikE, 0.0), writes=["ik"])
            TR.op("dve", lambda e: e.memset(ikO, 0.0), writes=["ik"])
            TR.dma("sp", lambda e: e.dma_start(out=ikE[0:64, :], in_=ikT_d[0:64, :]), "oc5", writes=["ik"])
            TR.dma("sp", lambda e: e.dma_start(out=ikO[64:128, :], in_=ikT_d[64:128, :]), "oc0", writes=["ik"])
            nrl = 0
            for I in range(NQB):
                S = 128 * (I + 1)
                s_ = I % 2
                TR.dma("sp", lambda e, I=I, s_=s_: e.dma_start(out=iq[s_], in_=iqT_d.rearrange("(c p) t -> p c t", p=128)[:, :, I * 128:(I + 1) * 128]),
                       "iq%d" % s_, writes=[("iq", s_)])
                TR.dma("sp", lambda e, I=I, s_=s_: e.dma_start(out=iw[s_], in_=iw_d[I * 128:(I + 1) * 128, :]), "iw%d" % s_, writes=[("iw", s_)])
                for c0 in range(0, S, 512):
                    n = min(512, S - c0)
                    ck = ("acc", c0 // 512)
                    for hh in range(16):
                        pb = (hh % 2) * 64
                        b = psbank()
                        TR.op("pe", lambda e, b=b, hh=hh, pb=pb, c0=c0, n=n, s_=s_: e.matmul(
                            ps[:, b, 0:n], lhsT=iq[s_][:, hh // 2, :], rhs=(ikE if hh % 2 == 0 else ikO)[:, c0:c0 + n], start=True, stop=True),
                            reads=[("iq", s_), "ik"], writes=[("ps", b)])
                        r_ = nrl % 3
                        nrl += 1
                        rb = rl[r_]
                        TR.op("act", lambda e, rb=rb, b=b, n=n: e.activation(out=rb[:, 0:n], in_=ps[:, b, 0:n], func=AF.Relu),
                              reads=[("ps", b)], writes=[("rl", r_)])
                        if hh == 0:
                            TR.op("dve", lambda e, rb=rb, c0=c0, n=n, s_=s_: e.tensor_scalar(out=acc[:, c0:c0 + n], in0=rb[:, 0:n], scalar1=iw[s_][:, 0:1], scalar2=None,
                                                                                             op0=ALU.mult), reads=[("rl", r_), ("iw", s_)], writes=[ck])
                        else:
                            TR.op("dve", lambda e, rb=rb, c0=c0, n=n, s_=s_, hh=hh: e.scalar_tensor_tensor(
                                out=acc[:, c0:c0 + n], in0=rb[:, 0:n], scalar=iw[s_][:, hh:hh + 1], in1=acc[:, c0:c0 + n], op0=ALU.mult, op1=ALU.add),
                                reads=[("rl", r_), ("iw", s_), ck], writes=[ck])
                acck = [("acc", c) for c in range((S + 511) // 512)]
                TR.op("dve", lambda e, S=S: e.memset(acc[0:64, S - 64:S], -1e30), reads=acck, writes=acck)
                if I >= 2:
                    for r in range(32):
                        src = acc if r == 0 else work
                        TR.op("dve", lambda e, src=src, S=S: e.max(out=m8, in_=src[:, 0:S]), reads=acck + ["work"], writes=["m8"])
                        if r < 31:
                            TR.op("dve", lambda e, src=src, S=S: e.match_replace(out=work[:, 0:S], in_to_replace=m8, in_values=src[:, 0:S], imm_value=-1e30),
                                  reads=acck + ["m8", "work"], writes=["work"])
                    thr = m8[:, 7:8]
                    thrk = "m8"
                else:
                    thr = thrc[:, 0:1]
                    thrk = "thrc"
                TR.op("dve", lambda e, S=S, thr=thr: e.tensor_scalar(out=nm[:, 0:S], in0=acc[:, 0:S], scalar1=thr, scalar2=-30000.0, op0=ALU.is_lt, op1=ALU.mult),
                      reads=acck + [thrk], writes=["nm"])
                ns = I % 2
                for J0 in range(0, I + 1, 8):
                    js = list(range(J0, min(J0 + 8, I + 1)))
                    b = psbank()
                    pbf = ps[:, b, :].bitcast(BF16)
                    for jj, J in enumerate(js):
                        TR.op("pe", lambda e, pbf=pbf, jj=jj, J=J: e.transpose(out=pbf[:, jj * 128:(jj + 1) * 128], in_=nm[:, J * 128:(J + 1) * 128], identity=identb),
                              reads=["nm", "identb"], writes=[("ps", b)])
                    nj = len(js)
                    TR.op("act", lambda e, pbf=pbf, J0=J0, nj=nj, ns=ns: e.activation(out=nmT[ns][:, J0:J0 + nj, :].rearrange("p j t -> p (j t)"), in_=pbf[:, 0:nj * 128], func=AF.Copy),
                          reads=[("ps", b)], writes=[("nmT", ns)])
                TR.dma("sp", lambda e, I=I, ns=ns: e.dma_start(out=nmT_d[I, :, 0:(I + 1) * 128], in_=nmT[ns][:, 0:I + 1, :].rearrange("p j t -> p (j t)")),
                       "nmT%d" % ns, reads=[("nmT", ns)], writes=[("nmT_d", I)])

        def odd_stage_c2(layer):
            A.reset()
            qh = [A.alloc(T, BF16) for _ in range(2)]
            kh = [A.alloc(T, BF16) for _ in range(2)]
            vh = [A.alloc(NQB * 130, BF16).rearrange("p (j e) -> p j e", e=130) for _ in range(2)]
            nmb = [A.alloc(NQB * 128, BF16).rearrange("p (j t) -> p j t", t=128) for _ in range(2)]
            bg = A.alloc(8 * 256, F32).rearrange("p (h k t) -> p h k t", h=8, k=2)
            t15 = A.alloc(8, F32)
            tab = A.alloc(256, F32)
            mb = A.alloc(8, F32)
            rc = A.alloc(8, F32)
            identb = A.alloc(128, BF16)
            mx = A.alloc(8, F32)
            mcol = A.alloc(1, F32)
            rcol = A.alloc(1, F32)
            rdiag = [A.alloc(128, F32) for _ in range(2)]
            pt = [A.alloc(512, BF16) for _ in range(3)]
            rcp = A.alloc(1, F32)
            yo = A.alloc(128, BF16)
            yT = [A.alloc(TT, BF16) for _ in range(2)]
            TR.op("dve", lambda e: e.tensor_copy(out=identb, in_=ident), reads=["ident"], writes=["identb"])
            for s_ in range(2):
                TR.op("dve", lambda e, s_=s_: e.memset(vh[s_].rearrange("p j e -> p (j e)"), 1.0), writes=[("vh", s_, q4) for q4 in range(4)])
            TR.dma("sp", lambda e: e.dma_start(out=bg.rearrange("p h k t -> p (h k t)"), in_=biasg_d), "oc1", writes=["bg"])
            TR.dma("sp", lambda e: e.dma_start(out=t15, in_=t15_d), "oc2", writes=["t15"])
            TR.dma("sp", lambda e: e.dma_start(out=tab, in_=tab_d), "oc3", writes=["tab"])
            for h in range(8):
                TR.op("dve", lambda e, h=h: e.tensor_scalar(out=bg[:, h].rearrange("p k t -> p (k t)"), in0=bg[:, h].rearrange("p k t -> p (k t)"),
                                                            scalar1=t15[:, h:h + 1], scalar2=1.0 / ASCALE, op0=ALU.subtract, op1=ALU.mult),
                      reads=["bg", "t15"], writes=["bg"])
            TR.op("dve", lambda e: e.tensor_reduce(out=mb, in_=tab.rearrange("p (b h) -> p h b", h=8), axis=AX.X, op=ALU.max),
                  reads=["tab"], writes=["mb"])
            TR.op("dve", lambda e: e.tensor_tensor(out=rc, in0=tab[:, 120:128], in1=mb, op=ALU.subtract), reads=["tab", "mb"], writes=["rc"])
            TR.op("dve", lambda e: e.tensor_scalar(out=rc, in0=rc, scalar1=1.0 / ASCALE, scalar2=None, op0=ALU.mult), reads=["rc"], writes=["rc"])
            npt = 0
            nblk = 0
            C.rot = [0, 1, 2, 3, 4, 5]
            npo = [0]
            for h in range(8):
                s_ = h % 2
                TR.dma("sp", lambda e, h=h, s_=s_: e.dma_start(out=qh[s_], in_=qT_d[h * 128:(h + 1) * 128, :]), "cq%d" % s_, writes=[("qh", s_)])
                TR.dma("sp", lambda e, h=h, s_=s_: e.dma_start(out=kh[s_], in_=kT_d[h * 128:(h + 1) * 128, :]), "ck%d" % s_, writes=[("kh", s_)])
                for q4 in range(4):
                    TR.dma("sp", lambda e, h=h, s_=s_, q4=q4: e.dma_start(out=vh[s_][:, q4 * 8:(q4 + 1) * 8, 0:128],
                                                                        in_=vc_d.rearrange("(j p) c -> p j c", p=128)[:, q4 * 8:(q4 + 1) * 8, h * 128:(h + 1) * 128]),
                           "vv%d_%d" % (s_, q4), writes=[("vh", s_, q4)])
                for I in range(NQB):
                    S = 128 * (I + 1)
                    ms = nblk % 2
                    nblk += 1
                    TR.dma("sp", lambda e, I=I, ms=ms: e.dma_start(out=nmb[ms][:, 0:I + 1, :].rearrange("p j t -> p (j t)"), in_=nmT_d[I, :, 0:(I + 1) * 128]),
                           "nmb%d" % ms, reads=[("nmT_d", I)], writes=[("nmb", ms)])
                    nch = (S + 511) // 512
                    for c in range(nch):
                        n = min(512, S - c * 512)
                        b = psbank()
                        TR.op("pe", lambda e, b=b, c=c, n=n, I=I, s_=s_: e.matmul(ps[:, b, 0:n], lhsT=qh[s_][:, I * 128:(I + 1) * 128], rhs=kh[s_][:, c * 512:c * 512 + n],
                                                                                 start=True, stop=True), reads=[("qh", s_), ("kh", s_)], writes=[("ps", b)])
                        TR.op("dve", lambda e, b=b, c=c, n=n: e.tensor_reduce(out=mx[:, c:c + 1], in_=ps[:, b, 0:n], axis=AX.X, op=ALU.max),
                              reads=[("ps", b)], writes=["mx"])
                    TR.op("dve", lambda e, nch=nch: e.tensor_reduce(out=mcol, in_=mx[:, 0:nch], axis=AX.X, op=ALU.max), reads=["mx"], writes=["mcol"])
                    TR.op("dve", lambda e, h=h: e.tensor_scalar(out=rcol, in0=mcol, scalar1=-1.0, scalar2=rc[:, h:h + 1], op0=ALU.mult, op1=ALU.add),
                          reads=["mcol", "rc"], writes=["rcol"])
                    rd_ = nblk % 2
                    rdg = rdiag[rd_]
                    TR.op("dve", lambda e, rdg=rdg: e.tensor_scalar(out=rdg, in0=ident, scalar1=rcol[:, 0:1], scalar2=None, op0=ALU.mult),
                          reads=["ident", "rcol"], writes=[("rdiag", rd_)])
                    po = 6 + npo[0] % 2
                    npo[0] += 1
                    for J0 in range(0, I + 1, 4):
                        js = list(range(J0, min(J0 + 4, I + 1)))
                        b = psbank()
                        for jj, J in enumerate(js):
                            o_ = ps[:, b, jj * 128:(jj + 1) * 128]
                            near = J >= I - 1
                            TR.op("pe", lambda e, o_=o_, J=J, I=I, s_=s_: e.matmul(o_, lhsT=kh[s_][:, J * 128:(J + 1) * 128], rhs=qh[s_][:, I * 128:(I + 1) * 128], start=True, stop=False),
                                  reads=[("kh", s_), ("qh", s_)], writes=[("ps", b)])
                            TR.op("pe", lambda e, o_=o_, rdg=rdg: e.matmul(o_, lhsT=ones, rhs=rdg, start=False, stop=False),
                                  reads=["ones", ("rdiag", rd_)], writes=[("ps", b)])
                            TR.op("pe", lambda e, o_=o_, J=J, ms=ms, near=near: e.matmul(o_, lhsT=identb, rhs=nmb[ms][:, J, :], start=False, stop=(not near)),
                                  reads=["identb", ("nmb", ms)], writes=[("ps", b)])
                            if near:
                                kb = J - (I - 1)
                                TR.op("pe", lambda e, o_=o_, kb=kb, h=h: e.matmul(o_, lhsT=ident, rhs=bg[:, h, kb, :], start=False, stop=True),
                                      reads=["ident", "bg"], writes=[("ps", b)])
                        p_ = npt % 3
                        npt += 1
                        ptb = pt[p_]
                        nj = len(js)
                        TR.op("act", lambda e, ptb=ptb, b=b, nj=nj: e.activation(out=ptb[:, 0:nj * 128], in_=ps[:, b, 0:nj * 128], func=AF.Exp, scale=ASCALE),
                              reads=[("ps", b)], writes=[("pt", p_)])
                        for jj, J in enumerate(js):
                            TR.op("pe", lambda e, ptb=ptb, jj=jj, J=J, I=I, po=po, s_=s_: e.matmul(ps[:, po, 0:129], lhsT=ptb[:, jj * 128:(jj + 1) * 128], rhs=vh[s_][:, J, 0:129],
                                                                                                  start=(J == 0), stop=(J == I)),
                                  reads=[("pt", p_), ("vh", s_, J // 8)], writes=[("ps", po)])
                    TR.op("dve", lambda e, po=po: e.reciprocal(out=rcp, in_=ps[:, po, 128:129]), reads=[("ps", po)], writes=["rcp"])
                    TR.op("act", lambda e, po=po: e.activation(out=yo, in_=ps[:, po, 0:128], func=AF.Copy, scale=rcp[:, 0:1]), reads=[("ps", po), "rcp"], writes=["yo"])
                    bt = psbank()
                    pbf = ps[:, bt, :].bitcast(BF16)
                    TR.op("pe", lambda e, pbf=pbf: e.transpose(out=pbf[:, 0:128], in_=yo, identity=identb), reads=["yo", "identb"], writes=[("ps", bt)])
                    ys = (I // 4) % 2
                    TR.op("dve", lambda e, pbf=pbf, I=I, ys=ys: e.tensor_copy(out=yT[ys][:, (I % 4) * 128:(I % 4 + 1) * 128], in_=pbf[:, 0:128]),
                          reads=[("ps", bt)], writes=[("yT", ys)])
                    if I % 4 == 3:
                        tg = I // 4
                        TR.dma("sp", lambda e, ys=ys, h=h, tg=tg: e.dma_start(out=catT_d[h * 128:(h + 1) * 128, tg * TT:(tg + 1) * TT], in_=yT[ys]),
                               "yT%d" % ys, reads=[("yT", ys)], writes=[("catT", h, tg)])

        def odd_stage_d(layer, hsrc, hdst):
            j = layer // 2
            A.reset()
            cat = [A.alloc(NFC * TT, BF16).rearrange("p (f t) -> p f t", t=TT) for _ in range(2)]
            wo = [A.alloc(NFC * 256, BF16).rearrange("p (k n) -> p k n", n=256) for _ in range(2)]
            epi = [A.alloc(TT, F32) for _ in range(3)]
            epi_n = [0]
            nw = 0
            cv = catT_d.rearrange("(f p) t -> p f t", p=128)
            for ti in range(NT):
                cs = ti % 2
                for q in range(4):
                    TR.dma("sp", lambda e, q=q, ti=ti, cs=cs: e.dma_start(out=cat[cs][:, q * 4:(q + 1) * 4, :], in_=cv[:, q * 4:(q + 1) * 4, ti * TT:(ti + 1) * TT]),
                           "cat%d_%d" % (cs, q), writes=[("cat", cs, q)])
                for c in range(8):
                    s_ = nw % 2
                    nw += 1
                    TR.dma("sp", lambda e, c=c, s_=s_: e.dma_start(out=wo[s_], in_=cdo_b[j, c]), "wo%d" % s_, reads=[("cdo", j, c)], writes=[("wos", s_)])
                    for hf in range(2):
                        b = psbank()
                        for k in range(NFC):
                            TR.op("pe", lambda e, s_=s_, hf=hf, k=k, b=b, cs=cs: e.matmul(ps[:, b, :], lhsT=wo[s_][:, k, hf * 128:(hf + 1) * 128], rhs=cat[cs][:, k, :],
                                                                                          start=(k == 0), stop=(k == NFC - 1)),
                                  reads=[("wos", s_), ("cat", cs, k // 4)], writes=[("ps", b)])
                        residual_epilogue(hsrc, hdst, ti, c * 2 + hf, b, 1.0, epi, epi_n)

        def phase_mix_odd(layer, hsrc, hdst, next_cast=None, stages="abcd"):
            if next_cast is not None:
                next_cast()
            odd_stage_a(layer, hsrc)
            TR.barrier()
            if "b" not in stages:
                A.reset()
                zt = A.alloc(T, BF16)
                TR.op("dve", lambda e: e.memset(zt, 0.0), writes=["zt"])
                for hh_ in range(8):
                    TR.dma("sp", lambda e, hh_=hh_: e.dma_start(out=catT_d[1024 + hh_ * 128:1024 + (hh_ + 1) * 128, :], in_=zt), "zt", reads=["zt"], writes=[("catz", 8 + hh_)])
                TR.barrier()
            if "b" in stages:
                odd_stage_b(layer)
                C.rot = list(range(8))
                TR.barrier()
            if "c" not in stages:
                A.reset()
                zt = A.alloc(T, BF16)
                TR.op("dve", lambda e: e.memset(zt, 0.0), writes=["zt"])
                for hh_ in range(8):
                    TR.dma("sp", lambda e, hh_=hh_: e.dma_start(out=catT_d[hh_ * 128:(hh_ + 1) * 128, :], in_=zt), "zt", reads=["zt"], writes=[("catz", hh_)])
                TR.barrier()
            if "c" in stages:
                odd_stage_c1(layer)
                TR.barrier()
                odd_stage_c2(layer)
                C.rot = list(range(8))
                TR.barrier()
            odd_stage_d(layer, hsrc, hdst)

        def phase_final(hsrc):
            A.reset()
            hT = A.alloc(NFC * TT, F32).rearrange("p (f t) -> p f t", t=TT)
            y = A.alloc(NFC * TT, F32).rearrange("p (f t) -> p f t", t=TT)
            sq = [A.alloc(TT, F32) for _ in range(2)]
            rstd = A.alloc(TT, F32)
            ot = [A.alloc(D, F32) for _ in range(2)]
            n = 0
            for ti in range(NT):
                rms_norm_tile(hsrc, ti, 12, hT, y, sq, rstd)
                for tb in range(TT // 128):
                    o = ot[n % 2]
                    okey = ("ot", n % 2)
                    n += 1
                    for f4 in range(NFC // 4):
                        b = psbank()
                        for k in range(4):
                            f = f4 * 4 + k
                            TR.op("pe", lambda e, f=f, k=k, b=b, tb=tb: e.transpose(
                                out=ps[:, b, k * 128:(k + 1) * 128], in_=y[:, f, tb * 128:(tb + 1) * 128], identity=ident),
                                reads=[("xn", f), "ident"], writes=[("ps", b)])
                        if f4 % 2 == 0:
                            TR.op("act", lambda e, o=o, b=b, f4=f4: e.activation(out=o[:, f4 * 512:(f4 + 1) * 512], in_=ps[:, b, :], func=AF.Copy),
                                  reads=[("ps", b)], writes=[okey])
                        else:
                            TR.op("dve", lambda e, o=o, b=b, f4=f4: e.tensor_copy(out=o[:, f4 * 512:(f4 + 1) * 512], in_=ps[:, b, :]),
                                  reads=[("ps", b)], writes=[okey])
                    r0 = ti * TT + tb * 128
                    TR.dma("sp", lambda e, o=o, r0=r0: e.dma_start(out=out[r0:r0 + 128, :], in_=o), "ot%d" % ((n - 1) % 2),
                           reads=[okey], writes=[("out", r0)])

        plan = []
        plan.append(("prologue",))
        for layer in range(DEPTH):
            plan.append(("ffn", layer * 2, layer * 3 + 0))
            plan.append(("mix", layer))
            plan.append(("ffn", layer * 2 + 1, layer * 3 + 2))
        plan.append(("final",))
        if phases is not None:
            plan = [p for p in plan if p in phases or p[0] in ("prologue", "final")]

        def caster(p):
            if p[0] == "ffn":
                return lambda: cast_ffn(p[1])
            if p[0] == "mix" and p[1] % 2 == 0:
                return lambda: cast_even(p[1] // 2)
            if p[0] == "mix":
                return lambda: cast_odd(p[1] // 2)
            return None
        wplan = [p for p in plan if caster(p) is not None]
        cur = 0
        if wplan:
            caster(wplan[0])()
        for p in plan:
            nxt = None
            if p in wplan:
                i = wplan.index(p)
                if i + 1 < len(wplan):
                    nxt = caster(wplan[i + 1])
            if p[0] == "prologue":
                phase_prologue(hbuf[cur])
            elif p[0] == "ffn":
                phase_ffn(p[1], p[2], hbuf[cur], hbuf[1 - cur], nxt)
                cur = 1 - cur
            elif p[0] == "mix":
                if p[1] % 2 == 0:
                    phase_mix_even(p[1], hbuf[cur], hbuf[1 - cur], nxt)
                    cur = 1 - cur
                else:
                    phase_mix_odd(p[1], hbuf[cur], hbuf[1 - cur], nxt, stages=odd_stages)
                    cur = 1 - cur
            elif p[0] == "final":
                phase_final(hbuf[cur])
            TR.barrier()
        TR.emit(block)
        C.nops = TR.nops
    return nc


def _t5_bucket_np(rel):
    import jax
    import jax.numpy as jnp
    import math
    with jax.default_device(jax.devices("cpu")[0]):
        rel = jnp.asarray(rel, dtype=jnp.int32)
        nb = 16
        max_exact = 8
        ret = jnp.where(rel > 0, nb, 0)
        n = jnp.abs(rel)
        large = max_exact + (jnp.log(jnp.maximum(n, 1).astype(jnp.float32) / max_exact)
                             / math.log(128 / max_exact) * (nb - max_exact)).astype(jnp.int32)
        large = jnp.minimum(large, nb - 1)
        return np.asarray(ret + jnp.where(n < max_exact, n, large))


def host_constants(rel_bias_table=None):
    t = np.arange(TT)
    invc = np.stack([1.0 / np.minimum(t + 1, w) for w in (2, 4, 8, 16)]).astype(np.float32)
    invc0 = np.ascontiguousarray(np.broadcast_to(invc.reshape(1, 4 * TT), (128, 4 * TT)))
    out = {"ident": np.eye(128, dtype=np.float32), "invc0": invc0}
    pos = np.arange(T, dtype=np.float64)
    gam = np.array([1.0 - 2.0 ** (-5.0 - h) for h in range(8)], dtype=np.float64)
    p = np.arange(128)
    i = p % 64
    f = i % 32
    sign = np.where(i < 32, -1.0, 1.0)
    freqs = 10000.0 ** (-f.astype(np.float64) / 32.0)
    ang = (pos[None, :].astype(np.float32) * freqs[:, None].astype(np.float32)).astype(np.float64)
    cos, sin = np.cos(ang), np.sin(ang) * sign[:, None]
    tl = (np.arange(T) % 128).astype(np.float64)
    rope = np.zeros((2, 4, 2, 128, T), dtype=np.float32)
    for c in range(4):
        hh = 2 * c + p // 64
        dq = gam[hh][:, None] ** tl[None, :]
        dk = (64.0 ** -0.5) * gam[hh][:, None] ** (-tl[None, :])
        rope[0, c, 0], rope[0, c, 1] = cos * dq, sin * dq
        rope[1, c, 0], rope[1, c, 1] = cos * dk, sin * dk
    out["rope_tab"] = rope
    jl = np.arange(128)[:, None]
    il = np.arange(128)[None, :]
    vis = (jl < ((il // 64) + 1) * 64)
    dd = np.zeros((128, 8, 128), dtype=np.float32)
    for h in range(8):
        m = np.where(il >= jl, 1.0, gam[h] ** (2.0 * (jl - il)))
        dd[:, h, :] = m * vis
    out["ret_diag"] = dd.reshape(128, 8 * 128)
    if rel_bias_table is not None:
        tab = np.asarray(rel_bias_table, dtype=np.float32)
        sl = np.arange(128)[:, None, None]
        blk = np.arange(2)[None, :, None]
        tl_ = np.arange(128)[None, None, :]
        rel = blk * 128 + sl - 128 - tl_
        bidx = _t5_bucket_np(rel)
        g = tab[bidx]
        out["bias_g"] = np.ascontiguousarray(np.transpose(g, (0, 3, 1, 2))).reshape(128, 8 * 2 * 128)
        out["bias_t15"] = np.ascontiguousarray(np.broadcast_to(tab[15:16, :], (128, 8)))
        out["bias_tab"] = np.ascontiguousarray(np.broadcast_to(tab.reshape(1, 256), (128, 256)))
    return out


def make_in_maps(inputs, n_cores=N_CORES):
    consts = host_constants(inputs.get("rel_bias_table"))
    shared = {
        "norm_gains": np.ascontiguousarray(np.asarray(inputs["norm_gains"], dtype=np.float32).reshape(DEPTH * 3, D)),
        "ffn_w_gate_up": np.ascontiguousarray(np.asarray(inputs["ffn_w_gate_up"], dtype=np.float32).reshape(DEPTH * 2, D, 2 * DFF)),
        "ffn_w_down": np.ascontiguousarray(np.asarray(inputs["ffn_w_down"], dtype=np.float32).reshape(DEPTH * 2, DFF, D)),
        "final_norm": np.ascontiguousarray(np.asarray(inputs["final_norm"], dtype=np.float32).reshape(1, D)),
    }
    for k in ("ab_w_in", "ab_conv_w", "ab_pool_w", "ab_pool_scale", "ab_w_out", "cd_w_in", "ret_norm_gain", "cd_w_out"):
        shared[k] = np.ascontiguousarray(np.asarray(inputs[k], dtype=np.float32))
    shared.update(consts)
    xs = np.asarray(inputs["x"], dtype=np.float32)
    maps = []
    for c in range(n_cores):
        m = dict(shared)
        m["x"] = np.ascontiguousarray(xs[c])
        maps.append(m)
    return maps


def kernel(**inputs):
    nc = build_program()
    in_maps = make_in_maps(inputs)
    res = run_bass_kernel_spmd(nc, in_maps, core_ids=list(range(N_CORES)))
    return np.stack([np.asarray(r["out"], dtype=np.float32) for r in res.results], axis=0)
```

```python
import numpy as np
import concourse.bass as bass
import concourse.mybir as mybir
from concourse.bass_utils import run_bass_kernel_spmd

F32 = mybir.dt.float32
BF16 = mybir.dt.bfloat16
AF = mybir.ActivationFunctionType
ALU = mybir.AluOpType
AX = mybir.AxisListType

D = 2048
T = 4096
DFF = 5632
DEPTH = 4
NFC = D // 128
TT = 512
NT = T // TT
RMS_EPS = 1e-6
N_CORES = 8

CENGS = ("pe", "act", "dve", "pool")
ENGS = CENGS + ("sp",)


class Op:
    __slots__ = ("eng", "fn", "deps", "xdeps", "is_dma", "dsem", "dval", "val", "waited")

    def __init__(self, eng, fn, is_dma=False):
        self.eng = eng
        self.fn = fn
        self.deps = []
        self.xdeps = []
        self.is_dma = is_dma
        self.dsem = None
        self.dval = 0
        self.val = None
        self.waited = False


SEM_ALIAS = {}
for _a in range(2):
    for _b in range(4):
        SEM_ALIAS["cat%d_%d" % (_a, _b)] = "wd%d_%d" % (_a, _b)
for _b in range(4):
    SEM_ALIAS["vv0_%d" % _b] = "hT%d" % _b
    SEM_ALIAS["vv1_%d" % _b] = "wd0_%d" % _b
for _a in range(2):
    SEM_ALIAS["wF%d" % _a] = "wg%d" % _a
    SEM_ALIAS["wT%d" % _a] = "wu%d" % _a
    SEM_ALIAS["wR%d_0" % _a] = "wc%d" % _a
    SEM_ALIAS["wR%d_1" % _a] = "wp%d" % _a
    SEM_ALIAS["cq%d" % _a] = "rq%d" % _a
    SEM_ALIAS["ck%d" % _a] = "rk%d" % _a
    SEM_ALIAS["xt%d" % _a] = "ot%d" % _a
for _a in range(3):
    SEM_ALIAS["st%d" % _a] = "stg%d" % _a


class Tracker:
    def __init__(self, nc):
        self.nc = nc
        self.esem = {e: nc.alloc_semaphore(name="es_" + e) for e in CENGS}
        self.ecount = {e: 0 for e in CENGS}
        self.dma_sems = {}
        self.dma_cnt = {}
        self.last_w = {}
        self.readers = {}
        self.byeng = {e: [] for e in ENGS}
        self.nops = 0

    def _add(self, op, reads, writes):
        deps = []
        for r in reads:
            w = self.last_w.get(r)
            if w is not None:
                deps.append(w)
        for w_ in writes:
            w = self.last_w.get(w_)
            if w is not None:
                deps.append(w)
            deps.extend(self.readers.get(w_, ()))
        for r in reads:
            self.readers.setdefault(r, []).append(op)
        for w_ in writes:
            self.last_w[w_] = op
            self.readers[w_] = []
        seen = set()
        for d in deps:
            if d is op or id(d) in seen:
                continue
            if op.eng == "pe" and d.eng == "pe" and not d.is_dma and not op.is_dma:
                continue
            seen.add(id(d))
            op.deps.append(d)
            d.waited = True
        self.byeng[op.eng].append(op)
        self.nops += 1
        return op

    def op(self, eng, fn, reads=(), writes=()):
        return self._add(Op(eng, fn), reads, writes)

    def dma(self, eng, fn, sem, reads=(), writes=()):
        sem = SEM_ALIAS.get(sem, sem)
        op = Op(eng, fn, is_dma=True)
        if sem not in self.dma_sems:
            self.dma_sems[sem] = self.nc.alloc_semaphore(name="ds_" + sem)
            self.dma_cnt[sem] = 0
        self.dma_cnt[sem] += 16
        op.dsem = sem
        op.dval = self.dma_cnt[sem]
        return self._add(op, reads, writes)

    def barrier(self):
        bs = []
        for e in CENGS:
            b = Op(e, None)
            b.waited = True
            for f in CENGS:
                if f != e:
                    for o in reversed(self.byeng[f]):
                        if not o.is_dma:
                            b.deps.append(o)
                            o.waited = True
                            break
            for s, c in self.dma_cnt.items():
                if c:
                    b.xdeps.append((s, c))
            bs.append(b)
        for b in bs:
            self.byeng[b.eng].append(b)
        for e in ENGS:
            c = Op(e, None)
            c.deps = list(bs)
            self.byeng[e].append(c)
        self.last_w = {}
        self.readers = {}

    def emit(self, block):
        for e in CENGS:
            for op in self.byeng[e]:
                if not op.is_dma and op.waited:
                    self.ecount[e] += 1
                    op.val = self.ecount[e]
        seen = {}

        def run(en, eng):
            def w(key, sem, v):
                if seen.get((en, key), 0) >= v:
                    return
                seen[(en, key)] = v
                eng.wait_ge(sem, v)

            for op in self.byeng[en]:
                for d in op.deps:
                    if d.is_dma:
                        w(("d", d.dsem), self.dma_sems[d.dsem], d.dval)
                    elif d.eng != "sp":
                        w(("e", d.eng), self.esem[d.eng], d.val)
                for (s, v) in op.xdeps:
                    w(("d", s), self.dma_sems[s], v)
                if op.fn is None:
                    if op.val is not None:
                        eng.nop().then_inc(self.esem[op.eng], 1)
                    continue
                ins = op.fn(eng)
                if op.is_dma:
                    ins.then_inc(self.dma_sems[op.dsem], 16)
                elif op.val is not None:
                    ins.then_inc(self.esem[op.eng], 1)

        @block.tensor
        def _(e):
            run("pe", e)

        @block.scalar
        def _(e):
            run("act", e)

        @block.vector
        def _(e):
            run("dve", e)

        @block.gpsimd
        def _(e):
            run("pool", e)

        @block.sync
        def _(e):
            run("sp", e)


class Arena:
    def __init__(self, big, n32):
        self.big = big
        self.n32 = n32
        self.off = 0
        self.mark = 0

    def set_mark(self):
        self.mark = self.off

    def reset(self):
        self.off = self.mark

    def alloc(self, n_elems, dtype):
        sz = 2 if dtype == BF16 else 4
        n32 = (n_elems * sz + 3) // 4
        n32 = (n32 + 7) // 8 * 8
        assert self.off + n32 <= self.n32, f"SBUF arena overflow {self.off}+{n32}>{self.n32}"
        ap = self.big[:, self.off:self.off + n32]
        self.off += n32
        if dtype != F32:
            ap = ap.bitcast(dtype)
        return ap[:, :n_elems]


class Ctx:
    pass


def build_program(phases=None, debug_out=False, odd_stages="abcd"):
    nc = bass.Bass("TRN2", target_bir_lowering=False)
    C = Ctx()
    C.nc = nc
    dt = nc.dram_tensor
    x = dt("x", [T, D], F32, kind="ExternalInput").ap()
    norm_gains = dt("norm_gains", [DEPTH * 3, D], F32, kind="ExternalInput").ap()
    w_gu = dt("ffn_w_gate_up", [DEPTH * 2, D, 2 * DFF], F32, kind="ExternalInput").ap()
    w_dn = dt("ffn_w_down", [DEPTH * 2, DFF, D], F32, kind="ExternalInput").ap()
    final_norm = dt("final_norm", [1, D], F32, kind="ExternalInput").ap()
    ident_d = dt("ident", [128, 128], F32, kind="ExternalInput").ap()
    ab_w_in = dt("ab_w_in", [2, D, 4096], F32, kind="ExternalInput").ap()
    ab_conv_w = dt("ab_conv_w", [2, 3, 1024], F32, kind="ExternalInput").ap()
    ab_pool_w = dt("ab_pool_w", [2, 4, 256, 256], F32, kind="ExternalInput").ap()
    ab_pool_scale = dt("ab_pool_scale", [2, 1024], F32, kind="ExternalInput").ap()
    ab_w_out = dt("ab_w_out", [2, D, D], F32, kind="ExternalInput").ap()
    invc0_d = dt("invc0", [128, 4 * TT], F32, kind="ExternalInput").ap()
    cd_w_in = dt("cd_w_in", [2, D, 7248], F32, kind="ExternalInput").ap()
    ret_norm_gain = dt("ret_norm_gain", [2, 1024], F32, kind="ExternalInput").ap()
    cd_w_out = dt("cd_w_out", [2, D, D], F32, kind="ExternalInput").ap()
    rope_d = dt("rope_tab", [2, 4, 2, 128, T], F32, kind="ExternalInput").ap()
    dd_d = dt("ret_diag", [128, 8 * 128], F32, kind="ExternalInput").ap()
    biasg_d = dt("bias_g", [128, 8 * 2 * 128], F32, kind="ExternalInput").ap()
    t15_d = dt("bias_t15", [128, 8], F32, kind="ExternalInput").ap()
    tab_d = dt("bias_tab", [128, 256], F32, kind="ExternalInput").ap()
    out = dt("out", [T, D], F32, kind="ExternalOutput").ap()
    hbuf = [dt("hA", [D, T], F32, kind="Internal").ap(), dt("hB", [D, T], F32, kind="Internal").ap()]
    NGC = DFF // 256
    NDC = D // 256
    NKF = DFF // 128
    wg_b = dt("wg_b", [DEPTH * 2, NGC, 128, NFC, 256], BF16, kind="Internal").ap()
    wu_b = dt("wu_b", [DEPTH * 2, NGC, 128, NFC, 256], BF16, kind="Internal").ap()
    wd_b = dt("wd_b", [DEPTH * 2, NDC, 128, NKF, 256], BF16, kind="Internal").ap()
    abc_b = dt("abc_b", [2, 8, 128, NFC, 384], BF16, kind="Internal").ap()
    abp_b = dt("abp_b", [2, 4, 128, NFC, 256], BF16, kind="Internal").ap()
    abo_b = dt("abo_b", [2, 8, 128, NFC, 256], BF16, kind="Internal").ap()
    cdF_b = dt("cdF_b", [2, 16, 128, NFC, 256], BF16, kind="Internal").ap()
    cdI_b = dt("cdI_b", [2, 128, NFC, 128], BF16, kind="Internal").ap()
    cdR_b = dt("cdR_b", [2, 16, 128, NFC, 128], BF16, kind="Internal").ap()
    cdT_b = dt("cdT_b", [2, 4, 128, NFC, 512], BF16, kind="Internal").ap()
    cdW_b = dt("cdW_b", [2, 128, NFC, 16], BF16, kind="Internal").ap()
    cdo_b = dt("cdo_b", [2, 8, 128, NFC, 256], BF16, kind="Internal").ap()
    qT_d = dt("qT_d", [1024, T], BF16, kind="Internal").ap()
    kT_d = dt("kT_d", [1024, T], BF16, kind="Internal").ap()
    iqT_d = dt("iqT_d", [1024, T], BF16, kind="Internal").ap()
    ikT_d = dt("ikT_d", [128, T], BF16, kind="Internal").ap()
    iw_d = dt("iw_d", [T, 16], F32, kind="Internal").ap()
    rqT_d = dt("rqT_d", [512, T], BF16, kind="Internal").ap()
    rkT_d = dt("rkT_d", [512, T], BF16, kind="Internal").ap()
    vc_d = dt("vc_d", [T, 1024], BF16, kind="Internal").ap()
    vr_d = dt("vr_d", [T, 1024], BF16, kind="Internal").ap()
    sg_d = dt("sg_d", [1024, T], F32, kind="Internal").ap()
    catT_d = dt("catT_d", [D, T], BF16, kind="Internal").ap()
    nmT_d = dt("nmT_d", [32, 128, 32 * 128], BF16, kind="Internal").ap()

    N32 = 51 * 1024 + 512
    es = nc.sbuf_tensor("big", [128, N32], F32)
    ps_cm = nc.psum_tensor("ps", [128, 8, 512], F32)
    with es as big, ps_cm as ps, nc.Block() as block:
        TR = Tracker(nc)
        A = Arena(big, N32)
        C.TR, C.A, C.ps = TR, A, ps
        C.psn = 0

        C.rot = list(range(8))

        def psbank():
            b = C.rot[C.psn % len(C.rot)]
            C.psn += 1
            return b
        C.psbank = psbank

        ident = A.alloc(128, F32)
        ones = A.alloc(128, F32)
        gains = A.alloc(13 * NFC, F32).rearrange("p (s f) -> p s f", f=NFC)
        TR.dma("sp", lambda e: e.dma_start(out=ident, in_=ident_d), "cid", writes=["ident"])
        TR.op("dve", lambda e: e.memset(ones, 1.0), writes=["ones"])
        for s in range(13):
            src = norm_gains[s] if s < 12 else final_norm[0]
            TR.dma("sp", lambda e, s=s, src=src: e.dma_start(
                out=gains[:, s, :], in_=src.rearrange("(f p) -> p f", p=128), allow_slow_non_contiguous=True),
                "const", writes=["gains"])
        C.ident, C.ones, C.gains = ident, ones, gains
        A.set_mark()

        def cast_ffn(fi):
            wv = w_gu[fi].rearrange("(kc p) n -> p kc n", p=128)
            for c in range(NGC):
                TR.dma("pool", lambda e, c=c: e.dma_start(out=wg_b[fi, c], in_=wv[:, :, c * 256:(c + 1) * 256]),
                       "cast", writes=[("wg", fi, c)])
                TR.dma("pool", lambda e, c=c: e.dma_start(out=wu_b[fi, c], in_=wv[:, :, DFF + c * 256:DFF + (c + 1) * 256]),
                       "cast", writes=[("wu", fi, c)])
            dv = w_dn[fi].rearrange("(kc p) n -> p kc n", p=128)
            for c in range(NDC):
                for k0 in range(0, NKF, 11):
                    TR.dma("pool", lambda e, c=c, k0=k0: e.dma_start(out=wd_b[fi, c, :, k0:k0 + 11, :],
                                                                      in_=dv[:, k0:k0 + 11, c * 256:(c + 1) * 256]),
                           "cast", writes=[("wd", fi, c, k0)])

        def rms_norm_tile(hsrc, ti, gslot, hT, xn, sq, rstd, xkey="xn"):
            t0 = ti * TT
            hv = hsrc.rearrange("(f p) t -> p f t", p=128)
            for q in range(4):
                TR.dma("sp", lambda e, q=q: e.dma_start(out=hT[:, q * 4:(q + 1) * 4, :], in_=hv[:, q * 4:(q + 1) * 4, t0:t0 + TT]),
                       "hT%d" % q, writes=[("hT", q)])
            b = psbank()
            for f in range(NFC):
                s = sq[f % 2]
                TR.op("act", lambda e, f=f, s=s: e.activation(out=s, in_=hT[:, f, :], func=AF.Square),
                      reads=[("hT", f // 4)], writes=[("sq", f % 2)])
                TR.op("pe", lambda e, f=f, s=s: e.matmul(ps[:, b, :], lhsT=ones, rhs=s, start=(f == 0), stop=(f == NFC - 1)),
                      reads=[("sq", f % 2), "ones"], writes=[("ps", b)])
            TR.op("dve", lambda e: e.tensor_scalar(out=rstd, in0=ps[:, b, :], scalar1=1.0 / D, scalar2=RMS_EPS,
                                                   op0=ALU.mult, op1=ALU.add), reads=[("ps", b)], writes=["rstd"])
            TR.op("act", lambda e: e.activation(out=rstd, in_=rstd, func=AF.Sqrt), reads=["rstd"], writes=["rstd"])
            TR.op("dve", lambda e: e.reciprocal(out=rstd, in_=rstd), reads=["rstd"], writes=["rstd"])
            for f in range(NFC):
                TR.op("dve", lambda e, f=f: e.scalar_tensor_tensor(out=xn[:, f, :], in0=hT[:, f, :], scalar=gains[:, gslot, f:f + 1],
                                                                 in1=rstd, op0=ALU.mult, op1=ALU.mult),
                      reads=[("hT", f // 4), "rstd", "gains"], writes=[(xkey, f)])

        def residual_epilogue(hsrc, hdst, ti, fo, b, scale, epi, epi_n):
            t0 = ti * TT
            slot = epi_n[0] % len(epi)
            epi_n[0] += 1
            et = epi[slot]
            TR.dma("act", lambda e: e.dma_start(out=et, in_=hsrc[fo * 128:(fo + 1) * 128, t0:t0 + TT]),
                   "epi%d" % slot, writes=[("epi", slot)])
            TR.op("dve", lambda e: e.scalar_tensor_tensor(out=et, in0=ps[:, b, :], scalar=float(scale), in1=et,
                                                          op0=ALU.mult, op1=ALU.add),
                  reads=[("ps", b)], writes=[("epi", slot)])
            TR.dma("act", lambda e: e.dma_start(out=hdst[fo * 128:(fo + 1) * 128, t0:t0 + TT], in_=et),
                   "epi%d" % slot, reads=[("epi", slot)], writes=[("hdst", fo, ti)])

        def phase_prologue(hdst):
            A.reset()
            xt = [A.alloc(D, F32) for _ in range(2)]
            st = [A.alloc(TT, F32) for _ in range(3)]
            n = 0
            for tb in range(T // 128):
                xs = xt[tb % 2]
                TR.dma("sp", lambda e, xs=xs, tb=tb: e.dma_start(out=xs, in_=x[tb * 128:(tb + 1) * 128, :]),
                       "xt%d" % (tb % 2), writes=[("xt", tb % 2)])
                for f4 in range(NFC // 4):
                    b = psbank()
                    for k in range(4):
                        f = f4 * 4 + k
                        TR.op("pe", lambda e, xs=xs, f=f, k=k, b=b: e.transpose(out=ps[:, b, k * 128:(k + 1) * 128],
                                                                              in_=xs[:, f * 128:(f + 1) * 128], identity=ident),
                              reads=[("xt", tb % 2), "ident"], writes=[("ps", b)])
                    s = n % 3
                    n += 1
                    sb = st[s]
                    eng = "act" if n % 2 == 0 else "dve"
                    if eng == "act":
                        TR.op("act", lambda e, sb=sb, b=b: e.activation(out=sb, in_=ps[:, b, :], func=AF.Copy),
                              reads=[("ps", b)], writes=[("st", s)])
                    else:
                        TR.op("dve", lambda e, sb=sb, b=b: e.tensor_copy(out=sb, in_=ps[:, b, :]),
                              reads=[("ps", b)], writes=[("st", s)])
                    dst = hdst.rearrange("(f p) t -> p f t", p=128)[:, f4 * 4:(f4 + 1) * 4, tb * 128:(tb + 1) * 128]
                    TR.dma("sp", lambda e, sb=sb, dst=dst: e.dma_start(out=dst, in_=sb.rearrange("p (k t) -> p k t", t=128)),
                           "st%d" % s, reads=[("st", s)], writes=[("hdst", f4, tb)])

        def phase_ffn(fi, gslot, hsrc, hdst, next_cast=None):
            A.reset()
            hT = A.alloc(NFC * TT, F32).rearrange("p (f t) -> p f t", t=TT)
            xns = [A.alloc(NFC * TT, BF16).rearrange("p (f t) -> p f t", t=TT) for _ in range(2)]
            act = A.alloc(NKF * TT, BF16).rearrange("p (f t) -> p f t", t=TT)
            wg = [A.alloc(NFC * 256, BF16).rearrange("p (k n) -> p k n", n=256) for _ in range(2)]
            wu = [A.alloc(NFC * 256, BF16).rearrange("p (k n) -> p k n", n=256) for _ in range(2)]
            wd = [A.alloc(NKF * 256, BF16).rearrange("p (k n) -> p k n", n=256) for _ in range(2)]
            sq = [A.alloc(TT, F32) for _ in range(2)]
            rstd = A.alloc(TT, F32)
            sg = [A.alloc(TT, F32) for _ in range(2)]
            epi = [A.alloc(TT, F32) for _ in range(4)]
            epi_n = [0]
            wn = [0, 0]
            if next_cast is not None:
                next_cast()
            rms_norm_tile(hsrc, 0, gslot, hT, xns[0], sq, rstd, xkey="xn0")
            for ti in range(NT):
                xn = xns[ti % 2]
                xk = "xn%d" % (ti % 2)
                for c in range(NGC):
                    s = wn[0] % 2
                    wn[0] += 1
                    TR.dma("sp", lambda e, c=c, s=s: e.dma_start(out=wg[s], in_=wg_b[fi, c]), "wg%d" % s,
                           reads=[("wg", fi, c)], writes=[("wgs", s)])
                    TR.dma("sp", lambda e, c=c, s=s: e.dma_start(out=wu[s], in_=wu_b[fi, c]), "wu%d" % s,
                           reads=[("wu", fi, c)], writes=[("wus", s)])
                    for hf in range(2):
                        bg = psbank()
                        bu = psbank()
                        for k in range(NFC):
                            TR.op("pe", lambda e, s=s, hf=hf, k=k, bg=bg, xn=xn: e.matmul(
                                ps[:, bg, :], lhsT=wg[s][:, k, hf * 128:(hf + 1) * 128], rhs=xn[:, k, :],
                                start=(k == 0), stop=(k == NFC - 1)),
                                reads=[("wgs", s), (xk, k)], writes=[("ps", bg)])
                        for k in range(NFC):
                            TR.op("pe", lambda e, s=s, hf=hf, k=k, bu=bu, xn=xn: e.matmul(
                                ps[:, bu, :], lhsT=wu[s][:, k, hf * 128:(hf + 1) * 128], rhs=xn[:, k, :],
                                start=(k == 0), stop=(k == NFC - 1)),
                                reads=[("wus", s), (xk, k)], writes=[("ps", bu)])
                        j = c * 2 + hf
                        sgt = sg[j % 2]
                        TR.op("act", lambda e, sgt=sgt, bg=bg: e.activation(out=sgt, in_=ps[:, bg, :], func=AF.Silu),
                              reads=[("ps", bg)], writes=[("sg", j % 2)])
                        TR.op("dve", lambda e, sgt=sgt, bu=bu, j=j: e.tensor_tensor(out=act[:, j, :], in0=ps[:, bu, :], in1=sgt,
                                                                                     op=ALU.mult),
                              reads=[("ps", bu), ("sg", j % 2)], writes=[("act", j)])
                for c in range(NDC):
                    s = wn[1] % 2
                    wn[1] += 1
                    for k0 in range(0, NKF, 11):
                        TR.dma("sp", lambda e, c=c, s=s, k0=k0: e.dma_start(out=wd[s][:, k0:k0 + 11, :], in_=wd_b[fi, c, :, k0:k0 + 11, :]),
                               "wd%d_%d" % (s, k0 // 11), reads=[("wd", fi, c, k0)], writes=[("wds", s, k0)])
                    if c == 2 and ti + 1 < NT:
                        rms_norm_tile(hsrc, ti + 1, gslot, hT, xns[(ti + 1) % 2], sq, rstd, xkey="xn%d" % ((ti + 1) % 2))
                    for hf in range(2):
                        b = psbank()
                        for k in range(NKF):
                            TR.op("pe", lambda e, s=s, hf=hf, k=k, b=b: e.matmul(
                                ps[:, b, :], lhsT=wd[s][:, k, hf * 128:(hf + 1) * 128], rhs=act[:, k, :],
                                start=(k == 0), stop=(k == NKF - 1)),
                                reads=[("wds", s, (k // 11) * 11), ("act", k)], writes=[("ps", b)])
                        residual_epilogue(hsrc, hdst, ti, c * 2 + hf, b, 0.5, epi, epi_n)

        def cast_even(j):
            wv = ab_w_in[j].rearrange("(kc p) n -> p kc n", p=128)
            for jj in range(8):
                for wh in range(3):
                    c0 = wh * 1024 + jj * 128
                    TR.dma("pool", lambda e, jj=jj, wh=wh, c0=c0: e.dma_start(out=abc_b[j, jj, :, :, wh * 128:(wh + 1) * 128],
                                                                             in_=wv[:, :, c0:c0 + 128]),
                           "cast", writes=[("abc", j, jj, wh)])
            for g in range(4):
                c0 = 3072 + g * 256
                TR.dma("pool", lambda e, g=g, c0=c0: e.dma_start(out=abp_b[j, g], in_=wv[:, :, c0:c0 + 256]),
                       "cast", writes=[("abp", j, g)])
            ov = ab_w_out[j].rearrange("(kc p) n -> p kc n", p=128)
            for c in range(8):
                TR.dma("pool", lambda e, c=c: e.dma_start(out=abo_b[j, c], in_=ov[:, :, c * 256:(c + 1) * 256]),
                       "cast", writes=[("abo", j, c)])

        def phase_mix_even(layer, hsrc, hdst, next_cast=None):
            j = layer // 2
            gslot = layer * 3 + 1
            A.reset()
            hT = A.alloc(NFC * TT, F32).rearrange("p (f t) -> p f t", t=TT)
            xn = A.alloc(NFC * TT, BF16).rearrange("p (f t) -> p f t", t=TT)
            cat = A.alloc(NFC * TT, BF16).rearrange("p (f t) -> p f t", t=TT)
            ucat = A.alloc(8 * 514, F32).rearrange("p (f t) -> p f t", t=514)
            pcat = A.alloc(8 * 527, F32).rearrange("p (f t) -> p f t", t=527)
            wc = [A.alloc(NFC * 384, BF16).rearrange("p (k n) -> p k n", n=384) for _ in range(2)]
            wp = [A.alloc(NFC * 256, BF16).rearrange("p (k n) -> p k n", n=256) for _ in range(2)]
            wo = [A.alloc(NFC * 256, BF16).rearrange("p (k n) -> p k n", n=256) for _ in range(2)]
            pw = A.alloc(8 * 256, BF16).rearrange("p (g n) -> p g n", n=256)
            cw = A.alloc(24, F32).rearrange("p (k j) -> p k j", j=8)
            psc = A.alloc(8, F32)
            invc0 = A.alloc(4 * TT, F32).rearrange("p (g t) -> p g t", t=TT)
            sq = [A.alloc(TT, F32) for _ in range(2)]
            rstd = A.alloc(TT, F32)
            tmp1 = A.alloc(TT, F32)
            yv = A.alloc(TT, F32)
            tA = A.alloc(528, F32)
            tB = A.alloc(528, F32)
            pl = A.alloc(2 * TT, BF16).rearrange("p (i t) -> p i t", t=TT)
            epi = [A.alloc(TT, F32) for _ in range(3)]
            epi_n = [0]
            wn = [0, 0, 0]
            TR.dma("pool", lambda e: e.dma_start(out=pw, in_=ab_pool_w[j].rearrange("g (i p) n -> p (g i) n", p=128)),
                   "oc4", writes=["pw"])
            TR.dma("sp", lambda e: e.dma_start(out=cw, in_=ab_conv_w[j].rearrange("k (j p) -> p k j", p=128),
                                               allow_slow_non_contiguous=True), "oc5", writes=["cw"])
            TR.dma("sp", lambda e: e.dma_start(out=psc, in_=ab_pool_scale[j].rearrange("(m p) -> p m", p=128),
                                               allow_slow_non_contiguous=True), "oc0", writes=["psc"])
            TR.dma("sp", lambda e: e.dma_start(out=invc0.rearrange("p g t -> p (g t)"), in_=invc0_d), "oc1", writes=["invc0"])
            TR.op("dve", lambda e: e.memset(ucat.rearrange("p f t -> p (f t)"), 0.0), writes=[("ucat", m) for m in range(8)])
            TR.op("dve", lambda e: e.memset(pcat.rearrange("p f t -> p (f t)"), 0.0), writes=[("pcat", m) for m in range(8)])
            if next_cast is not None:
                next_cast()
            for ti in range(NT):
                rms_norm_tile(hsrc, ti, gslot, hT, xn, sq, rstd)
                for jj in range(8):
                    s = wn[0] % 2
                    wn[0] += 1
                    TR.dma("sp", lambda e, jj=jj, s=s: e.dma_start(out=wc[s], in_=abc_b[j, jj]), "wc%d" % s,
                           reads=[("abc", j, jj, wh) for wh in range(3)], writes=[("wcs", s)])
                    banks = []
                    for wh in range(3):
                        b = psbank()
                        banks.append(b)
                        for k in range(NFC):
                            TR.op("pe", lambda e, s=s, wh=wh, k=k, b=b: e.matmul(
                                ps[:, b, :], lhsT=wc[s][:, k, wh * 128:(wh + 1) * 128], rhs=xn[:, k, :],
                                start=(k == 0), stop=(k == NFC - 1)),
                                reads=[("wcs", s), ("xn", k)], writes=[("ps", b)])
                    bb, bc, bh = banks
                    uk = ("ucat", jj)
                    TR.op("act", lambda e, bc=bc: e.activation(out=tmp1, in_=ps[:, bc, :], func=AF.Copy),
                          reads=[("ps", bc)], writes=["tmp1"])
                    TR.op("dve", lambda e, jj=jj, bh=bh: e.tensor_tensor(out=ucat[:, jj, 2:514], in0=ps[:, bh, :], in1=tmp1, op=ALU.mult),
                          reads=[("ps", bh), "tmp1"], writes=[uk])
                    TR.op("dve", lambda e, jj=jj: e.tensor_scalar(out=yv, in0=ucat[:, jj, 0:512], scalar1=cw[:, 0, jj:jj + 1], scalar2=None,
                                                                  op0=ALU.mult), reads=[uk, "cw"], writes=["yv"])
                    TR.op("dve", lambda e, jj=jj: e.scalar_tensor_tensor(out=yv, in0=ucat[:, jj, 1:513], scalar=cw[:, 1, jj:jj + 1], in1=yv,
                                                                         op0=ALU.mult, op1=ALU.add), reads=[uk, "cw", "yv"], writes=["yv"])
                    TR.op("dve", lambda e, jj=jj: e.scalar_tensor_tensor(out=yv, in0=ucat[:, jj, 2:514], scalar=cw[:, 2, jj:jj + 1], in1=yv,
                                                                         op0=ALU.mult, op1=ALU.add), reads=[uk, "cw", "yv"], writes=["yv"])
                    TR.op("dve", lambda e, jj=jj, bb=bb: e.tensor_tensor(out=cat[:, jj, :], in0=ps[:, bb, :], in1=yv, op=ALU.mult),
                          reads=[("ps", bb), "yv"], writes=[("cat", jj)])
                    TR.op("act", lambda e, jj=jj: e.activation(out=ucat[:, jj, 0:2], in_=ucat[:, jj, 512:514], func=AF.Copy),
                          reads=[uk], writes=[uk])
                for g in range(4):
                    W = (2, 4, 8, 16)[g]
                    s = wn[1] % 2
                    wn[1] += 1
                    TR.dma("sp", lambda e, g=g, s=s: e.dma_start(out=wp[s], in_=abp_b[j, g]), "wp%d" % s,
                           reads=[("abp", j, g)], writes=[("wps", s)])
                    for ic in range(2):
                        m = 2 * g + ic
                        pk = ("pcat", m)
                        b = psbank()
                        for k in range(NFC):
                            TR.op("pe", lambda e, s=s, ic=ic, k=k, b=b: e.matmul(
                                ps[:, b, :], lhsT=wp[s][:, k, ic * 128:(ic + 1) * 128], rhs=xn[:, k, :],
                                start=(k == 0), stop=(k == NFC - 1)),
                                reads=[("wps", s), ("xn", k)], writes=[("ps", b)])
                        TR.op("act", lambda e, m=m, b=b: e.activation(out=pcat[:, m, 15:527], in_=ps[:, b, :], func=AF.Copy),
                              reads=[("ps", b)], writes=[pk])
                        lo = 15 - (W - 1)
                        ln = 512 + W - 1
                        cur = pcat[:, m, lo:lo + ln]
                        curkey = pk
                        st = 1
                        bufs = [(tA, "tA"), (tB, "tB")]
                        bi = 0
                        while st < W:
                            nb, nk = bufs[bi]
                            bi ^= 1
                            nl = ln - st
                            TR.op("dve", lambda e, cur=cur, nb=nb, st=st, nl=nl: e.tensor_tensor(
                                out=nb[:, 0:nl], in0=cur[:, st:st + nl], in1=cur[:, 0:nl], op=ALU.add),
                                reads=[curkey], writes=[nk])
                            cur, curkey, ln = nb[:, 0:nl], nk, nl
                            st *= 2
                        assert ln == 512
                        if ti == 0:
                            TR.op("dve", lambda e, cur=cur, g=g: e.tensor_tensor(out=yv, in0=cur, in1=invc0[:, g, :], op=ALU.mult),
                                  reads=[curkey, "invc0"], writes=["yv"])
                            TR.op("dve", lambda e, m=m, ic=ic: e.tensor_tensor(out=pl[:, ic, :], in0=yv, in1=pcat[:, m, 15:527], op=ALU.subtract),
                                  reads=["yv", pk], writes=[("pl", ic)])
                        else:
                            TR.op("dve", lambda e, cur=cur, m=m, ic=ic, W=W: e.scalar_tensor_tensor(
                                out=pl[:, ic, :], in0=cur, scalar=1.0 / W, in1=pcat[:, m, 15:527], op0=ALU.mult, op1=ALU.subtract),
                                reads=[curkey, pk], writes=[("pl", ic)])
                        TR.op("act", lambda e, m=m: e.activation(out=pcat[:, m, 0:15], in_=pcat[:, m, 512:527], func=AF.Copy),
                              reads=[pk], writes=[pk])
                    for oc in range(2):
                        b = psbank()
                        for ic in range(2):
                            TR.op("pe", lambda e, g=g, ic=ic, oc=oc, b=b: e.matmul(
                                ps[:, b, :], lhsT=pw[:, 2 * g + ic, oc * 128:(oc + 1) * 128], rhs=pl[:, ic, :],
                                start=(ic == 0), stop=(ic == 1)),
                                reads=["pw", ("pl", ic)], writes=[("ps", b)])
                        mo = 2 * g + oc
                        TR.op("act", lambda e, mo=mo, b=b: e.activation(out=cat[:, 8 + mo, :], in_=ps[:, b, :], func=AF.Copy,
                                                                        scale=psc[:, mo:mo + 1]),
                              reads=[("ps", b), "psc"], writes=[("cat", 8 + mo)])
                for c in range(8):
                    s = wn[2] % 2
                    wn[2] += 1
                    TR.dma("sp", lambda e, c=c, s=s: e.dma_start(out=wo[s], in_=abo_b[j, c]), "wo%d" % s,
                           reads=[("abo", j, c)], writes=[("wos", s)])
                    for hf in range(2):
                        b = psbank()
                        for k in range(NFC):
                            TR.op("pe", lambda e, s=s, hf=hf, k=k, b=b: e.matmul(
                                ps[:, b, :], lhsT=wo[s][:, k, hf * 128:(hf + 1) * 128], rhs=cat[:, k, :],
                                start=(k == 0), stop=(k == NFC - 1)),
                                reads=[("wos", s), ("cat", k)], writes=[("ps", b)])
                        residual_epilogue(hsrc, hdst, ti, c * 2 + hf, b, 1.0, epi, epi_n)

        NQB = T // 128
        LN_EPS = 1e-5
        ASCALE = 128 ** -0.5
        GAM = [1.0 - 2.0 ** (-5.0 - h) for h in range(8)]

        def cast_odd(j):
            wv = cd_w_in[j].rearrange("(kc p) n -> p kc n", p=128)
            fcols = [0, 256, 512, 768, 1024, 1280, 1536, 1792, 3072, 3328, 3584, 3840, 6224, 6480, 6736, 6992]
            for g, c0 in enumerate(fcols):
                TR.dma("pool", lambda e, g=g, c0=c0: e.dma_start(out=cdF_b[j, g], in_=wv[:, :, c0:c0 + 256]),
                       "cast", writes=[("cdF", j, g)])
            for hh in range(2):
                TR.dma("pool", lambda e, hh=hh: e.dma_start(out=cdI_b[j, :, :, hh * 64:(hh + 1) * 64], in_=wv[:, :, 4096:4160]),
                       "cast", writes=[("cdI", j, hh)])
            for qk in range(2):
                base = 4176 + qk * 512
                for c in range(4):
                    TR.dma("pool", lambda e, qk=qk, c=c, base=base: e.dma_start(out=cdR_b[j, qk * 8 + c], in_=wv[:, :, base + c * 128:base + (c + 1) * 128]),
                           "cast", writes=[("cdR", j, qk * 8 + c, 0)])
                    for hh in range(2):
                        for half in range(2):
                            d0 = hh * 64 + half * 32
                            s0 = base + c * 128 + hh * 64 + (1 - half) * 32
                            TR.dma("pool", lambda e, qk=qk, c=c, d0=d0, s0=s0: e.dma_start(
                                out=cdR_b[j, qk * 8 + 4 + c, :, :, d0:d0 + 32], in_=wv[:, :, s0:s0 + 32]),
                                "cast", writes=[("cdR", j, qk * 8 + 4 + c, hh * 2 + half)])
            for g, c0 in enumerate([2048, 2560, 5200, 5712]):
                TR.dma("pool", lambda e, g=g, c0=c0: e.dma_start(out=cdT_b[j, g], in_=wv[:, :, c0:c0 + 512]),
                       "cast", writes=[("cdT", j, g)])
            TR.dma("pool", lambda e: e.dma_start(out=cdW_b[j], in_=wv[:, :, 4160:4176]), "cast", writes=[("cdW", j)])
            ov = cd_w_out[j].rearrange("(kc p) n -> p kc n", p=128)
            for c in range(8):
                TR.dma("pool", lambda e, c=c: e.dma_start(out=cdo_b[j, c], in_=ov[:, :, c * 256:(c + 1) * 256]),
                       "cast", writes=[("cdo", j, c)])

        def odd_stage_a(layer, hsrc):
            j = layer // 2
            gslot = layer * 3 + 1
            A.reset()
            hT = A.alloc(NFC * TT, F32).rearrange("p (f t) -> p f t", t=TT)
            xn = A.alloc(NFC * TT, BF16).rearrange("p (f t) -> p f t", t=TT)
            sq = [A.alloc(TT, F32) for _ in range(2)]
            rstd = A.alloc(TT, F32)
            wF = [A.alloc(NFC * 256, BF16).rearrange("p (k n) -> p k n", n=256) for _ in range(2)]
            wR = [A.alloc(NFC * 256, BF16).rearrange("p (a k n) -> p a k n", a=2, n=128) for _ in range(2)]
            wT = [A.alloc(NFC * 512, BF16).rearrange("p (k n) -> p k n", n=512) for _ in range(2)]
            wI = A.alloc(NFC * 128, BF16).rearrange("p (k n) -> p k n", n=128)
            wW = A.alloc(NFC * 16, BF16).rearrange("p (k n) -> p k n", n=16)
            stg = [A.alloc(TT, BF16) for _ in range(4)]
            stf = [A.alloc(TT, F32) for _ in range(3)]
            rtab = [A.alloc(2 * TT, F32).rearrange("p (a t) -> p a t", t=TT) for _ in range(2)]
            t1 = A.alloc(TT, F32)
            t2 = A.alloc(TT, F32)
            iwt = A.alloc(16, F32)
            cnt = {"F": 0, "R": 0, "T": 0, "stg": 0, "stf": 0, "rt": 0}
            TR.dma("sp", lambda e: e.dma_start(out=wI, in_=cdI_b[j]), "oc1", reads=[("cdI", j, 0), ("cdI", j, 1)], writes=["wI"])
            TR.dma("sp", lambda e: e.dma_start(out=wW, in_=cdW_b[j]), "oc2", reads=[("cdW", j)], writes=["wW"])

            def fm_group(ti, lhs_fn, wkeys, evac):
                b = psbank()
                for k in range(NFC):
                    TR.op("pe", lambda e, k=k, b=b: e.matmul(ps[:, b, :], lhsT=lhs_fn(k), rhs=xn[:, k, :],
                                                             start=(k == 0), stop=(k == NFC - 1)),
                          reads=list(wkeys) + [("xn", k)], writes=[("ps", b)])
                evac(b)

            def store_bf(ti, b, dst_rows, func=AF.Copy, use_act=True):
                sl = cnt["stg"] % 4
                cnt["stg"] += 1
                sb = stg[sl]
                if use_act:
                    TR.op("act", lambda e: e.activation(out=sb, in_=ps[:, b, :], func=func), reads=[("ps", b)], writes=[("stg", sl)])
                else:
                    TR.op("dve", lambda e: e.tensor_copy(out=sb, in_=ps[:, b, :]), reads=[("ps", b)], writes=[("stg", sl)])
                TR.dma("sp", lambda e: e.dma_start(out=dst_rows[:, ti * TT:(ti + 1) * TT], in_=sb), "stg%d" % sl,
                       reads=[("stg", sl)], writes=[("scr", id(dst_rows), ti)])

            for ti in range(NT):
                rms_norm_tile(hsrc, ti, gslot, hT, xn, sq, rstd)
                for g in range(16):
                    s_ = cnt["F"] % 2
                    cnt["F"] += 1
                    TR.dma("sp", lambda e, g=g, s_=s_: e.dma_start(out=wF[s_], in_=cdF_b[j, g]), "wF%d" % s_,
                           reads=[("cdF", j, g)], writes=[("wFs", s_)])
                    for hf in range(2):
                        ch = (g % 4) * 2 + hf
                        kind = g // 4
                        if kind < 3:
                            dst = (qT_d, kT_d, iqT_d)[kind][ch * 128:(ch + 1) * 128, :]
                            fm_group(ti, lambda k, s_=s_, hf=hf: wF[s_][:, k, hf * 128:(hf + 1) * 128], [("wFs", s_)],
                                     lambda b, dst=dst, ch=ch: store_bf(ti, b, dst, use_act=(ch % 2 == 0)))
                        else:
                            def ev(b, ch=ch, ti=ti):
                                sl = cnt["stf"] % 3
                                cnt["stf"] += 1
                                sb = stf[sl]
                                TR.op("act", lambda e: e.activation(out=sb, in_=ps[:, b, :], func=AF.Silu), reads=[("ps", b)], writes=[("stf", sl)])
                                TR.dma("sp", lambda e: e.dma_start(out=sg_d[ch * 128:(ch + 1) * 128, ti * TT:(ti + 1) * TT], in_=sb),
                                       "stf%d" % sl, reads=[("stf", sl)], writes=[("sg_d", ch, ti)])
                            fm_group(ti, lambda k, s_=s_, hf=hf: wF[s_][:, k, hf * 128:(hf + 1) * 128], [("wFs", s_)], ev)
                fm_group(ti, lambda k: wI[:, k, :], ["wI"], lambda b: store_bf(ti, b, ikT_d))
                for qk in range(2):
                    for c in range(4):
                        s_ = cnt["R"] % 2
                        cnt["R"] += 1
                        TR.dma("sp", lambda e, qk=qk, c=c, s_=s_: e.dma_start(out=wR[s_][:, 0], in_=cdR_b[j, qk * 8 + c]), "wR%d_0" % s_,
                               reads=[("cdR", j, qk * 8 + c, 0)], writes=[("wRs", s_, 0)])
                        TR.dma("sp", lambda e, qk=qk, c=c, s_=s_: e.dma_start(out=wR[s_][:, 1], in_=cdR_b[j, qk * 8 + 4 + c]), "wR%d_1" % s_,
                               reads=[("cdR", j, qk * 8 + 4 + c, q) for q in range(4)], writes=[("wRs", s_, 1)])
                        r_ = cnt["rt"] % 2
                        cnt["rt"] += 1
                        TR.dma("sp", lambda e, qk=qk, c=c, r_=r_, ti=ti: e.dma_start(out=rtab[r_], in_=rope_d[qk, c].rearrange("a p t -> p a t")[:, :, ti * TT:(ti + 1) * TT]),
                               "rt%d" % r_, writes=[("rtab", r_)])
                        bA = psbank()
                        bB = psbank()
                        for ab, b in ((0, bA), (1, bB)):
                            for k in range(NFC):
                                TR.op("pe", lambda e, k=k, b=b, ab=ab, s_=s_: e.matmul(ps[:, b, :], lhsT=wR[s_][:, ab, k, :], rhs=xn[:, k, :],
                                                                                        start=(k == 0), stop=(k == NFC - 1)),
                                      reads=[("wRs", s_, ab), ("xn", k)], writes=[("ps", b)])
                        TR.op("dve", lambda e, bA=bA, r_=r_: e.tensor_tensor(out=t1, in0=ps[:, bA, :], in1=rtab[r_][:, 0, :], op=ALU.mult),
                              reads=[("ps", bA), ("rtab", r_)], writes=["t1"])
                        TR.op("dve", lambda e, bB=bB, r_=r_: e.tensor_tensor(out=t2, in0=ps[:, bB, :], in1=rtab[r_][:, 1, :], op=ALU.mult),
                              reads=[("ps", bB), ("rtab", r_)], writes=["t2"])
                        sl = cnt["stg"] % 4
                        cnt["stg"] += 1
                        sb = stg[sl]
                        TR.op("dve", lambda e, sb=sb: e.tensor_tensor(out=sb, in0=t1, in1=t2, op=ALU.add), reads=["t1", "t2"], writes=[("stg", sl)])
                        dst = (rqT_d, rkT_d)[qk]
                        TR.dma("sp", lambda e, sb=sb, dst=dst, c=c, ti=ti: e.dma_start(out=dst[c * 128:(c + 1) * 128, ti * TT:(ti + 1) * TT], in_=sb),
                               "stg%d" % sl, reads=[("stg", sl)], writes=[("rqk", qk, c, ti)])
                for g in range(4):
                    s_ = cnt["T"] % 2
                    cnt["T"] += 1
                    TR.dma("sp", lambda e, g=g, s_=s_: e.dma_start(out=wT[s_], in_=cdT_b[j, g]), "wT%d" % s_,
                           reads=[("cdT", j, g)], writes=[("wTs", s_)])
                    dst = (vc_d, vr_d)[g // 2]
                    for tb in range(4):
                        b = psbank()
                        for k in range(NFC):
                            TR.op("pe", lambda e, k=k, b=b, tb=tb, s_=s_: e.matmul(ps[:, b, :], lhsT=xn[:, k, tb * 128:(tb + 1) * 128], rhs=wT[s_][:, k, :],
                                                                                    start=(k == 0), stop=(k == NFC - 1)),
                                  reads=[("wTs", s_), ("xn", k)], writes=[("ps", b)])
                        sl = cnt["stg"] % 4
                        cnt["stg"] += 1
                        sb = stg[sl]
                        if tb % 2 == 0:
                            TR.op("act", lambda e, sb=sb, b=b: e.activation(out=sb, in_=ps[:, b, :], func=AF.Copy), reads=[("ps", b)], writes=[("stg", sl)])
                        else:
                            TR.op("dve", lambda e, sb=sb, b=b: e.tensor_copy(out=sb, in_=ps[:, b, :]), reads=[("ps", b)], writes=[("stg", sl)])
                        r0 = ti * TT + tb * 128
                        c0 = (g % 2) * 512
                        TR.dma("sp", lambda e, sb=sb, dst=dst, r0=r0, c0=c0: e.dma_start(out=dst[r0:r0 + 128, c0:c0 + 512], in_=sb),
                               "stg%d" % sl, reads=[("stg", sl)], writes=[("v_d", g, r0)])
                for tb in range(4):
                    b = psbank()
                    for k in range(NFC):
                        TR.op("pe", lambda e, k=k, b=b, tb=tb: e.matmul(ps[:, b, 0:16], lhsT=xn[:, k, tb * 128:(tb + 1) * 128], rhs=wW[:, k, :],
                                                                         start=(k == 0), stop=(k == NFC - 1)),
                              reads=["wW", ("xn", k)], writes=[("ps", b)])
                    TR.op("act", lambda e, b=b: e.activation(out=iwt, in_=ps[:, b, 0:16], func=AF.Copy, scale=0.25 * 0.125),
                          reads=[("ps", b)], writes=["iwt"])
                    r0 = ti * TT + tb * 128
                    TR.dma("sp", lambda e, r0=r0: e.dma_start(out=iw_d[r0:r0 + 128, :], in_=iwt), "iwt", reads=["iwt"], writes=[("iw_d", r0)])

        def odd_stage_b(layer):
            j = layer // 2
            qh = [A.alloc(T, BF16) for _ in range(2)]
            kh = [A.alloc(T, BF16) for _ in range(2)]
            vh = [A.alloc(NQB * 128, BF16).rearrange("p (j e) -> p j e", e=128) for _ in range(2)]
            dd = A.alloc(8 * 128, F32).rearrange("p (h i) -> p h i", i=128)
            rgain = A.alloc(8, F32)
            NAT = 6
            at = [A.alloc(128, BF16) for _ in range(NAT)]
            dtmp = [A.alloc(128, F32) for _ in range(2)]
            oT = [A.alloc(TT, F32) for _ in range(2)]
            cen = A.alloc(TT, F32)
            sqv = A.alloc(TT, F32)
            rs = A.alloc(TT, F32)
            sgt = [A.alloc(TT, F32) for _ in range(2)]
            yb = [A.alloc(TT, BF16) for _ in range(2)]
            for s0_ in range(2):
                TR.op("pool", lambda e, s0_=s0_: e.memset(qh[s0_], 0.0), writes=[("qh", s0_)])
                TR.op("pool", lambda e, s0_=s0_: e.memset(kh[s0_], 0.0), writes=[("kh", s0_)])
            TR.dma("sp", lambda e: e.dma_start(out=dd.rearrange("p h i -> p (h i)"), in_=dd_d), "oc3", writes=["dd"])
            TR.dma("sp", lambda e: e.dma_start(out=rgain, in_=ret_norm_gain[j].rearrange("(h p) -> p h", p=128), allow_slow_non_contiguous=True),
                   "oc4", writes=["rgain"])
            n_at = 0
            n_o = 0
            n_dt = 0
            npo = [0]
            for h in range(8):
                s_ = h % 2
                TR.dma("sp", lambda e, h=h, s_=s_: e.dma_start(out=qh[s_][0:64, :], in_=rqT_d[h * 64:(h + 1) * 64, :]), "rq%d" % s_, writes=[("qh", s_)])
                TR.dma("sp", lambda e, h=h, s_=s_: e.dma_start(out=kh[s_][0:64, :], in_=rkT_d[h * 64:(h + 1) * 64, :]), "rk%d" % s_, writes=[("kh", s_)])
                for q4 in range(4):
                    TR.dma("sp", lambda e, h=h, s_=s_, q4=q4: e.dma_start(out=vh[s_][:, q4 * 8:(q4 + 1) * 8, :],
                                                                        in_=vr_d.rearrange("(j p) c -> p j c", p=128)[:, q4 * 8:(q4 + 1) * 8, h * 128:(h + 1) * 128]),
                           "vv%d_%d" % (s_, q4), writes=[("vh", s_, q4)])
                for tg in range(NT):
                    os_ = n_o % 2
                    n_o += 1
                    for ib in range(4):
                        I = tg * 4 + ib
                        po = 6 + npo[0] % 2
                        npo[0] += 1
                        pend = None
                        for J in range(I + 1):
                            b = psbank()
                            TR.op("pe", lambda e, b=b, J=J, I=I, s_=s_: e.matmul(
                                ps[:, b, 0:128], lhsT=kh[s_][:, J * 128:(J + 1) * 128], rhs=qh[s_][:, I * 128:(I + 1) * 128],
                                start=True, stop=True), reads=[("kh", s_), ("qh", s_)], writes=[("ps", b)])
                            a_ = n_at % NAT
                            n_at += 1
                            ab = at[a_]
                            if J < I:
                                sc = float(GAM[h] ** (128 * (I - J)))
                                TR.op("act", lambda e, ab=ab, b=b, sc=sc: e.activation(out=ab, in_=ps[:, b, 0:128], func=AF.Copy, scale=sc),
                                      reads=[("ps", b)], writes=[("at", a_)])
                            else:
                                d_ = n_dt % 2
                                n_dt += 1
                                dtm = dtmp[d_]
                                TR.op("act", lambda e, dtm=dtm, b=b: e.activation(out=dtm, in_=ps[:, b, 0:128], func=AF.Copy),
                                      reads=[("ps", b)], writes=[("dtmp", d_)])
                                TR.op("pool", lambda e, ab=ab, dtm=dtm, h=h: e.tensor_tensor(out=ab, in0=dtm, in1=dd[:, h, :], op=ALU.mult),
                                      reads=[("dtmp", d_), "dd"], writes=[("at", a_)])
                            if pend is not None:
                                pend()
                            pend = (lambda ab=ab, a_=a_, J=J, I=I, po=po, s_=s_: TR.op(
                                "pe", lambda e: e.matmul(ps[:, po, 0:128], lhsT=vh[s_][:, J, :], rhs=ab, start=(J == 0), stop=(J == I)),
                                reads=[("vh", s_, J // 8), ("at", a_)], writes=[("ps", po)]))
                        pend()
                        TR.op("act", lambda e, po=po, ib=ib, os_=os_: e.activation(out=oT[os_][:, ib * 128:(ib + 1) * 128], in_=ps[:, po, 0:128], func=AF.Copy),
                              reads=[("ps", po)], writes=[("oT", os_)])
                        yield I + 1
                    o_ = oT[os_]
                    b1 = psbank()
                    TR.op("pe", lambda e, b1=b1, o_=o_: e.matmul(ps[:, b1, :], lhsT=ones, rhs=o_, start=True, stop=True),
                          reads=[("oT", os_), "ones"], writes=[("ps", b1)])
                    TR.op("dve", lambda e, b1=b1, o_=o_: e.scalar_tensor_tensor(out=cen, in0=ps[:, b1, :], scalar=-1.0 / 128, in1=o_, op0=ALU.mult, op1=ALU.add),
                          reads=[("ps", b1), ("oT", os_)], writes=["cen"])
                    TR.op("act", lambda e: e.activation(out=sqv, in_=cen, func=AF.Square), reads=["cen"], writes=["sqv"])
                    b2 = psbank()
                    TR.op("pe", lambda e, b2=b2: e.matmul(ps[:, b2, :], lhsT=ones, rhs=sqv, start=True, stop=True),
                          reads=["sqv", "ones"], writes=[("ps", b2)])
                    TR.op("dve", lambda e, b2=b2: e.tensor_scalar(out=rs, in0=ps[:, b2, :], scalar1=1.0 / 128, scalar2=LN_EPS, op0=ALU.mult, op1=ALU.add),
                          reads=[("ps", b2)], writes=["rs"])
                    TR.op("act", lambda e: e.activation(out=rs, in_=rs, func=AF.Sqrt), reads=["rs"], writes=["rs"])
                    TR.op("dve", lambda e: e.reciprocal(out=rs, in_=rs), reads=["rs"], writes=["rs"])
                    TR.op("pool", lambda e: e.tensor_tensor(out=cen, in0=cen, in1=rs, op=ALU.mult), reads=["cen", "rs"], writes=["cen"])
                    g_ = sgt[os_]
                    TR.dma("sp", lambda e, g_=g_, h=h, tg=tg: e.dma_start(out=g_, in_=sg_d[h * 128:(h + 1) * 128, tg * TT:(tg + 1) * TT]), "sgt%d" % os_,
                           writes=[("sgt", os_)])
                    y_ = yb[os_]
                    TR.op("dve", lambda e, g_=g_, y_=y_, h=h: e.scalar_tensor_tensor(out=y_, in0=cen, scalar=rgain[:, h:h + 1], in1=g_, op0=ALU.mult, op1=ALU.mult),
                          reads=["cen", "rgain", ("sgt", os_)], writes=[("yb", os_)])
                    TR.dma("sp", lambda e, y_=y_, h=h, tg=tg: e.dma_start(out=catT_d[1024 + h * 128:1024 + (h + 1) * 128, tg * TT:(tg + 1) * TT], in_=y_),
                           "yb%d" % os_, reads=[("yb", os_)], writes=[("catT", 8 + h, tg)])
                    yield 1

        def odd_stage_c1(layer):
            ikE = A.alloc(T, BF16)
            ikO = A.alloc(T, BF16)
            iq = [A.alloc(8 * 128, BF16).rearrange("p (c t) -> p c t", t=128) for _ in range(2)]
            iw = [A.alloc(16, F32) for _ in range(2)]
            wabs = [A.alloc(16, F32) for _ in range(2)]
            wsg = [A.alloc(16, F32) for _ in range(2)]
            dg = [A.alloc(16 * 128, BF16).rearrange("p (h t) -> p h t", t=128) for _ in range(2)]
            acc = A.alloc(T, F32)
            work = A.alloc(T, F32)
            rl = [A.alloc(512, BF16) for _ in range(4)]
            m8 = A.alloc(8, F32)
            thrc = A.alloc(1, F32)
            nm = A.alloc(T, BF16)
            nmT = [A.alloc(NQB * 128, BF16).rearrange("p (j t) -> p j t", t=128) for _ in range(2)]
            identb = A.alloc(128, BF16)
            TR.op("pool", lambda e: e.tensor_copy(out=identb, in_=ident), reads=["ident"], writes=["identb1"])
            TR.op("pool", lambda e: e.memset(thrc, -1e29), writes=["thrc"])
            TR.op("pool", lambda e: e.memset(ikE, 0.0), writes=["ik"])
            TR.op("pool", lambda e: e.memset(ikO, 0.0), writes=["ik"])
            TR.dma("sp", lambda e: e.dma_start(out=ikE[0:64, :], in_=ikT_d[0:64, :]), "oc5", writes=["ik"])
            TR.dma("sp", lambda e: e.dma_start(out=ikO[64:128, :], in_=ikT_d[64:128, :]), "oc0", writes=["ik"])
            nrl = 0
            nacc = 0
            for I in range(NQB):
                S = 128 * (I + 1)
                s_ = I % 2
                TR.dma("sp", lambda e, I=I, s_=s_: e.dma_start(out=iq[s_], in_=iqT_d.rearrange("(c p) t -> p c t", p=128)[:, :, I * 128:(I + 1) * 128]),
                       "iq%d" % s_, writes=[("iq", s_)])
                TR.dma("sp", lambda e, I=I, s_=s_: e.dma_start(out=iw[s_], in_=iw_d[I * 128:(I + 1) * 128, :]), "iw%d" % s_, writes=[("iw", s_)])
                TR.op("dve", lambda e, s_=s_: e.tensor_scalar(out=wsg[s_], in0=iw[s_], scalar1=0.0, scalar2=2.0, op0=ALU.is_ge, op1=ALU.mult),
                      reads=[("iw", s_)], writes=[("wsg", s_)])
                TR.op("dve", lambda e, s_=s_: e.tensor_scalar(out=wsg[s_], in0=wsg[s_], scalar1=-1.0, scalar2=None, op0=ALU.add),
                      reads=[("wsg", s_)], writes=[("wsg", s_)])
                TR.op("dve", lambda e, s_=s_: e.tensor_tensor(out=wabs[s_], in0=iw[s_], in1=wsg[s_], op=ALU.mult), reads=[("iw", s_), ("wsg", s_)], writes=[("wabs", s_)])
                for hh in range(16):
                    TR.op("dve", lambda e, s_=s_, hh=hh: e.tensor_scalar(out=dg[s_][:, hh, :], in0=ident, scalar1=wsg[s_][:, hh:hh + 1], scalar2=None, op0=ALU.mult),
                          reads=["ident", ("wsg", s_)], writes=[("dg", s_)])
                for c0 in range(0, S, 512):
                    n = min(512, S - c0)
                    ck = ("acc", c0 // 512)
                    ba = 4 + nacc % 2
                    nacc += 1
                    pend = None
                    for hh in range(16):
                        b = psbank()
                        TR.op("pe", lambda e, b=b, hh=hh, c0=c0, n=n, s_=s_: e.matmul(
                            ps[:, b, 0:n], lhsT=iq[s_][:, hh // 2, :], rhs=(ikE if hh % 2 == 0 else ikO)[:, c0:c0 + n], start=True, stop=True),
                            reads=[("iq", s_), "ik"], writes=[("ps", b)])
                        r_ = nrl % 4
                        nrl += 1
                        rb = rl[r_]
                        TR.op("act", lambda e, rb=rb, b=b, n=n, s_=s_, hh=hh: e.activation(out=rb[:, 0:n], in_=ps[:, b, 0:n], func=AF.Relu, scale=wabs[s_][:, hh:hh + 1]),
                              reads=[("ps", b), ("wabs", s_)], writes=[("rl", r_)])
                        if pend is not None:
                            pend()
                        pend = (lambda rb=rb, r_=r_, hh=hh, n=n, ba=ba, s_=s_: TR.op(
                            "pe", lambda e: e.matmul(ps[:, ba, 0:n], lhsT=dg[s_][:, hh, :], rhs=rb[:, 0:n], start=(hh == 0), stop=(hh == 15)),
                            reads=[("dg", s_), ("rl", r_)], writes=[("ps", ba)]))
                    pend()
                    TR.op("act", lambda e, ba=ba, c0=c0, n=n: e.activation(out=acc[:, c0:c0 + n], in_=ps[:, ba, 0:n], func=AF.Copy),
                          reads=[("ps", ba)], writes=[ck])
                    yield 2
                acck = [("acc", c) for c in range((S + 511) // 512)]
                TR.op("pool", lambda e, S=S: e.memset(acc[0:64, S - 64:S], -1e30), reads=acck, writes=acck)
                if I >= 2:
                    for r in range(32):
                        src_ = acc if r == 0 else work
                        TR.op("dve", lambda e, src_=src_, S=S: e.max(out=m8, in_=src_[:, 0:S]), reads=acck + ["work"], writes=["m8"])
                        if r < 31:
                            TR.op("dve", lambda e, src_=src_, S=S: e.match_replace(out=work[:, 0:S], in_to_replace=m8, in_values=src_[:, 0:S], imm_value=-1e30),
                                  reads=acck + ["m8", "work"], writes=["work"])
                        if r % 2 == 1:
                            yield (I + 1) * 0.5
                    thr = m8[:, 7:8]
                    thrk = "m8"
                else:
                    thr = thrc[:, 0:1]
                    thrk = "thrc"
                TR.op("dve", lambda e, S=S, thr=thr: e.tensor_scalar(out=nm[:, 0:S], in0=acc[:, 0:S], scalar1=thr, scalar2=-30000.0, op0=ALU.is_lt, op1=ALU.mult),
                      reads=acck + [thrk], writes=["nm"])
                ns = I % 2
                for J0 in range(0, I + 1, 8):
                    js = list(range(J0, min(J0 + 8, I + 1)))
                    b = psbank()
                    pbf = ps[:, b, :].bitcast(BF16)
                    for jj, J in enumerate(js):
                        TR.op("pe", lambda e, pbf=pbf, jj=jj, J=J: e.transpose(out=pbf[:, jj * 128:(jj + 1) * 128], in_=nm[:, J * 128:(J + 1) * 128], identity=identb),
                              reads=["nm", "identb1"], writes=[("ps", b)])
                    nj = len(js)
                    TR.op("act", lambda e, pbf=pbf, J0=J0, nj=nj, ns=ns: e.activation(out=nmT[ns][:, J0:J0 + nj, :].rearrange("p j t -> p (j t)"), in_=pbf[:, 0:nj * 128], func=AF.Copy),
                          reads=[("ps", b)], writes=[("nmT", ns)])
                TR.dma("sp", lambda e, I=I, ns=ns: e.dma_start(out=nmT_d[I, :, 0:(I + 1) * 128], in_=nmT[ns][:, 0:I + 1, :].rearrange("p j t -> p (j t)")),
                       "nmT%d" % ns, reads=[("nmT", ns)], writes=[("nmT_d", I)])
                yield 1

        def odd_stage_c2(layer):
            A.reset()
            qh = [A.alloc(T, BF16) for _ in range(2)]
            kh = [A.alloc(T, BF16) for _ in range(2)]
            vh = [A.alloc(NQB * 130, BF16).rearrange("p (j e) -> p j e", e=130) for _ in range(2)]
            nmb = [A.alloc(NQB * 128, BF16).rearrange("p (j t) -> p j t", t=128) for _ in range(2)]
            bg = A.alloc(8 * 256, F32).rearrange("p (h k t) -> p h k t", h=8, k=2)
            t15 = A.alloc(8, F32)
            tab = A.alloc(256, F32)
            mb = A.alloc(8, F32)
            rc = A.alloc(8, F32)
            identb = A.alloc(128, BF16)
            mx = [A.alloc(8, F32) for _ in range(2)]
            mcol = [A.alloc(1, F32) for _ in range(2)]
            rcol = [A.alloc(1, F32) for _ in range(2)]
            rdiag = [A.alloc(128, F32) for _ in range(2)]
            pt = [A.alloc(512, BF16) for _ in range(4)]
            rcp = A.alloc(1, F32)
            yo = A.alloc(128, BF16)
            yT = [A.alloc(TT, BF16) for _ in range(2)]
            TR.op("dve", lambda e: e.tensor_copy(out=identb, in_=ident), reads=["ident"], writes=["identb"])
            for s_ in range(2):
                TR.op("dve", lambda e, s_=s_: e.memset(vh[s_].rearrange("p j e -> p (j e)"), 1.0), writes=[("vh", s_, q4) for q4 in range(4)])
            TR.dma("sp", lambda e: e.dma_start(out=bg.rearrange("p h k t -> p (h k t)"), in_=biasg_d), "oc1", writes=["bg"])
            TR.dma("sp", lambda e: e.dma_start(out=t15, in_=t15_d), "oc2", writes=["t15"])
            TR.dma("sp", lambda e: e.dma_start(out=tab, in_=tab_d), "oc3", writes=["tab"])
            for h in range(8):
                TR.op("dve", lambda e, h=h: e.tensor_scalar(out=bg[:, h].rearrange("p k t -> p (k t)"), in0=bg[:, h].rearrange("p k t -> p (k t)"),
                                                            scalar1=t15[:, h:h + 1], scalar2=1.0 / ASCALE, op0=ALU.subtract, op1=ALU.mult),
                      reads=["bg", "t15"], writes=["bg"])
            TR.op("dve", lambda e: e.tensor_reduce(out=mb, in_=tab.rearrange("p (b h) -> p h b", h=8), axis=AX.X, op=ALU.max),
                  reads=["tab"], writes=["mb"])
            TR.op("dve", lambda e: e.tensor_tensor(out=rc, in0=tab[:, 120:128], in1=mb, op=ALU.subtract), reads=["tab", "mb"], writes=["rc"])
            TR.op("dve", lambda e: e.tensor_scalar(out=rc, in0=rc, scalar1=1.0 / ASCALE, scalar2=None, op0=ALU.mult), reads=["rc"], writes=["rc"])
            C.rot = [0, 1, 2, 3, 4, 5]
            st = {"npt": 0, "npo": 0}

            def stage1(n, h, I):
                s_ = h % 2
                S = 128 * (I + 1)
                ms = n % 2
                if I == 0:
                    TR.dma("sp", lambda e: e.dma_start(out=qh[s_], in_=qT_d[h * 128:(h + 1) * 128, :]), "cq%d" % s_, writes=[("qh", s_)])
                    TR.dma("sp", lambda e: e.dma_start(out=kh[s_], in_=kT_d[h * 128:(h + 1) * 128, :]), "ck%d" % s_, writes=[("kh", s_)])
                    for q4 in range(4):
                        TR.dma("sp", lambda e, q4=q4: e.dma_start(out=vh[s_][:, q4 * 8:(q4 + 1) * 8, 0:128],
                                                                    in_=vc_d.rearrange("(j p) c -> p j c", p=128)[:, q4 * 8:(q4 + 1) * 8, h * 128:(h + 1) * 128]),
                               "vv%d_%d" % (s_, q4), writes=[("vh", s_, q4)])
                TR.dma("sp", lambda e: e.dma_start(out=nmb[ms][:, 0:I + 1, :].rearrange("p j t -> p (j t)"), in_=nmT_d[I, :, 0:(I + 1) * 128]),
                       "nmb%d" % ms, reads=[("nmT_d", I)], writes=[("nmb", ms)])
                nch = (S + 511) // 512
                for c in range(nch):
                    nn = min(512, S - c * 512)
                    b = psbank()
                    TR.op("pe", lambda e, b=b, c=c, nn=nn: e.matmul(ps[:, b, 0:nn], lhsT=qh[s_][:, I * 128:(I + 1) * 128], rhs=kh[s_][:, c * 512:c * 512 + nn],
                                                                     start=True, stop=True), reads=[("qh", s_), ("kh", s_)], writes=[("ps", b)])
                    TR.op("dve", lambda e, b=b, c=c, nn=nn: e.tensor_reduce(out=mx[ms][:, c:c + 1], in_=ps[:, b, 0:nn], axis=AX.X, op=ALU.max),
                          reads=[("ps", b)], writes=[("mx", ms)])
                TR.op("dve", lambda e: e.tensor_reduce(out=mcol[ms], in_=mx[ms][:, 0:nch], axis=AX.X, op=ALU.max), reads=[("mx", ms)], writes=[("mcol", ms)])
                TR.op("dve", lambda e: e.tensor_scalar(out=rcol[ms], in0=mcol[ms], scalar1=-1.0, scalar2=rc[:, h:h + 1], op0=ALU.mult, op1=ALU.add),
                      reads=[("mcol", ms), "rc"], writes=[("rcol", ms)])
                TR.op("dve", lambda e: e.tensor_scalar(out=rdiag[ms], in0=ident, scalar1=rcol[ms][:, 0:1], scalar2=None, op0=ALU.mult),
                      reads=["ident", ("rcol", ms)], writes=[("rdiag", ms)])

            def stage2(n, h, I):
                s_ = h % 2
                ms = n % 2
                rdg = rdiag[ms]
                po = 6 + st["npo"] % 2
                st["npo"] += 1
                pend = []
                for J0 in range(0, I + 1, 4):
                    js = list(range(J0, min(J0 + 4, I + 1)))
                    b = psbank()
                    for jj, J in enumerate(js):
                        o_ = ps[:, b, jj * 128:(jj + 1) * 128]
                        near = J >= I - 1
                        TR.op("pe", lambda e, o_=o_, J=J: e.matmul(o_, lhsT=kh[s_][:, J * 128:(J + 1) * 128], rhs=qh[s_][:, I * 128:(I + 1) * 128], start=True, stop=False),
                              reads=[("kh", s_), ("qh", s_)], writes=[("ps", b)])
                        TR.op("pe", lambda e, o_=o_: e.matmul(o_, lhsT=ones, rhs=rdg, start=False, stop=False),
                              reads=["ones", ("rdiag", ms)], writes=[("ps", b)])
                        TR.op("pe", lambda e, o_=o_, J=J, near=near: e.matmul(o_, lhsT=identb, rhs=nmb[ms][:, J, :], start=False, stop=(not near)),
                              reads=["identb", ("nmb", ms)], writes=[("ps", b)])
                        if near:
                            kb = J - (I - 1)
                            TR.op("pe", lambda e, o_=o_, kb=kb: e.matmul(o_, lhsT=ident, rhs=bg[:, h, kb, :], start=False, stop=True),
                                  reads=["ident", "bg"], writes=[("ps", b)])
                    p_ = st["npt"] % 4
                    st["npt"] += 1
                    ptb = pt[p_]
                    nj = len(js)
                    TR.op("act", lambda e, ptb=ptb, b=b, nj=nj: e.activation(out=ptb[:, 0:nj * 128], in_=ps[:, b, 0:nj * 128], func=AF.Exp, scale=ASCALE),
                          reads=[("ps", b)], writes=[("pt", p_)])
                    for f in pend:
                        f()
                    pend = []
                    for jj, J in enumerate(js):
                        pend.append(lambda ptb=ptb, p_=p_, jj=jj, J=J: TR.op(
                            "pe", lambda e: e.matmul(ps[:, po, 0:129], lhsT=ptb[:, jj * 128:(jj + 1) * 128], rhs=vh[s_][:, J, 0:129], start=(J == 0), stop=(J == I)),
                            reads=[("pt", p_), ("vh", s_, J // 8)], writes=[("ps", po)]))
                for f in pend:
                    f()
                TR.op("dve", lambda e: e.reciprocal(out=rcp, in_=ps[:, po, 128:129]), reads=[("ps", po)], writes=["rcp"])
                TR.op("act", lambda e: e.activation(out=yo, in_=ps[:, po, 0:128], func=AF.Copy, scale=rcp[:, 0:1]), reads=[("ps", po), "rcp"], writes=["yo"])
                bt = psbank()
                pbf = ps[:, bt, :].bitcast(BF16)
                TR.op("pe", lambda e: e.transpose(out=pbf[:, 0:128], in_=yo, identity=identb), reads=["yo", "identb"], writes=[("ps", bt)])
                ys = (I // 4) % 2
                TR.op("dve", lambda e: e.tensor_copy(out=yT[ys][:, (I % 4) * 128:(I % 4 + 1) * 128], in_=pbf[:, 0:128]),
                      reads=[("ps", bt)], writes=[("yT", ys)])
                if I % 4 == 3:
                    tg = I // 4
                    TR.dma("sp", lambda e: e.dma_start(out=catT_d[h * 128:(h + 1) * 128, tg * TT:(tg + 1) * TT], in_=yT[ys]),
                           "yT%d" % ys, reads=[("yT", ys)], writes=[("catT", h, tg)])

            items = [(h, I) for h in range(8) for I in range(NQB)]
            stage1(0, *items[0])
            for n in range(len(items)):
                if n + 1 < len(items):
                    stage1(n + 1, *items[n + 1])
                stage2(n, *items[n])


        def odd_stage_d(layer, hsrc, hdst):
            j = layer // 2
            A.reset()
            cat = [A.alloc(NFC * TT, BF16).rearrange("p (f t) -> p f t", t=TT) for _ in range(2)]
            wo = [A.alloc(NFC * 256, BF16).rearrange("p (k n) -> p k n", n=256) for _ in range(2)]
            epi = [A.alloc(TT, F32) for _ in range(3)]
            epi_n = [0]
            nw = 0
            cv = catT_d.rearrange("(f p) t -> p f t", p=128)
            for ti in range(NT):
                cs = ti % 2
                for q in range(4):
                    TR.dma("sp", lambda e, q=q, ti=ti, cs=cs: e.dma_start(out=cat[cs][:, q * 4:(q + 1) * 4, :], in_=cv[:, q * 4:(q + 1) * 4, ti * TT:(ti + 1) * TT]),
                           "cat%d_%d" % (cs, q), writes=[("cat", cs, q)])
                for c in range(8):
                    s_ = nw % 2
                    nw += 1
                    TR.dma("sp", lambda e, c=c, s_=s_: e.dma_start(out=wo[s_], in_=cdo_b[j, c]), "wo%d" % s_, reads=[("cdo", j, c)], writes=[("wos", s_)])
                    for hf in range(2):
                        b = psbank()
                        for k in range(NFC):
                            TR.op("pe", lambda e, s_=s_, hf=hf, k=k, b=b, cs=cs: e.matmul(ps[:, b, :], lhsT=wo[s_][:, k, hf * 128:(hf + 1) * 128], rhs=cat[cs][:, k, :],
                                                                                          start=(k == 0), stop=(k == NFC - 1)),
                                  reads=[("wos", s_), ("cat", cs, k // 4)], writes=[("ps", b)])
                        residual_epilogue(hsrc, hdst, ti, c * 2 + hf, b, 1.0, epi, epi_n)

        def run_merged(gens, totals):
            prog = [0.0] * len(gens)
            alive = [True] * len(gens)
            while any(alive):
                cand = [i for i in range(len(gens)) if alive[i]]
                i = min(cand, key=lambda k: prog[k] / totals[k])
                try:
                    prog[i] += next(gens[i])
                except StopIteration:
                    alive[i] = False

        def phase_mix_odd(layer, hsrc, hdst, next_cast=None, stages="abcd"):
            if next_cast is not None:
                next_cast()
            odd_stage_a(layer, hsrc)
            TR.barrier()
            if "b" not in stages:
                A.reset()
                zt = A.alloc(T, BF16)
                TR.op("dve", lambda e: e.memset(zt, 0.0), writes=["zt"])
                for hh_ in range(8):
                    TR.dma("sp", lambda e, hh_=hh_: e.dma_start(out=catT_d[1024 + hh_ * 128:1024 + (hh_ + 1) * 128, :], in_=zt), "zt", reads=["zt"], writes=[("catz", 8 + hh_)])
                TR.barrier()
            if "c" not in stages:
                A.reset()
                zt = A.alloc(T, BF16)
                TR.op("dve", lambda e: e.memset(zt, 0.0), writes=["zt"])
                for hh_ in range(8):
                    TR.dma("sp", lambda e, hh_=hh_: e.dma_start(out=catT_d[hh_ * 128:(hh_ + 1) * 128, :], in_=zt), "zt", reads=["zt"], writes=[("catz", hh_)])
                TR.barrier()
            A.reset()
            C.rot = [0, 1, 2, 3]
            gens, totals = [], []
            if "b" in stages:
                gens.append(odd_stage_b(layer))
                totals.append(8 * 528 + 64.0)
            if "c" in stages:
                gens.append(odd_stage_c1(layer))
                totals.append(2 * 148 + 16 * 525 * 0.5 + 32.0)
            run_merged(gens, totals)
            C.rot = list(range(8))
            TR.barrier()
            if "c" in stages:
                odd_stage_c2(layer)
                C.rot = list(range(8))
                TR.barrier()
            odd_stage_d(layer, hsrc, hdst)


        def phase_final(hsrc):
            A.reset()
            hT = A.alloc(NFC * TT, F32).rearrange("p (f t) -> p f t", t=TT)
            y = A.alloc(NFC * TT, F32).rearrange("p (f t) -> p f t", t=TT)
            sq = [A.alloc(TT, F32) for _ in range(2)]
            rstd = A.alloc(TT, F32)
            ot = [A.alloc(D, F32) for _ in range(2)]
            n = 0
            for ti in range(NT):
                rms_norm_tile(hsrc, ti, 12, hT, y, sq, rstd)
                for tb in range(TT // 128):
                    o = ot[n % 2]
                    okey = ("ot", n % 2)
                    n += 1
                    for f4 in range(NFC // 4):
                        b = psbank()
                        for k in range(4):
                            f = f4 * 4 + k
                            TR.op("pe", lambda e, f=f, k=k, b=b, tb=tb: e.transpose(
                                out=ps[:, b, k * 128:(k + 1) * 128], in_=y[:, f, tb * 128:(tb + 1) * 128], identity=ident),
                                reads=[("xn", f), "ident"], writes=[("ps", b)])
                        if f4 % 2 == 0:
                            TR.op("act", lambda e, o=o, b=b, f4=f4: e.activation(out=o[:, f4 * 512:(f4 + 1) * 512], in_=ps[:, b, :], func=AF.Copy),
                                  reads=[("ps", b)], writes=[okey])
                        else:
                            TR.op("dve", lambda e, o=o, b=b, f4=f4: e.tensor_copy(out=o[:, f4 * 512:(f4 + 1) * 512], in_=ps[:, b, :]),
                                  reads=[("ps", b)], writes=[okey])
                    r0 = ti * TT + tb * 128
                    TR.dma("sp", lambda e, o=o, r0=r0: e.dma_start(out=out[r0:r0 + 128, :], in_=o), "ot%d" % ((n - 1) % 2),
                           reads=[okey], writes=[("out", r0)])

        plan = []
        plan.append(("prologue",))
        for layer in range(DEPTH):
            plan.append(("ffn", layer * 2, layer * 3 + 0))
            plan.append(("mix", layer))
            plan.append(("ffn", layer * 2 + 1, layer * 3 + 2))
        plan.append(("final",))
        if phases is not None:
            plan = [p for p in plan if p in phases or p[0] in ("prologue", "final")]

        def caster(p):
            if p[0] == "ffn":
                return lambda: cast_ffn(p[1])
            if p[0] == "mix" and p[1] % 2 == 0:
                return lambda: cast_even(p[1] // 2)
            if p[0] == "mix":
                return lambda: cast_odd(p[1] // 2)
            return None
        wplan = [p for p in plan if caster(p) is not None]
        cur = 0
        if wplan:
            caster(wplan[0])()
        for p in plan:
            nxt = None
            if p in wplan:
                i = wplan.index(p)
                if i + 1 < len(wplan):
                    nxt = caster(wplan[i + 1])
            if p[0] == "prologue":
                phase_prologue(hbuf[cur])
            elif p[0] == "ffn":
                phase_ffn(p[1], p[2], hbuf[cur], hbuf[1 - cur], nxt)
                cur = 1 - cur
            elif p[0] == "mix":
                if p[1] % 2 == 0:
                    phase_mix_even(p[1], hbuf[cur], hbuf[1 - cur], nxt)
                    cur = 1 - cur
                else:
                    phase_mix_odd(p[1], hbuf[cur], hbuf[1 - cur], nxt, stages=odd_stages)
                    cur = 1 - cur
            elif p[0] == "final":
                phase_final(hbuf[cur])
            TR.barrier()
        TR.emit(block)
        C.nops = TR.nops
    return nc


def _t5_bucket_np(rel):
    import jax
    import jax.numpy as jnp
    import math
    with jax.default_device(jax.devices("cpu")[0]):
        rel = jnp.asarray(rel, dtype=jnp.int32)
        nb = 16
        max_exact = 8
        ret = jnp.where(rel > 0, nb, 0)
        n = jnp.abs(rel)
        large = max_exact + (jnp.log(jnp.maximum(n, 1).astype(jnp.float32) / max_exact)
                             / math.log(128 / max_exact) * (nb - max_exact)).astype(jnp.int32)
        large = jnp.minimum(large, nb - 1)
        return np.asarray(ret + jnp.where(n < max_exact, n, large))


def host_constants(rel_bias_table=None):
    t = np.arange(TT)
    invc = np.stack([1.0 / np.minimum(t + 1, w) for w in (2, 4, 8, 16)]).astype(np.float32)
    invc0 = np.ascontiguousarray(np.broadcast_to(invc.reshape(1, 4 * TT), (128, 4 * TT)))
    out = {"ident": np.eye(128, dtype=np.float32), "invc0": invc0}
    pos = np.arange(T, dtype=np.float64)
    gam = np.array([1.0 - 2.0 ** (-5.0 - h) for h in range(8)], dtype=np.float64)
    p = np.arange(128)
    i = p % 64
    f = i % 32
    sign = np.where(i < 32, -1.0, 1.0)
    freqs = 10000.0 ** (-f.astype(np.float64) / 32.0)
    ang = (pos[None, :].astype(np.float32) * freqs[:, None].astype(np.float32)).astype(np.float64)
    cos, sin = np.cos(ang), np.sin(ang) * sign[:, None]
    tl = (np.arange(T) % 128).astype(np.float64)
    rope = np.zeros((2, 4, 2, 128, T), dtype=np.float32)
    for c in range(4):
        hh = 2 * c + p // 64
        dq = gam[hh][:, None] ** tl[None, :]
        dk = (64.0 ** -0.5) * gam[hh][:, None] ** (-tl[None, :])
        rope[0, c, 0], rope[0, c, 1] = cos * dq, sin * dq
        rope[1, c, 0], rope[1, c, 1] = cos * dk, sin * dk
    out["rope_tab"] = rope
    jl = np.arange(128)[:, None]
    il = np.arange(128)[None, :]
    vis = (jl < ((il // 64) + 1) * 64)
    dd = np.zeros((128, 8, 128), dtype=np.float32)
    for h in range(8):
        m = np.where(il >= jl, 1.0, gam[h] ** (2.0 * (jl - il)))
        dd[:, h, :] = m * vis
    out["ret_diag"] = dd.reshape(128, 8 * 128)
    if rel_bias_table is not None:
        tab = np.asarray(rel_bias_table, dtype=np.float32)
        sl = np.arange(128)[:, None, None]
        blk = np.arange(2)[None, :, None]
        tl_ = np.arange(128)[None, None, :]
        rel = blk * 128 + sl - 128 - tl_
        bidx = _t5_bucket_np(rel)
        g = tab[bidx]
        out["bias_g"] = np.ascontiguousarray(np.transpose(g, (0, 3, 1, 2))).reshape(128, 8 * 2 * 128)
        out["bias_t15"] = np.ascontiguousarray(np.broadcast_to(tab[15:16, :], (128, 8)))
        out["bias_tab"] = np.ascontiguousarray(np.broadcast_to(tab.reshape(1, 256), (128, 256)))
    return out


def make_in_maps(inputs, n_cores=N_CORES):
    consts = host_constants(inputs.get("rel_bias_table"))
    shared = {
        "norm_gains": np.ascontiguousarray(np.asarray(inputs["norm_gains"], dtype=np.float32).reshape(DEPTH * 3, D)),
        "ffn_w_gate_up": np.ascontiguousarray(np.asarray(inputs["ffn_w_gate_up"], dtype=np.float32).reshape(DEPTH * 2, D, 2 * DFF)),
        "ffn_w_down": np.ascontiguousarray(np.asarray(inputs["ffn_w_down"], dtype=np.float32).reshape(DEPTH * 2, DFF, D)),
        "final_norm": np.ascontiguousarray(np.asarray(inputs["final_norm"], dtype=np.float32).reshape(1, D)),
    }
    for k in ("ab_w_in", "ab_conv_w", "ab_pool_w", "ab_pool_scale", "ab_w_out", "cd_w_in", "ret_norm_gain", "cd_w_out"):
        shared[k] = np.ascontiguousarray(np.asarray(inputs[k], dtype=np.float32))
    shared.update(consts)
    xs = np.asarray(inputs["x"], dtype=np.float32)
    maps = []
    for c in range(n_cores):
        m = dict(shared)
        m["x"] = np.ascontiguousarray(xs[c])
        maps.append(m)
    return maps


def kernel(**inputs):
    nc = build_program()
    in_maps = make_in_maps(inputs)
    res = run_bass_kernel_spmd(nc, in_maps, core_ids=list(range(N_CORES)))
    return np.stack([np.asarray(r["out"], dtype=np.float32) for r in res.results], axis=0)
```

```python
import numpy as np
import concourse.bass as bass
import concourse.mybir as mybir
from concourse.bass_utils import run_bass_kernel_spmd

F32 = mybir.dt.float32
BF16 = mybir.dt.bfloat16
AF = mybir.ActivationFunctionType
ALU = mybir.AluOpType
AX = mybir.AxisListType

D = 2048
T = 4096
DFF = 5632
DEPTH = 4
NFC = D // 128
TT = 512
NT = T // TT
RMS_EPS = 1e-6
N_CORES = 8

CENGS = ("pe", "act", "dve", "pool")
ENGS = CENGS + ("sp",)


class Op:
    __slots__ = ("eng", "fn", "deps", "xdeps", "is_dma", "dsem", "dval", "val", "waited")

    def __init__(self, eng, fn, is_dma=False):
        self.eng = eng
        self.fn = fn
        self.deps = []
        self.xdeps = []
        self.is_dma = is_dma
        self.dsem = None
        self.dval = 0
        self.val = None
        self.waited = False


SEM_ALIAS = {}
for _a in range(2):
    for _b in range(4):
        SEM_ALIAS["cat%d_%d" % (_a, _b)] = "wd%d_%d" % (_a, _b)
for _b in range(4):
    SEM_ALIAS["vv0_%d" % _b] = "hT%d" % _b
    SEM_ALIAS["vv1_%d" % _b] = "wd0_%d" % _b
for _a in range(2):
    SEM_ALIAS["wF%d" % _a] = "wg%d" % _a
    SEM_ALIAS["wT%d" % _a] = "wu%d" % _a
    SEM_ALIAS["wR%d_0" % _a] = "wc%d" % _a
    SEM_ALIAS["wR%d_1" % _a] = "wp%d" % _a
    SEM_ALIAS["cq%d" % _a] = "rq%d" % _a
    SEM_ALIAS["ck%d" % _a] = "rk%d" % _a
    SEM_ALIAS["xt%d" % _a] = "ot%d" % _a
for _a in range(3):
    SEM_ALIAS["st%d" % _a] = "stg%d" % _a


class Tracker:
    def __init__(self, nc):
        self.nc = nc
        self.esem = {e: nc.alloc_semaphore(name="es_" + e) for e in CENGS}
        self.ecount = {e: 0 for e in CENGS}
        self.dma_sems = {}
        self.dma_cnt = {}
        self.last_w = {}
        self.readers = {}
        self.byeng = {e: [] for e in ENGS}
        self.nops = 0

    def _add(self, op, reads, writes):
        deps = []
        for r in reads:
            w = self.last_w.get(r)
            if w is not None:
                deps.append(w)
        for w_ in writes:
            w = self.last_w.get(w_)
            if w is not None:
                deps.append(w)
            deps.extend(self.readers.get(w_, ()))
        for r in reads:
            self.readers.setdefault(r, []).append(op)
        for w_ in writes:
            self.last_w[w_] = op
            self.readers[w_] = []
        seen = set()
        for d in deps:
            if d is op or id(d) in seen:
                continue
            if op.eng == "pe" and d.eng == "pe" and not d.is_dma and not op.is_dma:
                continue
            seen.add(id(d))
            op.deps.append(d)
            d.waited = True
        self.byeng[op.eng].append(op)
        self.nops += 1
        return op

    def op(self, eng, fn, reads=(), writes=()):
        return self._add(Op(eng, fn), reads, writes)

    def dma(self, eng, fn, sem, reads=(), writes=()):
        sem = SEM_ALIAS.get(sem, sem)
        op = Op(eng, fn, is_dma=True)
        if sem not in self.dma_sems:
            self.dma_sems[sem] = self.nc.alloc_semaphore(name="ds_" + sem)
            self.dma_cnt[sem] = 0
        self.dma_cnt[sem] += 16
        op.dsem = sem
        op.dval = self.dma_cnt[sem]
        return self._add(op, reads, writes)

    def barrier(self):
        bs = []
        for e in CENGS:
            b = Op(e, None)
            b.waited = True
            for f in CENGS:
                if f != e:
                    for o in reversed(self.byeng[f]):
                        if not o.is_dma:
                            b.deps.append(o)
                            o.waited = True
                            break
            for s, c in self.dma_cnt.items():
                if c:
                    b.xdeps.append((s, c))
            bs.append(b)
        for b in bs:
            self.byeng[b.eng].append(b)
        for e in ENGS:
            c = Op(e, None)
            c.deps = list(bs)
            self.byeng[e].append(c)
        self.last_w = {}
        self.readers = {}

    def emit(self, block):
        for e in CENGS:
            for op in self.byeng[e]:
                if not op.is_dma and op.waited:
                    self.ecount[e] += 1
                    op.val = self.ecount[e]
        seen = {}

        def run(en, eng):
            def w(key, sem, v):
                if seen.get((en, key), 0) >= v:
                    return
                seen[(en, key)] = v
                eng.wait_ge(sem, v)

            for op in self.byeng[en]:
                for d in op.deps:
                    if d.is_dma:
                        w(("d", d.dsem), self.dma_sems[d.dsem], d.dval)
                    elif d.eng != "sp":
                        w(("e", d.eng), self.esem[d.eng], d.val)
                for (s, v) in op.xdeps:
                    w(("d", s), self.dma_sems[s], v)
                if op.fn is None:
                    if op.val is not None:
                        eng.nop().then_inc(self.esem[op.eng], 1)
                    continue
                ins = op.fn(eng)
                if op.is_dma:
                    ins.then_inc(self.dma_sems[op.dsem], 16)
                elif op.val is not None:
                    ins.then_inc(self.esem[op.eng], 1)

        @block.tensor
        def _(e):
            run("pe", e)

        @block.scalar
        def _(e):
            run("act", e)

        @block.vector
        def _(e):
            run("dve", e)

        @block.gpsimd
        def _(e):
            run("pool", e)

        @block.sync
        def _(e):
            run("sp", e)


class Arena:
    def __init__(self, big, n32):
        self.big = big
        self.n32 = n32
        self.off = 0
        self.mark = 0

    def set_mark(self):
        self.mark = self.off

    def reset(self):
        self.off = self.mark

    def alloc(self, n_elems, dtype):
        sz = 2 if dtype == BF16 else 4
        n32 = (n_elems * sz + 3) // 4
        n32 = (n32 + 7) // 8 * 8
        assert self.off + n32 <= self.n32, f"SBUF arena overflow {self.off}+{n32}>{self.n32}"
        ap = self.big[:, self.off:self.off + n32]
        self.off += n32
        if dtype != F32:
            ap = ap.bitcast(dtype)
        return ap[:, :n_elems]


class Ctx:
    pass


def build_program(phases=None, debug_out=False, odd_stages="abcd"):
    nc = bass.Bass("TRN2", target_bir_lowering=False)
    C = Ctx()
    C.nc = nc
    dt = nc.dram_tensor
    x = dt("x", [T, D], F32, kind="ExternalInput").ap()
    norm_gains = dt("norm_gains", [DEPTH * 3, D], F32, kind="ExternalInput").ap()
    w_gu = dt("ffn_w_gate_up", [DEPTH * 2, D, 2 * DFF], F32, kind="ExternalInput").ap()
    w_dn = dt("ffn_w_down", [DEPTH * 2, DFF, D], F32, kind="ExternalInput").ap()
    final_norm = dt("final_norm", [1, D], F32, kind="ExternalInput").ap()
    ident_d = dt("ident", [128, 128], F32, kind="ExternalInput").ap()
    ab_w_in = dt("ab_w_in", [2, D, 4096], F32, kind="ExternalInput").ap()
    ab_conv_w = dt("ab_conv_w", [2, 3, 1024], F32, kind="ExternalInput").ap()
    ab_pool_w = dt("ab_pool_w", [2, 4, 256, 256], F32, kind="ExternalInput").ap()
    ab_pool_scale = dt("ab_pool_scale", [2, 1024], F32, kind="ExternalInput").ap()
    ab_w_out = dt("ab_w_out", [2, D, D], F32, kind="ExternalInput").ap()
    invc0_d = dt("invc0", [128, 4 * TT], F32, kind="ExternalInput").ap()
    cd_w_in = dt("cd_w_in", [2, D, 7248], F32, kind="ExternalInput").ap()
    ret_norm_gain = dt("ret_norm_gain", [2, 1024], F32, kind="ExternalInput").ap()
    cd_w_out = dt("cd_w_out", [2, D, D], F32, kind="ExternalInput").ap()
    rope_d = dt("rope_tab", [2, 4, 2, 128, T], F32, kind="ExternalInput").ap()
    dd_d = dt("ret_diag", [128, 8 * 128], F32, kind="ExternalInput").ap()
    biasg_d = dt("bias_g", [128, 8 * 2 * 128], F32, kind="ExternalInput").ap()
    t15_d = dt("bias_t15", [128, 8], F32, kind="ExternalInput").ap()
    tab_d = dt("bias_tab", [128, 256], F32, kind="ExternalInput").ap()
    out = dt("out", [T, D], F32, kind="ExternalOutput").ap()
    hbuf = [dt("hA", [D, T], F32, kind="Internal").ap(), dt("hB", [D, T], F32, kind="Internal").ap()]
    NGC = DFF // 256
    NDC = D // 256
    NKF = DFF // 128
    wg_b = dt("wg_b", [DEPTH * 2, NGC, 128, NFC, 256], BF16, kind="Internal").ap()
    wu_b = dt("wu_b", [DEPTH * 2, NGC, 128, NFC, 256], BF16, kind="Internal").ap()
    wd_b = dt("wd_b", [DEPTH * 2, NDC, 128, NKF, 256], BF16, kind="Internal").ap()
    abc_b = dt("abc_b", [2, 8, 128, NFC, 384], BF16, kind="Internal").ap()
    abp_b = dt("abp_b", [2, 4, 128, NFC, 256], BF16, kind="Internal").ap()
    abo_b = dt("abo_b", [2, 8, 128, NFC, 256], BF16, kind="Internal").ap()
    cdF_b = dt("cdF_b", [2, 16, 128, NFC, 256], BF16, kind="Internal").ap()
    cdI_b = dt("cdI_b", [2, 128, NFC, 128], BF16, kind="Internal").ap()
    cdR_b = dt("cdR_b", [2, 16, 128, NFC, 128], BF16, kind="Internal").ap()
    cdT_b = dt("cdT_b", [2, 4, 128, NFC, 512], BF16, kind="Internal").ap()
    cdW_b = dt("cdW_b", [2, 128, NFC, 16], BF16, kind="Internal").ap()
    cdo_b = dt("cdo_b", [2, 8, 128, NFC, 256], BF16, kind="Internal").ap()
    qT_d = dt("qT_d", [1024, T], BF16, kind="Internal").ap()
    kT_d = dt("kT_d", [1024, T], BF16, kind="Internal").ap()
    iqT_d = dt("iqT_d", [1024, T], BF16, kind="Internal").ap()
    ikT_d = dt("ikT_d", [128, T], BF16, kind="Internal").ap()
    iw_d = dt("iw_d", [T, 16], F32, kind="Internal").ap()
    rqT_d = dt("rqT_d", [512, T], BF16, kind="Internal").ap()
    rkT_d = dt("rkT_d", [512, T], BF16, kind="Internal").ap()
    vc_d = dt("vc_d", [T, 1024], BF16, kind="Internal").ap()
    vr_d = dt("vr_d", [T, 1024], BF16, kind="Internal").ap()
    sg_d = dt("sg_d", [1024, T], F32, kind="Internal").ap()
    catT_d = dt("catT_d", [D, T], BF16, kind="Internal").ap()
    nmT_d = dt("nmT_d", [32, 128, 32 * 128], BF16, kind="Internal").ap()

    N32 = 51 * 1024 + 512
    es = nc.sbuf_tensor("big", [128, N32], F32)
    ps_cm = nc.psum_tensor("ps", [128, 8, 512], F32)
    with es as big, ps_cm as ps, nc.Block() as block:
        TR = Tracker(nc)
        A = Arena(big, N32)
        C.TR, C.A, C.ps = TR, A, ps
        C.psn = 0

        C.rot = list(range(8))

        def psbank():
            b = C.rot[C.psn % len(C.rot)]
            C.psn += 1
            return b
        C.psbank = psbank

        ident = A.alloc(128, F32)
        ones = A.alloc(128, F32)
        gains = A.alloc(13 * NFC, F32).rearrange("p (s f) -> p s f", f=NFC)
        TR.dma("sp", lambda e: e.dma_start(out=ident, in_=ident_d), "cid", writes=["ident"])
        TR.op("dve", lambda e: e.memset(ones, 1.0), writes=["ones"])
        for s in range(13):
            src = norm_gains[s] if s < 12 else final_norm[0]
            TR.dma("sp", lambda e, s=s, src=src: e.dma_start(
                out=gains[:, s, :], in_=src.rearrange("(f p) -> p f", p=128), allow_slow_non_contiguous=True),
                "const", writes=["gains"])
        C.ident, C.ones, C.gains = ident, ones, gains
        A.set_mark()

        def cast_ffn(fi):
            wv = w_gu[fi].rearrange("(kc p) n -> p kc n", p=128)
            for c in range(NGC):
                TR.dma("pool", lambda e, c=c: e.dma_start(out=wg_b[fi, c], in_=wv[:, :, c * 256:(c + 1) * 256]),
                       "cast", writes=[("wg", fi, c)])
                TR.dma("pool", lambda e, c=c: e.dma_start(out=wu_b[fi, c], in_=wv[:, :, DFF + c * 256:DFF + (c + 1) * 256]),
                       "cast", writes=[("wu", fi, c)])
            dv = w_dn[fi].rearrange("(kc p) n -> p kc n", p=128)
            for c in range(NDC):
                for k0 in range(0, NKF, 11):
                    TR.dma("pool", lambda e, c=c, k0=k0: e.dma_start(out=wd_b[fi, c, :, k0:k0 + 11, :],
                                                                      in_=dv[:, k0:k0 + 11, c * 256:(c + 1) * 256]),
                           "cast", writes=[("wd", fi, c, k0)])

        def rms_norm_tile(hsrc, ti, gslot, hT, xn, sq, rstd, xkey="xn"):
            t0 = ti * TT
            hv = hsrc.rearrange("(f p) t -> p f t", p=128)
            for q in range(4):
                TR.dma("sp", lambda e, q=q: e.dma_start(out=hT[:, q * 4:(q + 1) * 4, :], in_=hv[:, q * 4:(q + 1) * 4, t0:t0 + TT]),
                       "hT%d" % q, writes=[("hT", q)])
            b = psbank()
            for f in range(NFC):
                s = sq[f % 2]
                TR.op("act", lambda e, f=f, s=s: e.activation(out=s, in_=hT[:, f, :], func=AF.Square),
                      reads=[("hT", f // 4)], writes=[("sq", f % 2)])
                TR.op("pe", lambda e, f=f, s=s: e.matmul(ps[:, b, :], lhsT=ones, rhs=s, start=(f == 0), stop=(f == NFC - 1)),
                      reads=[("sq", f % 2), "ones"], writes=[("ps", b)])
            TR.op("dve", lambda e: e.tensor_scalar(out=rstd, in0=ps[:, b, :], scalar1=1.0 / D, scalar2=RMS_EPS,
                                                   op0=ALU.mult, op1=ALU.add), reads=[("ps", b)], writes=["rstd"])
            TR.op("act", lambda e: e.activation(out=rstd, in_=rstd, func=AF.Sqrt), reads=["rstd"], writes=["rstd"])
            TR.op("dve", lambda e: e.reciprocal(out=rstd, in_=rstd), reads=["rstd"], writes=["rstd"])
            for f in range(NFC):
                TR.op("dve", lambda e, f=f: e.scalar_tensor_tensor(out=xn[:, f, :], in0=hT[:, f, :], scalar=gains[:, gslot, f:f + 1],
                                                                 in1=rstd, op0=ALU.mult, op1=ALU.mult),
                      reads=[("hT", f // 4), "rstd", "gains"], writes=[(xkey, f)])

        def residual_epilogue(hsrc, hdst, ti, fo, b, scale, epi, epi_n):
            t0 = ti * TT
            slot = epi_n[0] % len(epi)
            epi_n[0] += 1
            et = epi[slot]
            TR.dma("act", lambda e: e.dma_start(out=et, in_=hsrc[fo * 128:(fo + 1) * 128, t0:t0 + TT]),
                   "epi%d" % slot, writes=[("epi", slot)])
            TR.op("dve", lambda e: e.scalar_tensor_tensor(out=et, in0=ps[:, b, :], scalar=float(scale), in1=et,
                                                          op0=ALU.mult, op1=ALU.add),
                  reads=[("ps", b)], writes=[("epi", slot)])
            TR.dma("act", lambda e: e.dma_start(out=hdst[fo * 128:(fo + 1) * 128, t0:t0 + TT], in_=et),
                   "epi%d" % slot, reads=[("epi", slot)], writes=[("hdst", fo, ti)])

        def phase_prologue(hdst):
            A.reset()
            xt = [A.alloc(D, F32) for _ in range(2)]
            st = [A.alloc(TT, F32) for _ in range(3)]
            n = 0
            for tb in range(T // 128):
                xs = xt[tb % 2]
                TR.dma("sp", lambda e, xs=xs, tb=tb: e.dma_start(out=xs, in_=x[tb * 128:(tb + 1) * 128, :]),
                       "xt%d" % (tb % 2), writes=[("xt", tb % 2)])
                for f4 in range(NFC // 4):
                    b = psbank()
                    for k in range(4):
                        f = f4 * 4 + k
                        TR.op("pe", lambda e, xs=xs, f=f, k=k, b=b: e.transpose(out=ps[:, b, k * 128:(k + 1) * 128],
                                                                              in_=xs[:, f * 128:(f + 1) * 128], identity=ident),
                              reads=[("xt", tb % 2), "ident"], writes=[("ps", b)])
                    s = n % 3
                    n += 1
                    sb = st[s]
                    eng = "act" if n % 2 == 0 else "dve"
                    if eng == "act":
                        TR.op("act", lambda e, sb=sb, b=b: e.activation(out=sb, in_=ps[:, b, :], func=AF.Copy),
                              reads=[("ps", b)], writes=[("st", s)])
                    else:
                        TR.op("dve", lambda e, sb=sb, b=b: e.tensor_copy(out=sb, in_=ps[:, b, :]),
                              reads=[("ps", b)], writes=[("st", s)])
                    dst = hdst.rearrange("(f p) t -> p f t", p=128)[:, f4 * 4:(f4 + 1) * 4, tb * 128:(tb + 1) * 128]
                    TR.dma("sp", lambda e, sb=sb, dst=dst: e.dma_start(out=dst, in_=sb.rearrange("p (k t) -> p k t", t=128)),
                           "st%d" % s, reads=[("st", s)], writes=[("hdst", f4, tb)])

        def phase_ffn(fi, gslot, hsrc, hdst, next_cast=None):
            A.reset()
            hT = A.alloc(NFC * TT, F32).rearrange("p (f t) -> p f t", t=TT)
            xns = [A.alloc(NFC * TT, BF16).rearrange("p (f t) -> p f t", t=TT) for _ in range(2)]
            act = A.alloc(NKF * TT, BF16).rearrange("p (f t) -> p f t", t=TT)
            wg = [A.alloc(NFC * 256, BF16).rearrange("p (k n) -> p k n", n=256) for _ in range(2)]
            wu = [A.alloc(NFC * 256, BF16).rearrange("p (k n) -> p k n", n=256) for _ in range(2)]
            wd = [A.alloc(NKF * 256, BF16).rearrange("p (k n) -> p k n", n=256) for _ in range(2)]
            sq = [A.alloc(TT, F32) for _ in range(2)]
            rstd = A.alloc(TT, F32)
            sg = [A.alloc(TT, F32) for _ in range(2)]
            epi = [A.alloc(TT, F32) for _ in range(4)]
            epi_n = [0]
            wn = [0, 0]
            if next_cast is not None:
                next_cast()
            rms_norm_tile(hsrc, 0, gslot, hT, xns[0], sq, rstd, xkey="xn0")
            for ti in range(NT):
                xn = xns[ti % 2]
                xk = "xn%d" % (ti % 2)
                for c in range(NGC):
                    s = wn[0] % 2
                    wn[0] += 1
                    TR.dma("sp", lambda e, c=c, s=s: e.dma_start(out=wg[s], in_=wg_b[fi, c]), "wg%d" % s,
                           reads=[("wg", fi, c)], writes=[("wgs", s)])
                    TR.dma("sp", lambda e, c=c, s=s: e.dma_start(out=wu[s], in_=wu_b[fi, c]), "wu%d" % s,
                           reads=[("wu", fi, c)], writes=[("wus", s)])
                    for hf in range(2):
                        bg = psbank()
                        bu = psbank()
                        for k in range(NFC):
                            TR.op("pe", lambda e, s=s, hf=hf, k=k, bg=bg, xn=xn: e.matmul(
                                ps[:, bg, :], lhsT=wg[s][:, k, hf * 128:(hf + 1) * 128], rhs=xn[:, k, :],
                                start=(k == 0), stop=(k == NFC - 1)),
                                reads=[("wgs", s), (xk, k)], writes=[("ps", bg)])
                        for k in range(NFC):
                            TR.op("pe", lambda e, s=s, hf=hf, k=k, bu=bu, xn=xn: e.matmul(
                                ps[:, bu, :], lhsT=wu[s][:, k, hf * 128:(hf + 1) * 128], rhs=xn[:, k, :],
                                start=(k == 0), stop=(k == NFC - 1)),
                                reads=[("wus", s), (xk, k)], writes=[("ps", bu)])
                        j = c * 2 + hf
                        sgt = sg[j % 2]
                        TR.op("act", lambda e, sgt=sgt, bg=bg: e.activation(out=sgt, in_=ps[:, bg, :], func=AF.Silu),
                              reads=[("ps", bg)], writes=[("sg", j % 2)])
                        TR.op("dve", lambda e, sgt=sgt, bu=bu, j=j: e.tensor_tensor(out=act[:, j, :], in0=ps[:, bu, :], in1=sgt,
                                                                                     op=ALU.mult),
                              reads=[("ps", bu), ("sg", j % 2)], writes=[("act", j)])
                for c in range(NDC):
                    s = wn[1] % 2
                    wn[1] += 1
                    for k0 in range(0, NKF, 11):
                        TR.dma("sp", lambda e, c=c, s=s, k0=k0: e.dma_start(out=wd[s][:, k0:k0 + 11, :], in_=wd_b[fi, c, :, k0:k0 + 11, :]),
                               "wd%d_%d" % (s, k0 // 11), reads=[("wd", fi, c, k0)], writes=[("wds", s, k0)])
                    if c == 2 and ti + 1 < NT:
                        rms_norm_tile(hsrc, ti + 1, gslot, hT, xns[(ti + 1) % 2], sq, rstd, xkey="xn%d" % ((ti + 1) % 2))
                    for hf in range(2):
                        b = psbank()
                        for k in range(NKF):
                            TR.op("pe", lambda e, s=s, hf=hf, k=k, b=b: e.matmul(
                                ps[:, b, :], lhsT=wd[s][:, k, hf * 128:(hf + 1) * 128], rhs=act[:, k, :],
                                start=(k == 0), stop=(k == NKF - 1)),
                                reads=[("wds", s, (k // 11) * 11), ("act", k)], writes=[("ps", b)])
                        residual_epilogue(hsrc, hdst, ti, c * 2 + hf, b, 0.5, epi, epi_n)

        def cast_even(j):
            wv = ab_w_in[j].rearrange("(kc p) n -> p kc n", p=128)
            for jj in range(8):
                for wh in range(3):
                    c0 = wh * 1024 + jj * 128
                    TR.dma("pool", lambda e, jj=jj, wh=wh, c0=c0: e.dma_start(out=abc_b[j, jj, :, :, wh * 128:(wh + 1) * 128],
                                                                             in_=wv[:, :, c0:c0 + 128]),
                           "cast", writes=[("abc", j, jj, wh)])
            for g in range(4):
                c0 = 3072 + g * 256
                TR.dma("pool", lambda e, g=g, c0=c0: e.dma_start(out=abp_b[j, g], in_=wv[:, :, c0:c0 + 256]),
                       "cast", writes=[("abp", j, g)])
            ov = ab_w_out[j].rearrange("(kc p) n -> p kc n", p=128)
            for c in range(8):
                TR.dma("pool", lambda e, c=c: e.dma_start(out=abo_b[j, c], in_=ov[:, :, c * 256:(c + 1) * 256]),
                       "cast", writes=[("abo", j, c)])

        def phase_mix_even(layer, hsrc, hdst, next_cast=None):
            j = layer // 2
            gslot = layer * 3 + 1
            A.reset()
            hT = A.alloc(NFC * TT, F32).rearrange("p (f t) -> p f t", t=TT)
            xn = A.alloc(NFC * TT, BF16).rearrange("p (f t) -> p f t", t=TT)
            cat = A.alloc(NFC * TT, BF16).rearrange("p (f t) -> p f t", t=TT)
            ucat = A.alloc(8 * 514, F32).rearrange("p (f t) -> p f t", t=514)
            pcat = A.alloc(8 * 527, F32).rearrange("p (f t) -> p f t", t=527)
            wc = [A.alloc(NFC * 384, BF16).rearrange("p (k n) -> p k n", n=384) for _ in range(2)]
            wp = [A.alloc(NFC * 256, BF16).rearrange("p (k n) -> p k n", n=256) for _ in range(2)]
            wo = [A.alloc(NFC * 256, BF16).rearrange("p (k n) -> p k n", n=256) for _ in range(2)]
            pw = A.alloc(8 * 256, BF16).rearrange("p (g n) -> p g n", n=256)
            cw = A.alloc(24, F32).rearrange("p (k j) -> p k j", j=8)
            psc = A.alloc(8, F32)
            invc0 = A.alloc(4 * TT, F32).rearrange("p (g t) -> p g t", t=TT)
            sq = [A.alloc(TT, F32) for _ in range(2)]
            rstd = A.alloc(TT, F32)
            tmp1 = A.alloc(TT, F32)
            yv = A.alloc(TT, F32)
            tA = A.alloc(528, F32)
            tB = A.alloc(528, F32)
            pl = A.alloc(2 * TT, BF16).rearrange("p (i t) -> p i t", t=TT)
            epi = [A.alloc(TT, F32) for _ in range(3)]
            epi_n = [0]
            wn = [0, 0, 0]
            TR.dma("pool", lambda e: e.dma_start(out=pw, in_=ab_pool_w[j].rearrange("g (i p) n -> p (g i) n", p=128)),
                   "oc4", writes=["pw"])
            TR.dma("sp", lambda e: e.dma_start(out=cw, in_=ab_conv_w[j].rearrange("k (j p) -> p k j", p=128),
                                               allow_slow_non_contiguous=True), "oc5", writes=["cw"])
            TR.dma("sp", lambda e: e.dma_start(out=psc, in_=ab_pool_scale[j].rearrange("(m p) -> p m", p=128),
                                               allow_slow_non_contiguous=True), "oc0", writes=["psc"])
            TR.dma("sp", lambda e: e.dma_start(out=invc0.rearrange("p g t -> p (g t)"), in_=invc0_d), "oc1", writes=["invc0"])
            TR.op("dve", lambda e: e.memset(ucat.rearrange("p f t -> p (f t)"), 0.0), writes=[("ucat", m) for m in range(8)])
            TR.op("dve", lambda e: e.memset(pcat.rearrange("p f t -> p (f t)"), 0.0), writes=[("pcat", m) for m in range(8)])
            if next_cast is not None:
                next_cast()
            for ti in range(NT):
                rms_norm_tile(hsrc, ti, gslot, hT, xn, sq, rstd)
                for jj in range(8):
                    s = wn[0] % 2
                    wn[0] += 1
                    TR.dma("sp", lambda e, jj=jj, s=s: e.dma_start(out=wc[s], in_=abc_b[j, jj]), "wc%d" % s,
                           reads=[("abc", j, jj, wh) for wh in range(3)], writes=[("wcs", s)])
                    banks = []
                    for wh in range(3):
                        b = psbank()
                        banks.append(b)
                        for k in range(NFC):
                            TR.op("pe", lambda e, s=s, wh=wh, k=k, b=b: e.matmul(
                                ps[:, b, :], lhsT=wc[s][:, k, wh * 128:(wh + 1) * 128], rhs=xn[:, k, :],
                                start=(k == 0), stop=(k == NFC - 1)),
                                reads=[("wcs", s), ("xn", k)], writes=[("ps", b)])
                    bb, bc, bh = banks
                    uk = ("ucat", jj)
                    TR.op("act", lambda e, bc=bc: e.activation(out=tmp1, in_=ps[:, bc, :], func=AF.Copy),
                          reads=[("ps", bc)], writes=["tmp1"])
                    TR.op("dve", lambda e, jj=jj, bh=bh: e.tensor_tensor(out=ucat[:, jj, 2:514], in0=ps[:, bh, :], in1=tmp1, op=ALU.mult),
                          reads=[("ps", bh), "tmp1"], writes=[uk])
                    TR.op("dve", lambda e, jj=jj: e.tensor_scalar(out=yv, in0=ucat[:, jj, 0:512], scalar1=cw[:, 0, jj:jj + 1], scalar2=None,
                                                                  op0=ALU.mult), reads=[uk, "cw"], writes=["yv"])
                    TR.op("dve", lambda e, jj=jj: e.scalar_tensor_tensor(out=yv, in0=ucat[:, jj, 1:513], scalar=cw[:, 1, jj:jj + 1], in1=yv,
                                                                         op0=ALU.mult, op1=ALU.add), reads=[uk, "cw", "yv"], writes=["yv"])
                    TR.op("dve", lambda e, jj=jj: e.scalar_tensor_tensor(out=yv, in0=ucat[:, jj, 2:514], scalar=cw[:, 2, jj:jj + 1], in1=yv,
                                                                         op0=ALU.mult, op1=ALU.add), reads=[uk, "cw", "yv"], writes=["yv"])
                    TR.op("dve", lambda e, jj=jj, bb=bb: e.tensor_tensor(out=cat[:, jj, :], in0=ps[:, bb, :], in1=yv, op=ALU.mult),
                          reads=[("ps", bb), "yv"], writes=[("cat", jj)])
                    TR.op("act", lambda e, jj=jj: e.activation(out=ucat[:, jj, 0:2], in_=ucat[:, jj, 512:514], func=AF.Copy),
                          reads=[uk], writes=[uk])
                for g in range(4):
                    W = (2, 4, 8, 16)[g]
                    s = wn[1] % 2
                    wn[1] += 1
                    TR.dma("sp", lambda e, g=g, s=s: e.dma_start(out=wp[s], in_=abp_b[j, g]), "wp%d" % s,
                           reads=[("abp", j, g)], writes=[("wps", s)])
                    for ic in range(2):
                        m = 2 * g + ic
                        pk = ("pcat", m)
                        b = psbank()
                        for k in range(NFC):
                            TR.op("pe", lambda e, s=s, ic=ic, k=k, b=b: e.matmul(
                                ps[:, b, :], lhsT=wp[s][:, k, ic * 128:(ic + 1) * 128], rhs=xn[:, k, :],
                                start=(k == 0), stop=(k == NFC - 1)),
                                reads=[("wps", s), ("xn", k)], writes=[("ps", b)])
                        TR.op("act", lambda e, m=m, b=b: e.activation(out=pcat[:, m, 15:527], in_=ps[:, b, :], func=AF.Copy),
                              reads=[("ps", b)], writes=[pk])
                        lo = 15 - (W - 1)
                        ln = 512 + W - 1
                        cur = pcat[:, m, lo:lo + ln]
                        curkey = pk
                        st = 1
                        bufs = [(tA, "tA"), (tB, "tB")]
                        bi = 0
                        while st < W:
                            nb, nk = bufs[bi]
                            bi ^= 1
                            nl = ln - st
                            TR.op("dve", lambda e, cur=cur, nb=nb, st=st, nl=nl: e.tensor_tensor(
                                out=nb[:, 0:nl], in0=cur[:, st:st + nl], in1=cur[:, 0:nl], op=ALU.add),
                                reads=[curkey], writes=[nk])
                            cur, curkey, ln = nb[:, 0:nl], nk, nl
                            st *= 2
                        assert ln == 512
                        if ti == 0:
                            TR.op("dve", lambda e, cur=cur, g=g: e.tensor_tensor(out=yv, in0=cur, in1=invc0[:, g, :], op=ALU.mult),
                                  reads=[curkey, "invc0"], writes=["yv"])
                            TR.op("dve", lambda e, m=m, ic=ic: e.tensor_tensor(out=pl[:, ic, :], in0=yv, in1=pcat[:, m, 15:527], op=ALU.subtract),
                                  reads=["yv", pk], writes=[("pl", ic)])
                        else:
                            TR.op("dve", lambda e, cur=cur, m=m, ic=ic, W=W: e.scalar_tensor_tensor(
                                out=pl[:, ic, :], in0=cur, scalar=1.0 / W, in1=pcat[:, m, 15:527], op0=ALU.mult, op1=ALU.subtract),
                                reads=[curkey, pk], writes=[("pl", ic)])
                        TR.op("act", lambda e, m=m: e.activation(out=pcat[:, m, 0:15], in_=pcat[:, m, 512:527], func=AF.Copy),
                              reads=[pk], writes=[pk])
                    for oc in range(2):
                        b = psbank()
                        for ic in range(2):
                            TR.op("pe", lambda e, g=g, ic=ic, oc=oc, b=b: e.matmul(
                                ps[:, b, :], lhsT=pw[:, 2 * g + ic, oc * 128:(oc + 1) * 128], rhs=pl[:, ic, :],
                                start=(ic == 0), stop=(ic == 1)),
                                reads=["pw", ("pl", ic)], writes=[("ps", b)])
                        mo = 2 * g + oc
                        TR.op("act", lambda e, mo=mo, b=b: e.activation(out=cat[:, 8 + mo, :], in_=ps[:, b, :], func=AF.Copy,
                                                                        scale=psc[:, mo:mo + 1]),
                              reads=[("ps", b), "psc"], writes=[("cat", 8 + mo)])
                for c in range(8):
                    s = wn[2] % 2
                    wn[2] += 1
                    TR.dma("sp", lambda e, c=c, s=s: e.dma_start(out=wo[s], in_=abo_b[j, c]), "wo%d" % s,
                           reads=[("abo", j, c)], writes=[("wos", s)])
                    for hf in range(2):
                        b = psbank()
                        for k in range(NFC):
                            TR.op("pe", lambda e, s=s, hf=hf, k=k, b=b: e.matmul(
                                ps[:, b, :], lhsT=wo[s][:, k, hf * 128:(hf + 1) * 128], rhs=cat[:, k, :],
                                start=(k == 0), stop=(k == NFC - 1)),
                                reads=[("wos", s), ("cat", k)], writes=[("ps", b)])
                        residual_epilogue(hsrc, hdst, ti, c * 2 + hf, b, 1.0, epi, epi_n)

        NQB = T // 128
        LN_EPS = 1e-5
        ASCALE = 128 ** -0.5
        GAM = [1.0 - 2.0 ** (-5.0 - h) for h in range(8)]

        def cast_odd(j):
            wv = cd_w_in[j].rearrange("(kc p) n -> p kc n", p=128)
            fcols = [0, 256, 512, 768, 1024, 1280, 1536, 1792, 3072, 3328, 3584, 3840, 6224, 6480, 6736, 6992]
            for g, c0 in enumerate(fcols):
                TR.dma("pool", lambda e, g=g, c0=c0: e.dma_start(out=cdF_b[j, g], in_=wv[:, :, c0:c0 + 256]),
                       "cast", writes=[("cdF", j, g)])
            for hh in range(2):
                TR.dma("pool", lambda e, hh=hh: e.dma_start(out=cdI_b[j, :, :, hh * 64:(hh + 1) * 64], in_=wv[:, :, 4096:4160]),
                       "cast", writes=[("cdI", j, hh)])
            for qk in range(2):
                base = 4176 + qk * 512
                for c in range(4):
                    TR.dma("pool", lambda e, qk=qk, c=c, base=base: e.dma_start(out=cdR_b[j, qk * 8 + c], in_=wv[:, :, base + c * 128:base + (c + 1) * 128]),
                           "cast", writes=[("cdR", j, qk * 8 + c, 0)])
                    for hh in range(2):
                        for half in range(2):
                            d0 = hh * 64 + half * 32
                            s0 = base + c * 128 + hh * 64 + (1 - half) * 32
                            TR.dma("pool", lambda e, qk=qk, c=c, d0=d0, s0=s0: e.dma_start(
                                out=cdR_b[j, qk * 8 + 4 + c, :, :, d0:d0 + 32], in_=wv[:, :, s0:s0 + 32]),
                                "cast", writes=[("cdR", j, qk * 8 + 4 + c, hh * 2 + half)])
            for g, c0 in enumerate([2048, 2560, 5200, 5712]):
                TR.dma("pool", lambda e, g=g, c0=c0: e.dma_start(out=cdT_b[j, g], in_=wv[:, :, c0:c0 + 512]),
                       "cast", writes=[("cdT", j, g)])
            TR.dma("pool", lambda e: e.dma_start(out=cdW_b[j], in_=wv[:, :, 4160:4176]), "cast", writes=[("cdW", j)])
            ov = cd_w_out[j].rearrange("(kc p) n -> p kc n", p=128)
            for c in range(8):
                TR.dma("pool", lambda e, c=c: e.dma_start(out=cdo_b[j, c], in_=ov[:, :, c * 256:(c + 1) * 256]),
                       "cast", writes=[("cdo", j, c)])

        def odd_stage_a(layer, hsrc):
            j = layer // 2
            gslot = layer * 3 + 1
            A.reset()
            hT = A.alloc(NFC * TT, F32).rearrange("p (f t) -> p f t", t=TT)
            xns = [A.alloc(NFC * TT, BF16).rearrange("p (f t) -> p f t", t=TT) for _ in range(2)]
            cur = {}
            sq = [A.alloc(TT, F32) for _ in range(2)]
            rstd = A.alloc(TT, F32)
            wF = [A.alloc(NFC * 256, BF16).rearrange("p (k n) -> p k n", n=256) for _ in range(2)]
            wR = [A.alloc(NFC * 256, BF16).rearrange("p (a k n) -> p a k n", a=2, n=128) for _ in range(2)]
            wT = [A.alloc(NFC * 512, BF16).rearrange("p (k n) -> p k n", n=512) for _ in range(2)]
            wI = A.alloc(NFC * 128, BF16).rearrange("p (k n) -> p k n", n=128)
            wW = A.alloc(NFC * 16, BF16).rearrange("p (k n) -> p k n", n=16)
            stg = [A.alloc(TT, BF16) for _ in range(4)]
            stf = [A.alloc(TT, F32) for _ in range(3)]
            rtab = [A.alloc(2 * TT, F32).rearrange("p (a t) -> p a t", t=TT) for _ in range(2)]
            t1 = A.alloc(TT, F32)
            t2 = A.alloc(TT, F32)
            iwt = A.alloc(16, F32)
            cnt = {"F": 0, "R": 0, "T": 0, "stg": 0, "stf": 0, "rt": 0}
            TR.dma("sp", lambda e: e.dma_start(out=wI, in_=cdI_b[j]), "oc1", reads=[("cdI", j, 0), ("cdI", j, 1)], writes=["wI"])
            TR.dma("sp", lambda e: e.dma_start(out=wW, in_=cdW_b[j]), "oc2", reads=[("cdW", j)], writes=["wW"])

            def fm_group(ti, lhs_fn, wkeys, evac):
                b = psbank()
                xn, xk = cur["xn"], cur["xk"]
                for k in range(NFC):
                    TR.op("pe", lambda e, k=k, b=b, xn=xn: e.matmul(ps[:, b, :], lhsT=lhs_fn(k), rhs=xn[:, k, :],
                                                                    start=(k == 0), stop=(k == NFC - 1)),
                          reads=list(wkeys) + [(xk, k)], writes=[("ps", b)])
                evac(b)

            def store_bf(ti, b, dst_rows, func=AF.Copy, use_act=True):
                sl = cnt["stg"] % 4
                cnt["stg"] += 1
                sb = stg[sl]
                if use_act:
                    TR.op("act", lambda e: e.activation(out=sb, in_=ps[:, b, :], func=func), reads=[("ps", b)], writes=[("stg", sl)])
                else:
                    TR.op("dve", lambda e: e.tensor_copy(out=sb, in_=ps[:, b, :]), reads=[("ps", b)], writes=[("stg", sl)])
                TR.dma("act", lambda e: e.dma_start(out=dst_rows[:, ti * TT:(ti + 1) * TT], in_=sb), "stg%d" % sl,
                       reads=[("stg", sl)], writes=[("scr", id(dst_rows), ti)])

            rms_norm_tile(hsrc, 0, gslot, hT, xns[0], sq, rstd, xkey="xn0")
            for ti in range(NT):
                xn = xns[ti % 2]
                xk = "xn%d" % (ti % 2)
                cur["xn"], cur["xk"] = xn, xk
                for g in range(16):
                    s_ = cnt["F"] % 2
                    cnt["F"] += 1
                    TR.dma("sp", lambda e, g=g, s_=s_: e.dma_start(out=wF[s_], in_=cdF_b[j, g]), "wF%d" % s_,
                           reads=[("cdF", j, g)], writes=[("wFs", s_)])
                    for hf in range(2):
                        ch = (g % 4) * 2 + hf
                        kind = g // 4
                        if kind < 3:
                            dst = (qT_d, kT_d, iqT_d)[kind][ch * 128:(ch + 1) * 128, :]
                            fm_group(ti, lambda k, s_=s_, hf=hf: wF[s_][:, k, hf * 128:(hf + 1) * 128], [("wFs", s_)],
                                     lambda b, dst=dst, ch=ch: store_bf(ti, b, dst, use_act=(ch % 2 == 0)))
                        else:
                            def ev(b, ch=ch, ti=ti):
                                sl = cnt["stf"] % 3
                                cnt["stf"] += 1
                                sb = stf[sl]
                                TR.op("act", lambda e: e.activation(out=sb, in_=ps[:, b, :], func=AF.Silu), reads=[("ps", b)], writes=[("stf", sl)])
                                TR.dma("act", lambda e: e.dma_start(out=sg_d[ch * 128:(ch + 1) * 128, ti * TT:(ti + 1) * TT], in_=sb),
                                       "stf%d" % sl, reads=[("stf", sl)], writes=[("sg_d", ch, ti)])
                            fm_group(ti, lambda k, s_=s_, hf=hf: wF[s_][:, k, hf * 128:(hf + 1) * 128], [("wFs", s_)], ev)
                fm_group(ti, lambda k: wI[:, k, :], ["wI"], lambda b: store_bf(ti, b, ikT_d))
                if ti + 1 < NT:
                    rms_norm_tile(hsrc, ti + 1, gslot, hT, xns[(ti + 1) % 2], sq, rstd, xkey="xn%d" % ((ti + 1) % 2))
                for qk in range(2):
                    for c in range(4):
                        s_ = cnt["R"] % 2
                        cnt["R"] += 1
                        TR.dma("sp", lambda e, qk=qk, c=c, s_=s_: e.dma_start(out=wR[s_][:, 0], in_=cdR_b[j, qk * 8 + c]), "wR%d_0" % s_,
                               reads=[("cdR", j, qk * 8 + c, 0)], writes=[("wRs", s_, 0)])
                        TR.dma("sp", lambda e, qk=qk, c=c, s_=s_: e.dma_start(out=wR[s_][:, 1], in_=cdR_b[j, qk * 8 + 4 + c]), "wR%d_1" % s_,
                               reads=[("cdR", j, qk * 8 + 4 + c, q) for q in range(4)], writes=[("wRs", s_, 1)])
                        r_ = cnt["rt"] % 2
                        cnt["rt"] += 1
                        TR.dma("sp", lambda e, qk=qk, c=c, r_=r_, ti=ti: e.dma_start(out=rtab[r_], in_=rope_d[qk, c].rearrange("a p t -> p a t")[:, :, ti * TT:(ti + 1) * TT]),
                               "rt%d" % r_, writes=[("rtab", r_)])
                        bA = psbank()
                        bB = psbank()
                        for ab, b in ((0, bA), (1, bB)):
                            for k in range(NFC):
                                TR.op("pe", lambda e, k=k, b=b, ab=ab, s_=s_, xn=xn: e.matmul(ps[:, b, :], lhsT=wR[s_][:, ab, k, :], rhs=xn[:, k, :],
                                                                                        start=(k == 0), stop=(k == NFC - 1)),
                                      reads=[("wRs", s_, ab), (xk, k)], writes=[("ps", b)])
                        TR.op("dve", lambda e, bA=bA, r_=r_: e.tensor_tensor(out=t1, in0=ps[:, bA, :], in1=rtab[r_][:, 0, :], op=ALU.mult),
                              reads=[("ps", bA), ("rtab", r_)], writes=["t1"])
                        TR.op("dve", lambda e, bB=bB, r_=r_: e.tensor_tensor(out=t2, in0=ps[:, bB, :], in1=rtab[r_][:, 1, :], op=ALU.mult),
                              reads=[("ps", bB), ("rtab", r_)], writes=["t2"])
                        sl = cnt["stg"] % 4
                        cnt["stg"] += 1
                        sb = stg[sl]
                        TR.op("dve", lambda e, sb=sb: e.tensor_tensor(out=sb, in0=t1, in1=t2, op=ALU.add), reads=["t1", "t2"], writes=[("stg", sl)])
                        dst = (rqT_d, rkT_d)[qk]
                        TR.dma("act", lambda e, sb=sb, dst=dst, c=c, ti=ti: e.dma_start(out=dst[c * 128:(c + 1) * 128, ti * TT:(ti + 1) * TT], in_=sb),
                               "stg%d" % sl, reads=[("stg", sl)], writes=[("rqk", qk, c, ti)])
                for g in range(4):
                    s_ = cnt["T"] % 2
                    cnt["T"] += 1
                    TR.dma("sp", lambda e, g=g, s_=s_: e.dma_start(out=wT[s_], in_=cdT_b[j, g]), "wT%d" % s_,
                           reads=[("cdT", j, g)], writes=[("wTs", s_)])
                    dst = (vc_d, vr_d)[g // 2]
                    for tb in range(4):
                        b = psbank()
                        for k in range(NFC):
                            TR.op("pe", lambda e, k=k, b=b, tb=tb, s_=s_, xn=xn: e.matmul(ps[:, b, :], lhsT=xn[:, k, tb * 128:(tb + 1) * 128], rhs=wT[s_][:, k, :],
                                                                                    start=(k == 0), stop=(k == NFC - 1)),
                                  reads=[("wTs", s_), (xk, k)], writes=[("ps", b)])
                        sl = cnt["stg"] % 4
                        cnt["stg"] += 1
                        sb = stg[sl]
                        if tb % 2 == 0:
                            TR.op("act", lambda e, sb=sb, b=b: e.activation(out=sb, in_=ps[:, b, :], func=AF.Copy), reads=[("ps", b)], writes=[("stg", sl)])
                        else:
                            TR.op("dve", lambda e, sb=sb, b=b: e.tensor_copy(out=sb, in_=ps[:, b, :]), reads=[("ps", b)], writes=[("stg", sl)])
                        r0 = ti * TT + tb * 128
                        c0 = (g % 2) * 512
                        TR.dma("act", lambda e, sb=sb, dst=dst, r0=r0, c0=c0: e.dma_start(out=dst[r0:r0 + 128, c0:c0 + 512], in_=sb),
                               "stg%d" % sl, reads=[("stg", sl)], writes=[("v_d", g, r0)])
                for tb in range(4):
                    b = psbank()
                    for k in range(NFC):
                        TR.op("pe", lambda e, k=k, b=b, tb=tb, xn=xn: e.matmul(ps[:, b, 0:16], lhsT=xn[:, k, tb * 128:(tb + 1) * 128], rhs=wW[:, k, :],
                                                                         start=(k == 0), stop=(k == NFC - 1)),
                              reads=["wW", (xk, k)], writes=[("ps", b)])
                    TR.op("act", lambda e, b=b: e.activation(out=iwt, in_=ps[:, b, 0:16], func=AF.Copy, scale=0.25 * 0.125),
                          reads=[("ps", b)], writes=["iwt"])
                    r0 = ti * TT + tb * 128
                    TR.dma("act", lambda e, r0=r0: e.dma_start(out=iw_d[r0:r0 + 128, :], in_=iwt), "iwt", reads=["iwt"], writes=[("iw_d", r0)])

        def odd_stage_b(layer):
            j = layer // 2
            qh = [A.alloc(T, BF16) for _ in range(2)]
            kh = [A.alloc(T, BF16) for _ in range(2)]
            vh = [A.alloc(NQB * 128, BF16).rearrange("p (j e) -> p j e", e=128) for _ in range(2)]
            dd = A.alloc(8 * 128, F32).rearrange("p (h i) -> p h i", i=128)
            rgain = A.alloc(8, F32)
            NAT = 6
            at = [A.alloc(128, BF16) for _ in range(NAT)]
            dtmp = [A.alloc(128, F32) for _ in range(2)]
            oT = [A.alloc(TT, F32) for _ in range(2)]
            cen = A.alloc(TT, F32)
            sqv = A.alloc(TT, F32)
            rs = A.alloc(TT, F32)
            sgt = [A.alloc(TT, F32) for _ in range(2)]
            yb = [A.alloc(TT, BF16) for _ in range(2)]
            for s0_ in range(2):
                TR.op("pool", lambda e, s0_=s0_: e.memset(qh[s0_], 0.0), writes=[("qh", s0_)])
                TR.op("pool", lambda e, s0_=s0_: e.memset(kh[s0_], 0.0), writes=[("kh", s0_)])
            TR.dma("sp", lambda e: e.dma_start(out=dd.rearrange("p h i -> p (h i)"), in_=dd_d), "oc3", writes=["dd"])
            TR.dma("sp", lambda e: e.dma_start(out=rgain, in_=ret_norm_gain[j].rearrange("(h p) -> p h", p=128), allow_slow_non_contiguous=True),
                   "oc4", writes=["rgain"])
            n_at = 0
            n_o = 0
            n_dt = 0
            npo = [0]
            for h in range(8):
                s_ = h % 2
                TR.dma("sp", lambda e, h=h, s_=s_: e.dma_start(out=qh[s_][0:64, :], in_=rqT_d[h * 64:(h + 1) * 64, :]), "rq%d" % s_, writes=[("qh", s_)])
                TR.dma("sp", lambda e, h=h, s_=s_: e.dma_start(out=kh[s_][0:64, :], in_=rkT_d[h * 64:(h + 1) * 64, :]), "rk%d" % s_, writes=[("kh", s_)])
                for q4 in range(4):
                    TR.dma("sp", lambda e, h=h, s_=s_, q4=q4: e.dma_start(out=vh[s_][:, q4 * 8:(q4 + 1) * 8, :],
                                                                        in_=vr_d.rearrange("(j p) c -> p j c", p=128)[:, q4 * 8:(q4 + 1) * 8, h * 128:(h + 1) * 128]),
                           "vv%d_%d" % (s_, q4), writes=[("vh", s_, q4)])
                for tg in range(NT):
                    os_ = n_o % 2
                    n_o += 1
                    for ib in range(4):
                        I = tg * 4 + ib
                        po = 6 + npo[0] % 2
                        npo[0] += 1
                        pend = None
                        for J in range(I + 1):
                            b = psbank()
                            TR.op("pe", lambda e, b=b, J=J, I=I, s_=s_: e.matmul(
                                ps[:, b, 0:128], lhsT=kh[s_][:, J * 128:(J + 1) * 128], rhs=qh[s_][:, I * 128:(I + 1) * 128],
                                start=True, stop=True), reads=[("kh", s_), ("qh", s_)], writes=[("ps", b)])
                            a_ = n_at % NAT
                            n_at += 1
                            ab = at[a_]
                            if J < I:
                                sc = float(GAM[h] ** (128 * (I - J)))
                                TR.op("act", lambda e, ab=ab, b=b, sc=sc: e.activation(out=ab, in_=ps[:, b, 0:128], func=AF.Copy, scale=sc),
                                      reads=[("ps", b)], writes=[("at", a_)])
                            else:
                                d_ = n_dt % 2
                                n_dt += 1
                                dtm = dtmp[d_]
                                TR.op("act", lambda e, dtm=dtm, b=b: e.activation(out=dtm, in_=ps[:, b, 0:128], func=AF.Copy),
                                      reads=[("ps", b)], writes=[("dtmp", d_)])
                                TR.op("pool", lambda e, ab=ab, dtm=dtm, h=h: e.tensor_tensor(out=ab, in0=dtm, in1=dd[:, h, :], op=ALU.mult),
                                      reads=[("dtmp", d_), "dd"], writes=[("at", a_)])
                            if pend is not None:
                                pend()
                            pend = (lambda ab=ab, a_=a_, J=J, I=I, po=po, s_=s_: TR.op(
                                "pe", lambda e: e.matmul(ps[:, po, 0:128], lhsT=vh[s_][:, J, :], rhs=ab, start=(J == 0), stop=(J == I)),
                                reads=[("vh", s_, J // 8), ("at", a_)], writes=[("ps", po)]))
                        pend()
                        TR.op("act", lambda e, po=po, ib=ib, os_=os_: e.activation(out=oT[os_][:, ib * 128:(ib + 1) * 128], in_=ps[:, po, 0:128], func=AF.Copy),
                              reads=[("ps", po)], writes=[("oT", os_)])
                        yield I + 1
                    o_ = oT[os_]
                    b1 = psbank()
                    TR.op("pe", lambda e, b1=b1, o_=o_: e.matmul(ps[:, b1, :], lhsT=ones, rhs=o_, start=True, stop=True),
                          reads=[("oT", os_), "ones"], writes=[("ps", b1)])
                    TR.op("dve", lambda e, b1=b1, o_=o_: e.scalar_tensor_tensor(out=cen, in0=ps[:, b1, :], scalar=-1.0 / 128, in1=o_, op0=ALU.mult, op1=ALU.add),
                          reads=[("ps", b1), ("oT", os_)], writes=["cen"])
                    TR.op("act", lambda e: e.activation(out=sqv, in_=cen, func=AF.Square), reads=["cen"], writes=["sqv"])
                    b2 = psbank()
                    TR.op("pe", lambda e, b2=b2: e.matmul(ps[:, b2, :], lhsT=ones, rhs=sqv, start=True, stop=True),
                          reads=["sqv", "ones"], writes=[("ps", b2)])
                    TR.op("dve", lambda e, b2=b2: e.tensor_scalar(out=rs, in0=ps[:, b2, :], scalar1=1.0 / 128, scalar2=LN_EPS, op0=ALU.mult, op1=ALU.add),
                          reads=[("ps", b2)], writes=["rs"])
                    TR.op("act", lambda e: e.activation(out=rs, in_=rs, func=AF.Sqrt), reads=["rs"], writes=["rs"])
                    TR.op("dve", lambda e: e.reciprocal(out=rs, in_=rs), reads=["rs"], writes=["rs"])
                    TR.op("pool", lambda e: e.tensor_tensor(out=cen, in0=cen, in1=rs, op=ALU.mult), reads=["cen", "rs"], writes=["cen"])
                    g_ = sgt[os_]
                    TR.dma("sp", lambda e, g_=g_, h=h, tg=tg: e.dma_start(out=g_, in_=sg_d[h * 128:(h + 1) * 128, tg * TT:(tg + 1) * TT]), "sgt%d" % os_,
                           writes=[("sgt", os_)])
                    y_ = yb[os_]
                    TR.op("dve", lambda e, g_=g_, y_=y_, h=h: e.scalar_tensor_tensor(out=y_, in0=cen, scalar=rgain[:, h:h + 1], in1=g_, op0=ALU.mult, op1=ALU.mult),
                          reads=["cen", "rgain", ("sgt", os_)], writes=[("yb", os_)])
                    TR.dma("sp", lambda e, y_=y_, h=h, tg=tg: e.dma_start(out=catT_d[1024 + h * 128:1024 + (h + 1) * 128, tg * TT:(tg + 1) * TT], in_=y_),
                           "yb%d" % os_, reads=[("yb", os_)], writes=[("catT", 8 + h, tg)])
                    yield 1

        def odd_stage_c1(layer):
            ikE = A.alloc(T, BF16)
            ikO = A.alloc(T, BF16)
            iq = [A.alloc(8 * 128, BF16).rearrange("p (c t) -> p c t", t=128) for _ in range(2)]
            iw = [A.alloc(16, F32) for _ in range(2)]
            wabs = [A.alloc(16, F32) for _ in range(2)]
            wsg = [A.alloc(16, F32) for _ in range(2)]
            dg = [A.alloc(16 * 128, BF16).rearrange("p (h t) -> p h t", t=128) for _ in range(2)]
            acc = A.alloc(T, F32)
            work = A.alloc(T, F32)
            rl = [A.alloc(512, BF16) for _ in range(4)]
            m8 = A.alloc(8, F32)
            thrc = A.alloc(1, F32)
            nm = A.alloc(T, BF16)
            nmT = [A.alloc(NQB * 128, BF16).rearrange("p (j t) -> p j t", t=128) for _ in range(2)]
            identb = A.alloc(128, BF16)
            TR.op("pool", lambda e: e.tensor_copy(out=identb, in_=ident), reads=["ident"], writes=["identb1"])
            TR.op("pool", lambda e: e.memset(thrc, -1e29), writes=["thrc"])
            TR.op("pool", lambda e: e.memset(ikE, 0.0), writes=["ik"])
            TR.op("pool", lambda e: e.memset(ikO, 0.0), writes=["ik"])
            TR.dma("sp", lambda e: e.dma_start(out=ikE[0:64, :], in_=ikT_d[0:64, :]), "oc5", writes=["ik"])
            TR.dma("sp", lambda e: e.dma_start(out=ikO[64:128, :], in_=ikT_d[64:128, :]), "oc0", writes=["ik"])
            nrl = 0
            nacc = 0
            for I in range(NQB):
                S = 128 * (I + 1)
                s_ = I % 2
                TR.dma("sp", lambda e, I=I, s_=s_: e.dma_start(out=iq[s_], in_=iqT_d.rearrange("(c p) t -> p c t", p=128)[:, :, I * 128:(I + 1) * 128]),
                       "iq%d" % s_, writes=[("iq", s_)])
                TR.dma("sp", lambda e, I=I, s_=s_: e.dma_start(out=iw[s_], in_=iw_d[I * 128:(I + 1) * 128, :]), "iw%d" % s_, writes=[("iw", s_)])
                TR.op("dve", lambda e, s_=s_: e.tensor_scalar(out=wsg[s_], in0=iw[s_], scalar1=0.0, scalar2=2.0, op0=ALU.is_ge, op1=ALU.mult),
                      reads=[("iw", s_)], writes=[("wsg", s_)])
                TR.op("dve", lambda e, s_=s_: e.tensor_scalar(out=wsg[s_], in0=wsg[s_], scalar1=-1.0, scalar2=None, op0=ALU.add),
                      reads=[("wsg", s_)], writes=[("wsg", s_)])
                TR.op("dve", lambda e, s_=s_: e.tensor_tensor(out=wabs[s_], in0=iw[s_], in1=wsg[s_], op=ALU.mult), reads=[("iw", s_), ("wsg", s_)], writes=[("wabs", s_)])
                for hh in range(16):
                    TR.op("dve", lambda e, s_=s_, hh=hh: e.tensor_scalar(out=dg[s_][:, hh, :], in0=ident, scalar1=wsg[s_][:, hh:hh + 1], scalar2=None, op0=ALU.mult),
                          reads=["ident", ("wsg", s_)], writes=[("dg", s_)])
                for c0 in range(0, S, 512):
                    n = min(512, S - c0)
                    ck = ("acc", c0 // 512)
                    ba = 4 + nacc % 2
                    nacc += 1
                    pend = None
                    for hh in range(16):
                        b = psbank()
                        TR.op("pe", lambda e, b=b, hh=hh, c0=c0, n=n, s_=s_: e.matmul(
                            ps[:, b, 0:n], lhsT=iq[s_][:, hh // 2, :], rhs=(ikE if hh % 2 == 0 else ikO)[:, c0:c0 + n], start=True, stop=True),
                            reads=[("iq", s_), "ik"], writes=[("ps", b)])
                        r_ = nrl % 4
                        nrl += 1
                        rb = rl[r_]
                        TR.op("act", lambda e, rb=rb, b=b, n=n, s_=s_, hh=hh: e.activation(out=rb[:, 0:n], in_=ps[:, b, 0:n], func=AF.Relu, scale=wabs[s_][:, hh:hh + 1]),
                              reads=[("ps", b), ("wabs", s_)], writes=[("rl", r_)])
                        if pend is not None:
                            pend()
                        pend = (lambda rb=rb, r_=r_, hh=hh, n=n, ba=ba, s_=s_: TR.op(
                            "pe", lambda e: e.matmul(ps[:, ba, 0:n], lhsT=dg[s_][:, hh, :], rhs=rb[:, 0:n], start=(hh == 0), stop=(hh == 15)),
                            reads=[("dg", s_), ("rl", r_)], writes=[("ps", ba)]))
                    pend()
                    TR.op("act", lambda e, ba=ba, c0=c0, n=n: e.activation(out=acc[:, c0:c0 + n], in_=ps[:, ba, 0:n], func=AF.Copy),
                          reads=[("ps", ba)], writes=[ck])
                    yield 2
                acck = [("acc", c) for c in range((S + 511) // 512)]
                TR.op("pool", lambda e, S=S: e.memset(acc[0:64, S - 64:S], -1e30), reads=acck, writes=acck)
                if I >= 2:
                    for r in range(32):
                        src_ = acc if r == 0 else work
                        TR.op("dve", lambda e, src_=src_, S=S: e.max(out=m8, in_=src_[:, 0:S]), reads=acck + ["work"], writes=["m8"])
                        if r < 31:
                            TR.op("dve", lambda e, src_=src_, S=S: e.match_replace(out=work[:, 0:S], in_to_replace=m8, in_values=src_[:, 0:S], imm_value=-1e30),
                                  reads=acck + ["m8", "work"], writes=["work"])
                        if r % 2 == 1:
                            yield (I + 1) * 0.5
                    thr = m8[:, 7:8]
                    thrk = "m8"
                else:
                    thr = thrc[:, 0:1]
                    thrk = "thrc"
                TR.op("dve", lambda e, S=S, thr=thr: e.tensor_scalar(out=nm[:, 0:S], in0=acc[:, 0:S], scalar1=thr, scalar2=-30000.0, op0=ALU.is_lt, op1=ALU.mult),
                      reads=acck + [thrk], writes=["nm"])
                ns = I % 2
                for J0 in range(0, I + 1, 8):
                    js = list(range(J0, min(J0 + 8, I + 1)))
                    b = psbank()
                    pbf = ps[:, b, :].bitcast(BF16)
                    for jj, J in enumerate(js):
                        TR.op("pe", lambda e, pbf=pbf, jj=jj, J=J: e.transpose(out=pbf[:, jj * 128:(jj + 1) * 128], in_=nm[:, J * 128:(J + 1) * 128], identity=identb),
                              reads=["nm", "identb1"], writes=[("ps", b)])
                    nj = len(js)
                    TR.op("act", lambda e, pbf=pbf, J0=J0, nj=nj, ns=ns: e.activation(out=nmT[ns][:, J0:J0 + nj, :].rearrange("p j t -> p (j t)"), in_=pbf[:, 0:nj * 128], func=AF.Copy),
                          reads=[("ps", b)], writes=[("nmT", ns)])
                TR.dma("sp", lambda e, I=I, ns=ns: e.dma_start(out=nmT_d[I, :, 0:(I + 1) * 128], in_=nmT[ns][:, 0:I + 1, :].rearrange("p j t -> p (j t)")),
                       "nmT%d" % ns, reads=[("nmT", ns)], writes=[("nmT_d", I)])
                yield 1

        def odd_stage_c2(layer):
            A.reset()
            qh = [A.alloc(T, BF16) for _ in range(2)]
            kh = [A.alloc(T, BF16) for _ in range(2)]
            vh = [A.alloc(NQB * 130, BF16).rearrange("p (j e) -> p j e", e=130) for _ in range(2)]
            nmb = [A.alloc(NQB * 128, BF16).rearrange("p (j t) -> p j t", t=128) for _ in range(2)]
            bg = A.alloc(8 * 256, F32).rearrange("p (h k t) -> p h k t", h=8, k=2)
            t15 = A.alloc(8, F32)
            tab = A.alloc(256, F32)
            mb = A.alloc(8, F32)
            rc = A.alloc(8, F32)
            identb = A.alloc(128, BF16)
            mx = [A.alloc(8, F32) for _ in range(2)]
            mcol = [A.alloc(1, F32) for _ in range(2)]
            rcol = [A.alloc(1, F32) for _ in range(2)]
            rdiag = [A.alloc(128, BF16) for _ in range(2)]
            onesb = A.alloc(128, BF16)
            pt = [A.alloc(512, BF16) for _ in range(4)]
            rcp = A.alloc(1, F32)
            yo = A.alloc(128, BF16)
            yT = [A.alloc(TT, BF16) for _ in range(2)]
            TR.op("dve", lambda e: e.tensor_copy(out=identb, in_=ident), reads=["ident"], writes=["identb"])
            TR.op("dve", lambda e: e.memset(onesb, 1.0), writes=["onesb"])
            for s_ in range(2):
                TR.op("dve", lambda e, s_=s_: e.memset(vh[s_].rearrange("p j e -> p (j e)"), 1.0), writes=[("vh", s_, q4) for q4 in range(4)])
            TR.dma("sp", lambda e: e.dma_start(out=bg.rearrange("p h k t -> p (h k t)"), in_=biasg_d), "oc1", writes=["bg"])
            TR.dma("sp", lambda e: e.dma_start(out=t15, in_=t15_d), "oc2", writes=["t15"])
            TR.dma("sp", lambda e: e.dma_start(out=tab, in_=tab_d), "oc3", writes=["tab"])
            for h in range(8):
                TR.op("dve", lambda e, h=h: e.tensor_scalar(out=bg[:, h].rearrange("p k t -> p (k t)"), in0=bg[:, h].rearrange("p k t -> p (k t)"),
                                                            scalar1=t15[:, h:h + 1], scalar2=1.0 / ASCALE, op0=ALU.subtract, op1=ALU.mult),
                      reads=["bg", "t15"], writes=["bg"])
            TR.op("dve", lambda e: e.tensor_reduce(out=mb, in_=tab.rearrange("p (b h) -> p h b", h=8), axis=AX.X, op=ALU.max),
                  reads=["tab"], writes=["mb"])
            TR.op("dve", lambda e: e.tensor_tensor(out=rc, in0=tab[:, 120:128], in1=mb, op=ALU.subtract), reads=["tab", "mb"], writes=["rc"])
            TR.op("dve", lambda e: e.tensor_scalar(out=rc, in0=rc, scalar1=1.0 / ASCALE, scalar2=None, op0=ALU.mult), reads=["rc"], writes=["rc"])
            C.rot = [0, 1, 2, 3, 4, 5]
            st = {"npt": 0, "npo": 0}

            def stage1(n, h, I):
                s_ = h % 2
                S = 128 * (I + 1)
                ms = n % 2
                if I == 0:
                    TR.dma("sp", lambda e: e.dma_start(out=qh[s_], in_=qT_d[h * 128:(h + 1) * 128, :]), "cq%d" % s_, writes=[("qh", s_)])
                    TR.dma("sp", lambda e: e.dma_start(out=kh[s_], in_=kT_d[h * 128:(h + 1) * 128, :]), "ck%d" % s_, writes=[("kh", s_)])
                    for q4 in range(4):
                        TR.dma("sp", lambda e, q4=q4: e.dma_start(out=vh[s_][:, q4 * 8:(q4 + 1) * 8, 0:128],
                                                                    in_=vc_d.rearrange("(j p) c -> p j c", p=128)[:, q4 * 8:(q4 + 1) * 8, h * 128:(h + 1) * 128]),
                               "vv%d_%d" % (s_, q4), writes=[("vh", s_, q4)])
                TR.dma("sp", lambda e: e.dma_start(out=nmb[ms][:, 0:I + 1, :].rearrange("p j t -> p (j t)"), in_=nmT_d[I, :, 0:(I + 1) * 128]),
                       "nmb%d" % ms, reads=[("nmT_d", I)], writes=[("nmb", ms)])
                nch = (S + 511) // 512
                for c in range(nch):
                    nn = min(512, S - c * 512)
                    b = psbank()
                    TR.op("pe", lambda e, b=b, c=c, nn=nn: e.matmul(ps[:, b, 0:nn], lhsT=qh[s_][:, I * 128:(I + 1) * 128], rhs=kh[s_][:, c * 512:c * 512 + nn],
                                                                     start=True, stop=True), reads=[("qh", s_), ("kh", s_)], writes=[("ps", b)])
                    TR.op("dve", lambda e, b=b, c=c, nn=nn: e.tensor_reduce(out=mx[ms][:, c:c + 1], in_=ps[:, b, 0:nn], axis=AX.X, op=ALU.max),
                          reads=[("ps", b)], writes=[("mx", ms)])
                TR.op("dve", lambda e: e.tensor_reduce(out=mcol[ms], in_=mx[ms][:, 0:nch], axis=AX.X, op=ALU.max), reads=[("mx", ms)], writes=[("mcol", ms)])
                TR.op("dve", lambda e: e.tensor_scalar(out=rcol[ms], in0=mcol[ms], scalar1=-1.0, scalar2=rc[:, h:h + 1], op0=ALU.mult, op1=ALU.add),
                      reads=[("mcol", ms), "rc"], writes=[("rcol", ms)])
                TR.op("dve", lambda e: e.tensor_scalar(out=rdiag[ms], in0=ident, scalar1=rcol[ms][:, 0:1], scalar2=None, op0=ALU.mult),
                      reads=["ident", ("rcol", ms)], writes=[("rdiag", ms)])

            def stage2(n, h, I):
                s_ = h % 2
                ms = n % 2
                rdg = rdiag[ms]
                po = 6 + st["npo"] % 2
                st["npo"] += 1
                pend = []
                for J0 in range(0, I + 1, 4):
                    js = list(range(J0, min(J0 + 4, I + 1)))
                    b = psbank()
                    for jj, J in enumerate(js):
                        o_ = ps[:, b, jj * 128:(jj + 1) * 128]
                        near = J >= I - 1
                        TR.op("pe", lambda e, o_=o_, J=J: e.matmul(o_, lhsT=kh[s_][:, J * 128:(J + 1) * 128], rhs=qh[s_][:, I * 128:(I + 1) * 128], start=True, stop=False),
                              reads=[("kh", s_), ("qh", s_)], writes=[("ps", b)])
                        TR.op("pe", lambda e, o_=o_: e.matmul(o_, lhsT=onesb, rhs=rdg, start=False, stop=False),
                              reads=["onesb", ("rdiag", ms)], writes=[("ps", b)])
                        TR.op("pe", lambda e, o_=o_, J=J, near=near: e.matmul(o_, lhsT=identb, rhs=nmb[ms][:, J, :], start=False, stop=(not near)),
                              reads=["identb", ("nmb", ms)], writes=[("ps", b)])
                        if near:
                            kb = J - (I - 1)
                            TR.op("pe", lambda e, o_=o_, kb=kb: e.matmul(o_, lhsT=ident, rhs=bg[:, h, kb, :], start=False, stop=True),
                                  reads=["ident", "bg"], writes=[("ps", b)])
                    p_ = st["npt"] % 4
                    st["npt"] += 1
                    ptb = pt[p_]
                    nj = len(js)
                    TR.op("act", lambda e, ptb=ptb, b=b, nj=nj: e.activation(out=ptb[:, 0:nj * 128], in_=ps[:, b, 0:nj * 128], func=AF.Exp, scale=ASCALE),
                          reads=[("ps", b)], writes=[("pt", p_)])
                    for f in pend:
                        f()
                    pend = []
                    for jj, J in enumerate(js):
                        pend.append(lambda ptb=ptb, p_=p_, jj=jj, J=J: TR.op(
                            "pe", lambda e: e.matmul(ps[:, po, 0:129], lhsT=ptb[:, jj * 128:(jj + 1) * 128], rhs=vh[s_][:, J, 0:129], start=(J == 0), stop=(J == I)),
                            reads=[("pt", p_), ("vh", s_, J // 8)], writes=[("ps", po)]))
                for f in pend:
                    f()
                TR.op("dve", lambda e: e.reciprocal(out=rcp, in_=ps[:, po, 128:129]), reads=[("ps", po)], writes=["rcp"])
                TR.op("act", lambda e: e.activation(out=yo, in_=ps[:, po, 0:128], func=AF.Copy, scale=rcp[:, 0:1]), reads=[("ps", po), "rcp"], writes=["yo"])
                bt = psbank()
                pbf = ps[:, bt, :].bitcast(BF16)
                TR.op("pe", lambda e: e.transpose(out=pbf[:, 0:128], in_=yo, identity=identb), reads=["yo", "identb"], writes=[("ps", bt)])
                ys = (I // 4) % 2
                TR.op("dve", lambda e: e.tensor_copy(out=yT[ys][:, (I % 4) * 128:(I % 4 + 1) * 128], in_=pbf[:, 0:128]),
                      reads=[("ps", bt)], writes=[("yT", ys)])
                if I % 4 == 3:
                    tg = I // 4
                    TR.dma("sp", lambda e: e.dma_start(out=catT_d[h * 128:(h + 1) * 128, tg * TT:(tg + 1) * TT], in_=yT[ys]),
                           "yT%d" % ys, reads=[("yT", ys)], writes=[("catT", h, tg)])

            items = [(h, I) for h in range(8) for I in range(NQB)]
            stage1(0, *items[0])
            for n in range(len(items)):
                if n + 1 < len(items):
                    stage1(n + 1, *items[n + 1])
                stage2(n, *items[n])


        def odd_stage_d(layer, hsrc, hdst):
            j = layer // 2
            A.reset()
            cat = [A.alloc(NFC * TT, BF16).rearrange("p (f t) -> p f t", t=TT) for _ in range(2)]
            wo = [A.alloc(NFC * 256, BF16).rearrange("p (k n) -> p k n", n=256) for _ in range(2)]
            epi = [A.alloc(TT, F32) for _ in range(3)]
            epi_n = [0]
            nw = 0
            cv = catT_d.rearrange("(f p) t -> p f t", p=128)
            for ti in range(NT):
                cs = ti % 2
                for q in range(4):
                    TR.dma("sp", lambda e, q=q, ti=ti, cs=cs: e.dma_start(out=cat[cs][:, q * 4:(q + 1) * 4, :], in_=cv[:, q * 4:(q + 1) * 4, ti * TT:(ti + 1) * TT]),
                           "cat%d_%d" % (cs, q), writes=[("cat", cs, q)])
                for c in range(8):
                    s_ = nw % 2
                    nw += 1
                    TR.dma("sp", lambda e, c=c, s_=s_: e.dma_start(out=wo[s_], in_=cdo_b[j, c]), "wo%d" % s_, reads=[("cdo", j, c)], writes=[("wos", s_)])
                    for hf in range(2):
                        b = psbank()
                        for k in range(NFC):
                            TR.op("pe", lambda e, s_=s_, hf=hf, k=k, b=b, cs=cs: e.matmul(ps[:, b, :], lhsT=wo[s_][:, k, hf * 128:(hf + 1) * 128], rhs=cat[cs][:, k, :],
                                                                                          start=(k == 0), stop=(k == NFC - 1)),
                                  reads=[("wos", s_), ("cat", cs, k // 4)], writes=[("ps", b)])
                        residual_epilogue(hsrc, hdst, ti, c * 2 + hf, b, 1.0, epi, epi_n)

        def run_merged(gens, totals):
            prog = [0.0] * len(gens)
            alive = [True] * len(gens)
            while any(alive):
                cand = [i for i in range(len(gens)) if alive[i]]
                i = min(cand, key=lambda k: prog[k] / totals[k])
                try:
                    prog[i] += next(gens[i])
                except StopIteration:
                    alive[i] = False

        def phase_mix_odd(layer, hsrc, hdst, next_cast=None, stages="abcd"):
            if next_cast is not None:
                next_cast()
            odd_stage_a(layer, hsrc)
            TR.barrier()
            if "b" not in stages:
                A.reset()
                zt = A.alloc(T, BF16)
                TR.op("dve", lambda e: e.memset(zt, 0.0), writes=["zt"])
                for hh_ in range(8):
                    TR.dma("sp", lambda e, hh_=hh_: e.dma_start(out=catT_d[1024 + hh_ * 128:1024 + (hh_ + 1) * 128, :], in_=zt), "zt", reads=["zt"], writes=[("catz", 8 + hh_)])
                TR.barrier()
            if "c" not in stages:
                A.reset()
                zt = A.alloc(T, BF16)
                TR.op("dve", lambda e: e.memset(zt, 0.0), writes=["zt"])
                for hh_ in range(8):
                    TR.dma("sp", lambda e, hh_=hh_: e.dma_start(out=catT_d[hh_ * 128:(hh_ + 1) * 128, :], in_=zt), "zt", reads=["zt"], writes=[("catz", hh_)])
                TR.barrier()
            A.reset()
            C.rot = [0, 1, 2, 3]
            gens, totals = [], []
            if "b" in stages:
                gens.append(odd_stage_b(layer))
                totals.append(8 * 528 + 64.0)
            if "c" in stages:
                gens.append(odd_stage_c1(layer))
                totals.append(2 * 148 + 16 * 525 * 0.5 + 32.0)
            run_merged(gens, totals)
            C.rot = list(range(8))
            TR.barrier()
            if "c" in stages:
                odd_stage_c2(layer)
                C.rot = list(range(8))
                TR.barrier()
            odd_stage_d(layer, hsrc, hdst)


        def phase_final(hsrc):
            A.reset()
            hT = A.alloc(NFC * TT, F32).rearrange("p (f t) -> p f t", t=TT)
            y = A.alloc(NFC * TT, F32).rearrange("p (f t) -> p f t", t=TT)
            sq = [A.alloc(TT, F32) for _ in range(2)]
            rstd = A.alloc(TT, F32)
            ot = [A.alloc(D, F32) for _ in range(2)]
            n = 0
            for ti in range(NT):
                rms_norm_tile(hsrc, ti, 12, hT, y, sq, rstd)
                for tb in range(TT // 128):
                    o = ot[n % 2]
                    okey = ("ot", n % 2)
                    n += 1
                    for f4 in range(NFC // 4):
                        b = psbank()
                        for k in range(4):
                            f = f4 * 4 + k
                            TR.op("pe", lambda e, f=f, k=k, b=b, tb=tb: e.transpose(
                                out=ps[:, b, k * 128:(k + 1) * 128], in_=y[:, f, tb * 128:(tb + 1) * 128], identity=ident),
                                reads=[("xn", f), "ident"], writes=[("ps", b)])
                        if f4 % 2 == 0:
                            TR.op("act", lambda e, o=o, b=b, f4=f4: e.activation(out=o[:, f4 * 512:(f4 + 1) * 512], in_=ps[:, b, :], func=AF.Copy),
                                  reads=[("ps", b)], writes=[okey])
                        else:
                            TR.op("dve", lambda e, o=o, b=b, f4=f4: e.tensor_copy(out=o[:, f4 * 512:(f4 + 1) * 512], in_=ps[:, b, :]),
                                  reads=[("ps", b)], writes=[okey])
                    r0 = ti * TT + tb * 128
                    TR.dma("sp", lambda e, o=o, r0=r0: e.dma_start(out=out[r0:r0 + 128, :], in_=o), "ot%d" % ((n - 1) % 2),
                           reads=[okey], writes=[("out", r0)])

        plan = []
        plan.append(("prologue",))
        for layer in range(DEPTH):
            plan.append(("ffn", layer * 2, layer * 3 + 0))
            plan.append(("mix", layer))
            plan.append(("ffn", layer * 2 + 1, layer * 3 + 2))
        plan.append(("final",))
        if phases is not None:
            plan = [p for p in plan if p in phases or p[0] in ("prologue", "final")]

        def caster(p):
            if p[0] == "ffn":
                return lambda: cast_ffn(p[1])
            if p[0] == "mix" and p[1] % 2 == 0:
                return lambda: cast_even(p[1] // 2)
            if p[0] == "mix":
                return lambda: cast_odd(p[1] // 2)
            return None
        wplan = [p for p in plan if caster(p) is not None]
        cur = 0
        if wplan:
            caster(wplan[0])()
        for p in plan:
            nxt = None
            if p in wplan:
                i = wplan.index(p)
                if i + 1 < len(wplan):
                    nxt = caster(wplan[i + 1])
            if p[0] == "prologue":
                phase_prologue(hbuf[cur])
            elif p[0] == "ffn":
                phase_ffn(p[1], p[2], hbuf[cur], hbuf[1 - cur], nxt)
                cur = 1 - cur
            elif p[0] == "mix":
                if p[1] % 2 == 0:
                    phase_mix_even(p[1], hbuf[cur], hbuf[1 - cur], nxt)
                    cur = 1 - cur
                else:
                    phase_mix_odd(p[1], hbuf[cur], hbuf[1 - cur], nxt, stages=odd_stages)
                    cur = 1 - cur
            elif p[0] == "final":
                phase_final(hbuf[cur])
            TR.barrier()
        TR.emit(block)
        C.nops = TR.nops
    return nc


def _t5_bucket_np(rel):
    import jax
    import jax.numpy as jnp
    import math
    with jax.default_device(jax.devices("cpu")[0]):
        rel = jnp.asarray(rel, dtype=jnp.int32)
        nb = 16
        max_exact = 8
        ret = jnp.where(rel > 0, nb, 0)
        n = jnp.abs(rel)
        large = max_exact + (jnp.log(jnp.maximum(n, 1).astype(jnp.float32) / max_exact)
                             / math.log(128 / max_exact) * (nb - max_exact)).astype(jnp.int32)
        large = jnp.minimum(large, nb - 1)
        return np.asarray(ret + jnp.where(n < max_exact, n, large))


def host_constants(rel_bias_table=None):
    t = np.arange(TT)
    invc = np.stack([1.0 / np.minimum(t + 1, w) for w in (2, 4, 8, 16)]).astype(np.float32)
    invc0 = np.ascontiguousarray(np.broadcast_to(invc.reshape(1, 4 * TT), (128, 4 * TT)))
    out = {"ident": np.eye(128, dtype=np.float32), "invc0": invc0}
    pos = np.arange(T, dtype=np.float64)
    gam = np.array([1.0 - 2.0 ** (-5.0 - h) for h in range(8)], dtype=np.float64)
    p = np.arange(128)
    i = p % 64
    f = i % 32
    sign = np.where(i < 32, -1.0, 1.0)
    freqs = 10000.0 ** (-f.astype(np.float64) / 32.0)
    ang = (pos[None, :].astype(np.float32) * freqs[:, None].astype(np.float32)).astype(np.float64)
    cos, sin = np.cos(ang), np.sin(ang) * sign[:, None]
    tl = (np.arange(T) % 128).astype(np.float64)
    rope = np.zeros((2, 4, 2, 128, T), dtype=np.float32)
    for c in range(4):
        hh = 2 * c + p // 64
        dq = gam[hh][:, None] ** tl[None, :]
        dk = (64.0 ** -0.5) * gam[hh][:, None] ** (-tl[None, :])
        rope[0, c, 0], rope[0, c, 1] = cos * dq, sin * dq
        rope[1, c, 0], rope[1, c, 1] = cos * dk, sin * dk
    out["rope_tab"] = rope
    jl = np.arange(128)[:, None]
    il = np.arange(128)[None, :]
    vis = (jl < ((il // 64) + 1) * 64)
    dd = np.zeros((128, 8, 128), dtype=np.float32)
    for h in range(8):
        m = np.where(il >= jl, 1.0, gam[h] ** (2.0 * (jl - il)))
        dd[:, h, :] = m * vis
    out["ret_diag"] = dd.reshape(128, 8 * 128)
    if rel_bias_table is not None:
        tab = np.asarray(rel_bias_table, dtype=np.float32)
        sl = np.arange(128)[:, None, None]
        blk = np.arange(2)[None, :, None]
        tl_ = np.arange(128)[None, None, :]
        rel = blk * 128 + sl - 128 - tl_
        bidx = _t5_bucket_np(rel)
        g = tab[bidx]
        out["bias_g"] = np.ascontiguousarray(np.transpose(g, (0, 3, 1, 2))).reshape(128, 8 * 2 * 128)
        out["bias_t15"] = np.ascontiguousarray(np.broadcast_to(tab[15:16, :], (128, 8)))
        out["bias_tab"] = np.ascontiguousarray(np.broadcast_to(tab.reshape(1, 256), (128, 256)))
    return out


def make_in_maps(inputs, n_cores=N_CORES):
    consts = host_constants(inputs.get("rel_bias_table"))
    shared = {
        "norm_gains": np.ascontiguousarray(np.asarray(inputs["norm_gains"], dtype=np.float32).reshape(DEPTH * 3, D)),
        "ffn_w_gate_up": np.ascontiguousarray(np.asarray(inputs["ffn_w_gate_up"], dtype=np.float32).reshape(DEPTH * 2, D, 2 * DFF)),
        "ffn_w_down": np.ascontiguousarray(np.asarray(inputs["ffn_w_down"], dtype=np.float32).reshape(DEPTH * 2, DFF, D)),
        "final_norm": np.ascontiguousarray(np.asarray(inputs["final_norm"], dtype=np.float32).reshape(1, D)),
    }
    for k in ("ab_w_in", "ab_conv_w", "ab_pool_w", "ab_pool_scale", "ab_w_out", "cd_w_in", "ret_norm_gain", "cd_w_out"):
        shared[k] = np.ascontiguousarray(np.asarray(inputs[k], dtype=np.float32))
    shared.update(consts)
    xs = np.asarray(inputs["x"], dtype=np.float32)
    maps = []
    for c in range(n_cores):
        m = dict(shared)
        m["x"] = np.ascontiguousarray(xs[c])
        maps.append(m)
    return maps


def kernel(**inputs):
    nc = build_program()
    in_maps = make_in_maps(inputs)
    res = run_bass_kernel_spmd(nc, in_maps, core_ids=list(range(N_CORES)))
    return np.stack([np.asarray(r["out"], dtype=np.float32) for r in res.results], axis=0)
```

```python
import numpy as np
import concourse.bass as bass
import concourse.mybir as mybir
from concourse.bass_utils import run_bass_kernel_spmd

F32 = mybir.dt.float32
BF16 = mybir.dt.bfloat16
AF = mybir.ActivationFunctionType
ALU = mybir.AluOpType
AX = mybir.AxisListType

D = 2048
T = 4096
DFF = 5632
DEPTH = 4
NFC = D // 128
TT = 512
NT = T // TT
RMS_EPS = 1e-6
N_CORES = 8

CENGS = ("pe", "act", "dve", "pool")
ENGS = CENGS + ("sp",)


class Op:
    __slots__ = ("eng", "fn", "deps", "xdeps", "is_dma", "dsem", "dval", "val", "waited")

    def __init__(self, eng, fn, is_dma=False):
        self.eng = eng
        self.fn = fn
        self.deps = []
        self.xdeps = []
        self.is_dma = is_dma
        self.dsem = None
        self.dval = 0
        self.val = None
        self.waited = False


SEM_ALIAS = {}
for _a in range(2):
    for _b in range(4):
        SEM_ALIAS["cat%d_%d" % (_a, _b)] = "wd%d_%d" % (_a, _b)
for _b in range(4):
    SEM_ALIAS["vv0_%d" % _b] = "hT%d" % _b
    SEM_ALIAS["vv1_%d" % _b] = "wd0_%d" % _b
for _a in range(2):
    SEM_ALIAS["wF%d" % _a] = "wg%d" % _a
    SEM_ALIAS["wT%d" % _a] = "wu%d" % _a
    SEM_ALIAS["wR%d_0" % _a] = "wc%d" % _a
    SEM_ALIAS["wR%d_1" % _a] = "wp%d" % _a
    SEM_ALIAS["cq%d" % _a] = "rq%d" % _a
    SEM_ALIAS["ck%d" % _a] = "rk%d" % _a
    SEM_ALIAS["xt%d" % _a] = "ot%d" % _a
for _a in range(3):
    SEM_ALIAS["st%d" % _a] = "stg%d" % _a


class Tracker:
    def __init__(self, nc):
        self.nc = nc
        self.esem = {e: nc.alloc_semaphore(name="es_" + e) for e in CENGS}
        self.ecount = {e: 0 for e in CENGS}
        self.dma_sems = {}
        self.dma_cnt = {}
        self.last_w = {}
        self.readers = {}
        self.byeng = {e: [] for e in ENGS}
        self.nops = 0

    def _add(self, op, reads, writes):
        deps = []
        for r in reads:
            w = self.last_w.get(r)
            if w is not None:
                deps.append(w)
        for w_ in writes:
            w = self.last_w.get(w_)
            if w is not None:
                deps.append(w)
            deps.extend(self.readers.get(w_, ()))
        for r in reads:
            self.readers.setdefault(r, []).append(op)
        for w_ in writes:
            self.last_w[w_] = op
            self.readers[w_] = []
        seen = set()
        for d in deps:
            if d is op or id(d) in seen:
                continue
            if op.eng == "pe" and d.eng == "pe" and not d.is_dma and not op.is_dma:
                continue
            seen.add(id(d))
            op.deps.append(d)
            d.waited = True
        self.byeng[op.eng].append(op)
        self.nops += 1
        return op

    def op(self, eng, fn, reads=(), writes=()):
        return self._add(Op(eng, fn), reads, writes)

    def dma(self, eng, fn, sem, reads=(), writes=()):
        sem = SEM_ALIAS.get(sem, sem)
        op = Op(eng, fn, is_dma=True)
        if sem not in self.dma_sems:
            self.dma_sems[sem] = self.nc.alloc_semaphore(name="ds_" + sem)
            self.dma_cnt[sem] = 0
        self.dma_cnt[sem] += 16
        op.dsem = sem
        op.dval = self.dma_cnt[sem]
        return self._add(op, reads, writes)

    def barrier(self):
        bs = []
        for e in CENGS:
            b = Op(e, None)
            b.waited = True
            for f in CENGS:
                if f != e:
                    for o in reversed(self.byeng[f]):
                        if not o.is_dma:
                            b.deps.append(o)
                            o.waited = True
                            break
            for s, c in self.dma_cnt.items():
                if c:
                    b.xdeps.append((s, c))
            bs.append(b)
        for b in bs:
            self.byeng[b.eng].append(b)
        for e in ENGS:
            c = Op(e, None)
            c.deps = list(bs)
            self.byeng[e].append(c)
        self.last_w = {}
        self.readers = {}

    def emit(self, block):
        for e in CENGS:
            for op in self.byeng[e]:
                if not op.is_dma and op.waited:
                    self.ecount[e] += 1
                    op.val = self.ecount[e]
        seen = {}

        def run(en, eng):
            def w(key, sem, v):
                if seen.get((en, key), 0) >= v:
                    return
                seen[(en, key)] = v
                eng.wait_ge(sem, v)

            for op in self.byeng[en]:
                for d in op.deps:
                    if d.is_dma:
                        w(("d", d.dsem), self.dma_sems[d.dsem], d.dval)
                    elif d.eng != "sp":
                        w(("e", d.eng), self.esem[d.eng], d.val)
                for (s, v) in op.xdeps:
                    w(("d", s), self.dma_sems[s], v)
                if op.fn is None:
                    if op.val is not None:
                        eng.nop().then_inc(self.esem[op.eng], 1)
                    continue
                ins = op.fn(eng)
                if op.is_dma:
                    ins.then_inc(self.dma_sems[op.dsem], 16)
                elif op.val is not None:
                    ins.then_inc(self.esem[op.eng], 1)

        @block.tensor
        def _(e):
            run("pe", e)

        @block.scalar
        def _(e):
            run("act", e)

        @block.vector
        def _(e):
            run("dve", e)

        @block.gpsimd
        def _(e):
            run("pool", e)

        @block.sync
        def _(e):
            run("sp", e)


class Arena:
    def __init__(self, big, n32):
        self.big = big
        self.n32 = n32
        self.off = 0
        self.mark = 0

    def set_mark(self):
        self.mark = self.off

    def reset(self):
        self.off = self.mark

    def alloc(self, n_elems, dtype):
        sz = 2 if dtype == BF16 else 4
        n32 = (n_elems * sz + 3) // 4
        n32 = (n32 + 7) // 8 * 8
        assert self.off + n32 <= self.n32, f"SBUF arena overflow {self.off}+{n32}>{self.n32}"
        ap = self.big[:, self.off:self.off + n32]
        self.off += n32
        if dtype != F32:
            ap = ap.bitcast(dtype)
        return ap[:, :n_elems]


class Ctx:
    pass


def build_program(phases=None, debug_out=False, odd_stages="abcd"):
    nc = bass.Bass("TRN2", target_bir_lowering=False)
    C = Ctx()
    C.nc = nc
    dt = nc.dram_tensor
    x = dt("x", [T, D], F32, kind="ExternalInput").ap()
    norm_gains = dt("norm_gains", [DEPTH * 3, D], F32, kind="ExternalInput").ap()
    w_gu = dt("ffn_w_gate_up", [DEPTH * 2, D, 2 * DFF], F32, kind="ExternalInput").ap()
    w_dn = dt("ffn_w_down", [DEPTH * 2, DFF, D], F32, kind="ExternalInput").ap()
    final_norm = dt("final_norm", [1, D], F32, kind="ExternalInput").ap()
    ident_d = dt("ident", [128, 128], F32, kind="ExternalInput").ap()
    ab_w_in = dt("ab_w_in", [2, D, 4096], F32, kind="ExternalInput").ap()
    ab_conv_w = dt("ab_conv_w", [2, 3, 1024], F32, kind="ExternalInput").ap()
    ab_pool_w = dt("ab_pool_w", [2, 4, 256, 256], F32, kind="ExternalInput").ap()
    ab_pool_scale = dt("ab_pool_scale", [2, 1024], F32, kind="ExternalInput").ap()
    ab_w_out = dt("ab_w_out", [2, D, D], F32, kind="ExternalInput").ap()
    invc0_d = dt("invc0", [128, 4 * TT], F32, kind="ExternalInput").ap()
    cd_w_in = dt("cd_w_in", [2, D, 7248], F32, kind="ExternalInput").ap()
    ret_norm_gain = dt("ret_norm_gain", [2, 1024], F32, kind="ExternalInput").ap()
    cd_w_out = dt("cd_w_out", [2, D, D], F32, kind="ExternalInput").ap()
    rope_d = dt("rope_tab", [2, 4, 2, 128, T], F32, kind="ExternalInput").ap()
    dd_d = dt("ret_diag", [128, 8 * 128], F32, kind="ExternalInput").ap()
    biasg_d = dt("bias_g", [128, 8 * 2 * 128], F32, kind="ExternalInput").ap()
    t15_d = dt("bias_t15", [128, 8], F32, kind="ExternalInput").ap()
    tab_d = dt("bias_tab", [128, 256], F32, kind="ExternalInput").ap()
    out = dt("out", [T, D], F32, kind="ExternalOutput").ap()
    hbuf = [dt("hA", [D, T], F32, kind="Internal").ap(), dt("hB", [D, T], F32, kind="Internal").ap()]
    NGC = DFF // 256
    NDC = D // 256
    NKF = DFF // 128
    wg_b = dt("wg_b", [DEPTH * 2, NGC, 128, NFC, 256], BF16, kind="Internal").ap()
    wu_b = dt("wu_b", [DEPTH * 2, NGC, 128, NFC, 256], BF16, kind="Internal").ap()
    wd_b = dt("wd_b", [DEPTH * 2, NDC, 128, NKF, 256], BF16, kind="Internal").ap()
    abc_b = dt("abc_b", [2, 8, 128, NFC, 384], BF16, kind="Internal").ap()
    abp_b = dt("abp_b", [2, 4, 128, NFC, 256], BF16, kind="Internal").ap()
    abo_b = dt("abo_b", [2, 8, 128, NFC, 256], BF16, kind="Internal").ap()
    cdF_b = dt("cdF_b", [2, 16, 128, NFC, 256], BF16, kind="Internal").ap()
    cdI_b = dt("cdI_b", [2, 128, NFC, 128], BF16, kind="Internal").ap()
    cdR_b = dt("cdR_b", [2, 16, 128, NFC, 128], BF16, kind="Internal").ap()
    cdT_b = dt("cdT_b", [2, 4, 128, NFC, 512], BF16, kind="Internal").ap()
    cdW_b = dt("cdW_b", [2, 128, NFC, 16], BF16, kind="Internal").ap()
    cdo_b = dt("cdo_b", [2, 8, 128, NFC, 256], BF16, kind="Internal").ap()
    qT_d = dt("qT_d", [1024, T], BF16, kind="Internal").ap()
    kT_d = dt("kT_d", [1024, T], BF16, kind="Internal").ap()
    iqT_d = dt("iqT_d", [1024, T], BF16, kind="Internal").ap()
    ikT_d = dt("ikT_d", [128, T], BF16, kind="Internal").ap()
    iw_d = dt("iw_d", [T, 16], F32, kind="Internal").ap()
    rqT_d = dt("rqT_d", [512, T], BF16, kind="Internal").ap()
    rkT_d = dt("rkT_d", [512, T], BF16, kind="Internal").ap()
    vc_d = dt("vc_d", [T, 1024], BF16, kind="Internal").ap()
    vr_d = dt("vr_d", [T, 1024], BF16, kind="Internal").ap()
    sg_d = dt("sg_d", [1024, T], F32, kind="Internal").ap()
    catT_d = dt("catT_d", [D, T], BF16, kind="Internal").ap()
    nmT_d = dt("nmT_d", [32, 128, 32 * 128], BF16, kind="Internal").ap()

    N32 = 51 * 1024 + 512
    es = nc.sbuf_tensor("big", [128, N32], F32)
    ps_cm = nc.psum_tensor("ps", [128, 8, 512], F32)
    with es as big, ps_cm as ps, nc.Block() as block:
        TR = Tracker(nc)
        A = Arena(big, N32)
        C.TR, C.A, C.ps = TR, A, ps
        C.psn = 0

        C.rot = list(range(8))

        def psbank():
            b = C.rot[C.psn % len(C.rot)]
            C.psn += 1
            return b
        C.psbank = psbank

        ident = A.alloc(128, F32)
        ones = A.alloc(128, F32)
        gains = A.alloc(13 * NFC, F32).rearrange("p (s f) -> p s f", f=NFC)
        TR.dma("sp", lambda e: e.dma_start(out=ident, in_=ident_d), "cid", writes=["ident"])
        TR.op("dve", lambda e: e.memset(ones, 1.0), writes=["ones"])
        for s in range(13):
            src = norm_gains[s] if s < 12 else final_norm[0]
            TR.dma("sp", lambda e, s=s, src=src: e.dma_start(
                out=gains[:, s, :], in_=src.rearrange("(f p) -> p f", p=128), allow_slow_non_contiguous=True),
                "const", writes=["gains"])
        C.ident, C.ones, C.gains = ident, ones, gains
        A.set_mark()

        def cast_ffn(fi):
            wv = w_gu[fi].rearrange("(kc p) n -> p kc n", p=128)
            for c in range(NGC):
                TR.dma("pool", lambda e, c=c: e.dma_start(out=wg_b[fi, c], in_=wv[:, :, c * 256:(c + 1) * 256]),
                       "cast", writes=[("wg", fi, c)])
                TR.dma("pool", lambda e, c=c: e.dma_start(out=wu_b[fi, c], in_=wv[:, :, DFF + c * 256:DFF + (c + 1) * 256]),
                       "cast", writes=[("wu", fi, c)])
            dv = w_dn[fi].rearrange("(kc p) n -> p kc n", p=128)
            for c in range(NDC):
                for k0 in range(0, NKF, 11):
                    TR.dma("pool", lambda e, c=c, k0=k0: e.dma_start(out=wd_b[fi, c, :, k0:k0 + 11, :],
                                                                      in_=dv[:, k0:k0 + 11, c * 256:(c + 1) * 256]),
                           "cast", writes=[("wd", fi, c, k0)])

        def rms_norm_tile(hsrc, ti, gslot, hT, xn, sq, rstd, xkey="xn"):
            t0 = ti * TT
            hv = hsrc.rearrange("(f p) t -> p f t", p=128)
            for q in range(4):
                TR.dma("sp", lambda e, q=q: e.dma_start(out=hT[:, q * 4:(q + 1) * 4, :], in_=hv[:, q * 4:(q + 1) * 4, t0:t0 + TT]),
                       "hT%d" % q, writes=[("hT", q)])
            b = psbank()
            for f in range(NFC):
                s = sq[f % 2]
                TR.op("act", lambda e, f=f, s=s: e.activation(out=s, in_=hT[:, f, :], func=AF.Square),
                      reads=[("hT", f // 4)], writes=[("sq", f % 2)])
                TR.op("pe", lambda e, f=f, s=s: e.matmul(ps[:, b, :], lhsT=ones, rhs=s, start=(f == 0), stop=(f == NFC - 1)),
                      reads=[("sq", f % 2), "ones"], writes=[("ps", b)])
            TR.op("dve", lambda e: e.tensor_scalar(out=rstd, in0=ps[:, b, :], scalar1=1.0 / D, scalar2=RMS_EPS,
                                                   op0=ALU.mult, op1=ALU.add), reads=[("ps", b)], writes=["rstd"])
            TR.op("act", lambda e: e.activation(out=rstd, in_=rstd, func=AF.Sqrt), reads=["rstd"], writes=["rstd"])
            TR.op("dve", lambda e: e.reciprocal(out=rstd, in_=rstd), reads=["rstd"], writes=["rstd"])
            for f in range(NFC):
                TR.op("dve", lambda e, f=f: e.scalar_tensor_tensor(out=xn[:, f, :], in0=hT[:, f, :], scalar=gains[:, gslot, f:f + 1],
                                                                 in1=rstd, op0=ALU.mult, op1=ALU.mult),
                      reads=[("hT", f // 4), "rstd", "gains"], writes=[(xkey, f)])

        def residual_epilogue(hsrc, hdst, ti, fo, b, scale, epi, epi_n):
            t0 = ti * TT
            slot = epi_n[0] % len(epi)
            epi_n[0] += 1
            et = epi[slot]
            TR.dma("act", lambda e: e.dma_start(out=et, in_=hsrc[fo * 128:(fo + 1) * 128, t0:t0 + TT]),
                   "epi%d" % slot, writes=[("epi", slot)])
            TR.op("dve", lambda e: e.scalar_tensor_tensor(out=et, in0=ps[:, b, :], scalar=float(scale), in1=et,
                                                          op0=ALU.mult, op1=ALU.add),
                  reads=[("ps", b)], writes=[("epi", slot)])
            TR.dma("act", lambda e: e.dma_start(out=hdst[fo * 128:(fo + 1) * 128, t0:t0 + TT], in_=et),
                   "epi%d" % slot, reads=[("epi", slot)], writes=[("hdst", fo, ti)])

        def phase_prologue(hdst):
            A.reset()
            xt = [A.alloc(D, F32) for _ in range(2)]
            st = [A.alloc(TT, F32) for _ in range(3)]
            n = 0
            for tb in range(T // 128):
                xs = xt[tb % 2]
                TR.dma("sp", lambda e, xs=xs, tb=tb: e.dma_start(out=xs, in_=x[tb * 128:(tb + 1) * 128, :]),
                       "xt%d" % (tb % 2), writes=[("xt", tb % 2)])
                for f4 in range(NFC // 4):
                    b = psbank()
                    for k in range(4):
                        f = f4 * 4 + k
                        TR.op("pe", lambda e, xs=xs, f=f, k=k, b=b: e.transpose(out=ps[:, b, k * 128:(k + 1) * 128],
                                                                              in_=xs[:, f * 128:(f + 1) * 128], identity=ident),
                              reads=[("xt", tb % 2), "ident"], writes=[("ps", b)])
                    s = n % 3
                    n += 1
                    sb = st[s]
                    eng = "act" if n % 2 == 0 else "dve"
                    if eng == "act":
                        TR.op("act", lambda e, sb=sb, b=b: e.activation(out=sb, in_=ps[:, b, :], func=AF.Copy),
                              reads=[("ps", b)], writes=[("st", s)])
                    else:
                        TR.op("dve", lambda e, sb=sb, b=b: e.tensor_copy(out=sb, in_=ps[:, b, :]),
                              reads=[("ps", b)], writes=[("st", s)])
                    dst = hdst.rearrange("(f p) t -> p f t", p=128)[:, f4 * 4:(f4 + 1) * 4, tb * 128:(tb + 1) * 128]
                    TR.dma("sp", lambda e, sb=sb, dst=dst: e.dma_start(out=dst, in_=sb.rearrange("p (k t) -> p k t", t=128)),
                           "st%d" % s, reads=[("st", s)], writes=[("hdst", f4, tb)])

        def phase_ffn(fi, gslot, hsrc, hdst, next_cast=None):
            A.reset()
            hT = A.alloc(NFC * TT, F32).rearrange("p (f t) -> p f t", t=TT)
            xns = [A.alloc(NFC * TT, BF16).rearrange("p (f t) -> p f t", t=TT) for _ in range(2)]
            act = A.alloc(NKF * TT, BF16).rearrange("p (f t) -> p f t", t=TT)
            wg = [A.alloc(NFC * 256, BF16).rearrange("p (k n) -> p k n", n=256) for _ in range(2)]
            wu = [A.alloc(NFC * 256, BF16).rearrange("p (k n) -> p k n", n=256) for _ in range(2)]
            wd = [A.alloc(NKF * 256, BF16).rearrange("p (k n) -> p k n", n=256) for _ in range(2)]
            sq = [A.alloc(TT, F32) for _ in range(2)]
            rstd = A.alloc(TT, F32)
            sg = [A.alloc(TT, F32) for _ in range(2)]
            epi = [A.alloc(TT, F32) for _ in range(4)]
            epi_n = [0]
            wn = [0, 0]
            if next_cast is not None:
                next_cast()
            rms_norm_tile(hsrc, 0, gslot, hT, xns[0], sq, rstd, xkey="xn0")
            for ti in range(NT):
                xn = xns[ti % 2]
                xk = "xn%d" % (ti % 2)
                for c in range(NGC):
                    s = wn[0] % 2
                    wn[0] += 1
                    TR.dma("sp", lambda e, c=c, s=s: e.dma_start(out=wg[s], in_=wg_b[fi, c]), "wg%d" % s,
                           reads=[("wg", fi, c)], writes=[("wgs", s)])
                    TR.dma("sp", lambda e, c=c, s=s: e.dma_start(out=wu[s], in_=wu_b[fi, c]), "wu%d" % s,
                           reads=[("wu", fi, c)], writes=[("wus", s)])
                    for hf in range(2):
                        bg = psbank()
                        bu = psbank()
                        for k in range(NFC):
                            TR.op("pe", lambda e, s=s, hf=hf, k=k, bg=bg, xn=xn: e.matmul(
                                ps[:, bg, :], lhsT=wg[s][:, k, hf * 128:(hf + 1) * 128], rhs=xn[:, k, :],
                                start=(k == 0), stop=(k == NFC - 1)),
                                reads=[("wgs", s), (xk, k)], writes=[("ps", bg)])
                        for k in range(NFC):
                            TR.op("pe", lambda e, s=s, hf=hf, k=k, bu=bu, xn=xn: e.matmul(
                                ps[:, bu, :], lhsT=wu[s][:, k, hf * 128:(hf + 1) * 128], rhs=xn[:, k, :],
                                start=(k == 0), stop=(k == NFC - 1)),
                                reads=[("wus", s), (xk, k)], writes=[("ps", bu)])
                        j = c * 2 + hf
                        sgt = sg[j % 2]
                        TR.op("act", lambda e, sgt=sgt, bg=bg: e.activation(out=sgt, in_=ps[:, bg, :], func=AF.Silu),
                              reads=[("ps", bg)], writes=[("sg", j % 2)])
                        TR.op("dve", lambda e, sgt=sgt, bu=bu, j=j: e.tensor_tensor(out=act[:, j, :], in0=ps[:, bu, :], in1=sgt,
                                                                                     op=ALU.mult),
                              reads=[("ps", bu), ("sg", j % 2)], writes=[("act", j)])
                for c in range(NDC):
                    s = wn[1] % 2
                    wn[1] += 1
                    for k0 in range(0, NKF, 11):
                        TR.dma("sp", lambda e, c=c, s=s, k0=k0: e.dma_start(out=wd[s][:, k0:k0 + 11, :], in_=wd_b[fi, c, :, k0:k0 + 11, :]),
                               "wd%d_%d" % (s, k0 // 11), reads=[("wd", fi, c, k0)], writes=[("wds", s, k0)])
                    if c == 2 and ti + 1 < NT:
                        rms_norm_tile(hsrc, ti + 1, gslot, hT, xns[(ti + 1) % 2], sq, rstd, xkey="xn%d" % ((ti + 1) % 2))
                    for hf in range(2):
                        b = psbank()
                        for k in range(NKF):
                            TR.op("pe", lambda e, s=s, hf=hf, k=k, b=b: e.matmul(
                                ps[:, b, :], lhsT=wd[s][:, k, hf * 128:(hf + 1) * 128], rhs=act[:, k, :],
                                start=(k == 0), stop=(k == NKF - 1)),
                                reads=[("wds", s, (k // 11) * 11), ("act", k)], writes=[("ps", b)])
                        residual_epilogue(hsrc, hdst, ti, c * 2 + hf, b, 0.5, epi, epi_n)

        def cast_even(j):
            wv = ab_w_in[j].rearrange("(kc p) n -> p kc n", p=128)
            for jj in range(8):
                for wh in range(3):
                    c0 = wh * 1024 + jj * 128
                    TR.dma("pool", lambda e, jj=jj, wh=wh, c0=c0: e.dma_start(out=abc_b[j, jj, :, :, wh * 128:(wh + 1) * 128],
                                                                             in_=wv[:, :, c0:c0 + 128]),
                           "cast", writes=[("abc", j, jj, wh)])
            for g in range(4):
                c0 = 3072 + g * 256
                TR.dma("pool", lambda e, g=g, c0=c0: e.dma_start(out=abp_b[j, g], in_=wv[:, :, c0:c0 + 256]),
                       "cast", writes=[("abp", j, g)])
            ov = ab_w_out[j].rearrange("(kc p) n -> p kc n", p=128)
            for c in range(8):
                TR.dma("pool", lambda e, c=c: e.dma_start(out=abo_b[j, c], in_=ov[:, :, c * 256:(c + 1) * 256]),
                       "cast", writes=[("abo", j, c)])

        def phase_mix_even(layer, hsrc, hdst, next_cast=None):
            j = layer // 2
            gslot = layer * 3 + 1
            A.reset()
            hT = A.alloc(NFC * TT, F32).rearrange("p (f t) -> p f t", t=TT)
            xns = [A.alloc(NFC * TT, BF16).rearrange("p (f t) -> p f t", t=TT) for _ in range(2)]
            cat = A.alloc(NFC * TT, BF16).rearrange("p (f t) -> p f t", t=TT)
            ucat = A.alloc(8 * 514, F32).rearrange("p (f t) -> p f t", t=514)
            pcat = A.alloc(8 * 527, F32).rearrange("p (f t) -> p f t", t=527)
            wc = [A.alloc(NFC * 384, BF16).rearrange("p (k n) -> p k n", n=384) for _ in range(2)]
            wp = [A.alloc(NFC * 256, BF16).rearrange("p (k n) -> p k n", n=256) for _ in range(2)]
            wo = [A.alloc(NFC * 256, BF16).rearrange("p (k n) -> p k n", n=256) for _ in range(2)]
            pw = A.alloc(8 * 256, BF16).rearrange("p (g n) -> p g n", n=256)
            cw = A.alloc(24, F32).rearrange("p (k j) -> p k j", j=8)
            psc = A.alloc(8, F32)
            invc0 = A.alloc(4 * TT, F32).rearrange("p (g t) -> p g t", t=TT)
            sq = [A.alloc(TT, F32) for _ in range(2)]
            rstd = A.alloc(TT, F32)
            tmp1 = A.alloc(TT, F32)
            yv = A.alloc(TT, F32)
            tA = A.alloc(528, F32)
            tB = A.alloc(528, F32)
            pl = A.alloc(2 * TT, BF16).rearrange("p (i t) -> p i t", t=TT)
            epi = [A.alloc(TT, F32) for _ in range(3)]
            epi_n = [0]
            wn = [0, 0, 0]
            TR.dma("pool", lambda e: e.dma_start(out=pw, in_=ab_pool_w[j].rearrange("g (i p) n -> p (g i) n", p=128)),
                   "oc4", writes=["pw"])
            TR.dma("sp", lambda e: e.dma_start(out=cw, in_=ab_conv_w[j].rearrange("k (j p) -> p k j", p=128),
                                               allow_slow_non_contiguous=True), "oc5", writes=["cw"])
            TR.dma("sp", lambda e: e.dma_start(out=psc, in_=ab_pool_scale[j].rearrange("(m p) -> p m", p=128),
                                               allow_slow_non_contiguous=True), "oc0", writes=["psc"])
            TR.dma("sp", lambda e: e.dma_start(out=invc0.rearrange("p g t -> p (g t)"), in_=invc0_d), "oc1", writes=["invc0"])
            TR.op("dve", lambda e: e.memset(ucat.rearrange("p f t -> p (f t)"), 0.0), writes=[("ucat", m) for m in range(8)])
            TR.op("dve", lambda e: e.memset(pcat.rearrange("p f t -> p (f t)"), 0.0), writes=[("pcat", m) for m in range(8)])
            if next_cast is not None:
                next_cast()
            rms_norm_tile(hsrc, 0, gslot, hT, xns[0], sq, rstd, xkey="xn0")
            for ti in range(NT):
                xn = xns[ti % 2]
                xk = "xn%d" % (ti % 2)
                for jj in range(8):
                    s = wn[0] % 2
                    wn[0] += 1
                    TR.dma("sp", lambda e, jj=jj, s=s: e.dma_start(out=wc[s], in_=abc_b[j, jj]), "wc%d" % s,
                           reads=[("abc", j, jj, wh) for wh in range(3)], writes=[("wcs", s)])
                    banks = []
                    for wh in range(3):
                        b = psbank()
                        banks.append(b)
                        for k in range(NFC):
                            TR.op("pe", lambda e, s=s, wh=wh, k=k, b=b, xn=xn: e.matmul(
                                ps[:, b, :], lhsT=wc[s][:, k, wh * 128:(wh + 1) * 128], rhs=xn[:, k, :],
                                start=(k == 0), stop=(k == NFC - 1)),
                                reads=[("wcs", s), (xk, k)], writes=[("ps", b)])
                    bb, bc, bh = banks
                    uk = ("ucat", jj)
                    TR.op("act", lambda e, bc=bc: e.activation(out=tmp1, in_=ps[:, bc, :], func=AF.Copy),
                          reads=[("ps", bc)], writes=["tmp1"])
                    TR.op("dve", lambda e, jj=jj, bh=bh: e.tensor_tensor(out=ucat[:, jj, 2:514], in0=ps[:, bh, :], in1=tmp1, op=ALU.mult),
                          reads=[("ps", bh), "tmp1"], writes=[uk])
                    TR.op("dve", lambda e, jj=jj: e.tensor_scalar(out=yv, in0=ucat[:, jj, 0:512], scalar1=cw[:, 0, jj:jj + 1], scalar2=None,
                                                                  op0=ALU.mult), reads=[uk, "cw"], writes=["yv"])
                    TR.op("dve", lambda e, jj=jj: e.scalar_tensor_tensor(out=yv, in0=ucat[:, jj, 1:513], scalar=cw[:, 1, jj:jj + 1], in1=yv,
                                                                         op0=ALU.mult, op1=ALU.add), reads=[uk, "cw", "yv"], writes=["yv"])
                    TR.op("dve", lambda e, jj=jj: e.scalar_tensor_tensor(out=yv, in0=ucat[:, jj, 2:514], scalar=cw[:, 2, jj:jj + 1], in1=yv,
                                                                         op0=ALU.mult, op1=ALU.add), reads=[uk, "cw", "yv"], writes=["yv"])
                    TR.op("dve", lambda e, jj=jj, bb=bb: e.tensor_tensor(out=cat[:, jj, :], in0=ps[:, bb, :], in1=yv, op=ALU.mult),
                          reads=[("ps", bb), "yv"], writes=[("cat", jj)])
                    TR.op("act", lambda e, jj=jj: e.activation(out=ucat[:, jj, 0:2], in_=ucat[:, jj, 512:514], func=AF.Copy),
                          reads=[uk], writes=[uk])
                for g in range(4):
                    W = (2, 4, 8, 16)[g]
                    s = wn[1] % 2
                    wn[1] += 1
                    TR.dma("sp", lambda e, g=g, s=s: e.dma_start(out=wp[s], in_=abp_b[j, g]), "wp%d" % s,
                           reads=[("abp", j, g)], writes=[("wps", s)])
                    for ic in range(2):
                        m = 2 * g + ic
                        pk = ("pcat", m)
                        b = psbank()
                        for k in range(NFC):
                            TR.op("pe", lambda e, s=s, ic=ic, k=k, b=b, xn=xn: e.matmul(
                                ps[:, b, :], lhsT=wp[s][:, k, ic * 128:(ic + 1) * 128], rhs=xn[:, k, :],
                                start=(k == 0), stop=(k == NFC - 1)),
                                reads=[("wps", s), (xk, k)], writes=[("ps", b)])
                        TR.op("act", lambda e, m=m, b=b: e.activation(out=pcat[:, m, 15:527], in_=ps[:, b, :], func=AF.Copy),
                              reads=[("ps", b)], writes=[pk])
                        lo = 15 - (W - 1)
                        ln = 512 + W - 1
                        cur = pcat[:, m, lo:lo + ln]
                        curkey = pk
                        st = 1
                        bufs = [(tA, "tA"), (tB, "tB")]
                        bi = 0
                        while st < W:
                            nb, nk = bufs[bi]
                            bi ^= 1
                            nl = ln - st
                            TR.op("dve", lambda e, cur=cur, nb=nb, st=st, nl=nl: e.tensor_tensor(
                                out=nb[:, 0:nl], in0=cur[:, st:st + nl], in1=cur[:, 0:nl], op=ALU.add),
                                reads=[curkey], writes=[nk])
                            cur, curkey, ln = nb[:, 0:nl], nk, nl
                            st *= 2
                        assert ln == 512
                        if ti == 0:
                            TR.op("dve", lambda e, cur=cur, g=g: e.tensor_tensor(out=yv, in0=cur, in1=invc0[:, g, :], op=ALU.mult),
                                  reads=[curkey, "invc0"], writes=["yv"])
                            TR.op("dve", lambda e, m=m, ic=ic: e.tensor_tensor(out=pl[:, ic, :], in0=yv, in1=pcat[:, m, 15:527], op=ALU.subtract),
                                  reads=["yv", pk], writes=[("pl", ic)])
                        else:
                            TR.op("dve", lambda e, cur=cur, m=m, ic=ic, W=W: e.scalar_tensor_tensor(
                                out=pl[:, ic, :], in0=cur, scalar=1.0 / W, in1=pcat[:, m, 15:527], op0=ALU.mult, op1=ALU.subtract),
                                reads=[curkey, pk], writes=[("pl", ic)])
                        TR.op("act", lambda e, m=m: e.activation(out=pcat[:, m, 0:15], in_=pcat[:, m, 512:527], func=AF.Copy),
                              reads=[pk], writes=[pk])
                    for oc in range(2):
                        b = psbank()
                        for ic in range(2):
                            TR.op("pe", lambda e, g=g, ic=ic, oc=oc, b=b: e.matmul(
                                ps[:, b, :], lhsT=pw[:, 2 * g + ic, oc * 128:(oc + 1) * 128], rhs=pl[:, ic, :],
                                start=(ic == 0), stop=(ic == 1)),
                                reads=["pw", ("pl", ic)], writes=[("ps", b)])
                        mo = 2 * g + oc
                        TR.op("act", lambda e, mo=mo, b=b: e.activation(out=cat[:, 8 + mo, :], in_=ps[:, b, :], func=AF.Copy,
                                                                        scale=psc[:, mo:mo + 1]),
                              reads=[("ps", b), "psc"], writes=[("cat", 8 + mo)])
                if ti + 1 < NT:
                    rms_norm_tile(hsrc, ti + 1, gslot, hT, xns[(ti + 1) % 2], sq, rstd, xkey="xn%d" % ((ti + 1) % 2))
                for c in range(8):
                    s = wn[2] % 2
                    wn[2] += 1
                    TR.dma("sp", lambda e, c=c, s=s: e.dma_start(out=wo[s], in_=abo_b[j, c]), "wo%d" % s,
                           reads=[("abo", j, c)], writes=[("wos", s)])
                    for hf in range(2):
                        b = psbank()
                        for k in range(NFC):
                            TR.op("pe", lambda e, s=s, hf=hf, k=k, b=b: e.matmul(
                                ps[:, b, :], lhsT=wo[s][:, k, hf * 128:(hf + 1) * 128], rhs=cat[:, k, :],
                                start=(k == 0), stop=(k == NFC - 1)),
                                reads=[("wos", s), ("cat", k)], writes=[("ps", b)])
                        residual_epilogue(hsrc, hdst, ti, c * 2 + hf, b, 1.0, epi, epi_n)

        NQB = T // 128
        LN_EPS = 1e-5
        ASCALE = 128 ** -0.5
        GAM = [1.0 - 2.0 ** (-5.0 - h) for h in range(8)]

        def cast_odd(j):
            wv = cd_w_in[j].rearrange("(kc p) n -> p kc n", p=128)
            fcols = [0, 256, 512, 768, 1024, 1280, 1536, 1792, 3072, 3328, 3584, 3840, 6224, 6480, 6736, 6992]
            for g, c0 in enumerate(fcols):
                TR.dma("pool", lambda e, g=g, c0=c0: e.dma_start(out=cdF_b[j, g], in_=wv[:, :, c0:c0 + 256]),
                       "cast", writes=[("cdF", j, g)])
            for hh in range(2):
                TR.dma("pool", lambda e, hh=hh: e.dma_start(out=cdI_b[j, :, :, hh * 64:(hh + 1) * 64], in_=wv[:, :, 4096:4160]),
                       "cast", writes=[("cdI", j, hh)])
            for qk in range(2):
                base = 4176 + qk * 512
                for c in range(4):
                    TR.dma("pool", lambda e, qk=qk, c=c, base=base: e.dma_start(out=cdR_b[j, qk * 8 + c], in_=wv[:, :, base + c * 128:base + (c + 1) * 128]),
                           "cast", writes=[("cdR", j, qk * 8 + c, 0)])
                    for hh in range(2):
                        for half in range(2):
                            d0 = hh * 64 + half * 32
                            s0 = base + c * 128 + hh * 64 + (1 - half) * 32
                            TR.dma("pool", lambda e, qk=qk, c=c, d0=d0, s0=s0: e.dma_start(
                                out=cdR_b[j, qk * 8 + 4 + c, :, :, d0:d0 + 32], in_=wv[:, :, s0:s0 + 32]),
                                "cast", writes=[("cdR", j, qk * 8 + 4 + c, hh * 2 + half)])
            for g, c0 in enumerate([2048, 2560, 5200, 5712]):
                TR.dma("pool", lambda e, g=g, c0=c0: e.dma_start(out=cdT_b[j, g], in_=wv[:, :, c0:c0 + 512]),
                       "cast", writes=[("cdT", j, g)])
            TR.dma("pool", lambda e: e.dma_start(out=cdW_b[j], in_=wv[:, :, 4160:4176]), "cast", writes=[("cdW", j)])
            ov = cd_w_out[j].rearrange("(kc p) n -> p kc n", p=128)
            for c in range(8):
                TR.dma("pool", lambda e, c=c: e.dma_start(out=cdo_b[j, c], in_=ov[:, :, c * 256:(c + 1) * 256]),
                       "cast", writes=[("cdo", j, c)])

        def odd_stage_a(layer, hsrc):
            j = layer // 2
            gslot = layer * 3 + 1
            A.reset()
            hT = A.alloc(NFC * TT, F32).rearrange("p (f t) -> p f t", t=TT)
            xns = [A.alloc(NFC * TT, BF16).rearrange("p (f t) -> p f t", t=TT) for _ in range(2)]
            cur = {}
            sq = [A.alloc(TT, F32) for _ in range(2)]
            rstd = A.alloc(TT, F32)
            wF = [A.alloc(NFC * 256, BF16).rearrange("p (k n) -> p k n", n=256) for _ in range(2)]
            wR = [A.alloc(NFC * 256, BF16).rearrange("p (a k n) -> p a k n", a=2, n=128) for _ in range(2)]
            wT = [A.alloc(NFC * 512, BF16).rearrange("p (k n) -> p k n", n=512) for _ in range(2)]
            wI = A.alloc(NFC * 128, BF16).rearrange("p (k n) -> p k n", n=128)
            wW = A.alloc(NFC * 16, BF16).rearrange("p (k n) -> p k n", n=16)
            stg = [A.alloc(TT, BF16) for _ in range(4)]
            stf = [A.alloc(TT, F32) for _ in range(3)]
            rtab = [A.alloc(2 * TT, F32).rearrange("p (a t) -> p a t", t=TT) for _ in range(2)]
            t1 = A.alloc(TT, F32)
            t2 = A.alloc(TT, F32)
            iwt = A.alloc(16, F32)
            cnt = {"F": 0, "R": 0, "T": 0, "stg": 0, "stf": 0, "rt": 0}
            TR.dma("sp", lambda e: e.dma_start(out=wI, in_=cdI_b[j]), "oc1", reads=[("cdI", j, 0), ("cdI", j, 1)], writes=["wI"])
            TR.dma("sp", lambda e: e.dma_start(out=wW, in_=cdW_b[j]), "oc2", reads=[("cdW", j)], writes=["wW"])

            def fm_group(ti, lhs_fn, wkeys, evac):
                b = psbank()
                xn, xk = cur["xn"], cur["xk"]
                for k in range(NFC):
                    TR.op("pe", lambda e, k=k, b=b, xn=xn: e.matmul(ps[:, b, :], lhsT=lhs_fn(k), rhs=xn[:, k, :],
                                                                    start=(k == 0), stop=(k == NFC - 1)),
                          reads=list(wkeys) + [(xk, k)], writes=[("ps", b)])
                evac(b)

            def store_bf(ti, b, dst_rows, func=AF.Copy, use_act=True):
                sl = cnt["stg"] % 4
                cnt["stg"] += 1
                sb = stg[sl]
                if use_act:
                    TR.op("act", lambda e: e.activation(out=sb, in_=ps[:, b, :], func=func), reads=[("ps", b)], writes=[("stg", sl)])
                else:
                    TR.op("dve", lambda e: e.tensor_copy(out=sb, in_=ps[:, b, :]), reads=[("ps", b)], writes=[("stg", sl)])
                TR.dma("act", lambda e: e.dma_start(out=dst_rows[:, ti * TT:(ti + 1) * TT], in_=sb), "stg%d" % sl,
                       reads=[("stg", sl)], writes=[("scr", id(dst_rows), ti)])

            rms_norm_tile(hsrc, 0, gslot, hT, xns[0], sq, rstd, xkey="xn0")
            for ti in range(NT):
                xn = xns[ti % 2]
                xk = "xn%d" % (ti % 2)
                cur["xn"], cur["xk"] = xn, xk
                for g in range(16):
                    s_ = cnt["F"] % 2
                    cnt["F"] += 1
                    TR.dma("sp", lambda e, g=g, s_=s_: e.dma_start(out=wF[s_], in_=cdF_b[j, g]), "wF%d" % s_,
                           reads=[("cdF", j, g)], writes=[("wFs", s_)])
                    for hf in range(2):
                        ch = (g % 4) * 2 + hf
                        kind = g // 4
                        if kind < 3:
                            dst = (qT_d, kT_d, iqT_d)[kind][ch * 128:(ch + 1) * 128, :]
                            fm_group(ti, lambda k, s_=s_, hf=hf: wF[s_][:, k, hf * 128:(hf + 1) * 128], [("wFs", s_)],
                                     lambda b, dst=dst, ch=ch: store_bf(ti, b, dst, use_act=(ch % 2 == 0)))
                        else:
                            def ev(b, ch=ch, ti=ti):
                                sl = cnt["stf"] % 3
                                cnt["stf"] += 1
                                sb = stf[sl]
                                TR.op("act", lambda e: e.activation(out=sb, in_=ps[:, b, :], func=AF.Silu), reads=[("ps", b)], writes=[("stf", sl)])
                                TR.dma("act", lambda e: e.dma_start(out=sg_d[ch * 128:(ch + 1) * 128, ti * TT:(ti + 1) * TT], in_=sb),
                                       "stf%d" % sl, reads=[("stf", sl)], writes=[("sg_d", ch, ti)])
                            fm_group(ti, lambda k, s_=s_, hf=hf: wF[s_][:, k, hf * 128:(hf + 1) * 128], [("wFs", s_)], ev)
                fm_group(ti, lambda k: wI[:, k, :], ["wI"], lambda b: store_bf(ti, b, ikT_d))
                if ti + 1 < NT:
                    rms_norm_tile(hsrc, ti + 1, gslot, hT, xns[(ti + 1) % 2], sq, rstd, xkey="xn%d" % ((ti + 1) % 2))
                for qk in range(2):
                    for c in range(4):
                        s_ = cnt["R"] % 2
                        cnt["R"] += 1
                        TR.dma("sp", lambda e, qk=qk, c=c, s_=s_: e.dma_start(out=wR[s_][:, 0], in_=cdR_b[j, qk * 8 + c]), "wR%d_0" % s_,
                               reads=[("cdR", j, qk * 8 + c, 0)], writes=[("wRs", s_, 0)])
                        TR.dma("sp", lambda e, qk=qk, c=c, s_=s_: e.dma_start(out=wR[s_][:, 1], in_=cdR_b[j, qk * 8 + 4 + c]), "wR%d_1" % s_,
                               reads=[("cdR", j, qk * 8 + 4 + c, q) for q in range(4)], writes=[("wRs", s_, 1)])
                        r_ = cnt["rt"] % 2
                        cnt["rt"] += 1
                        TR.dma("sp", lambda e, qk=qk, c=c, r_=r_, ti=ti: e.dma_start(out=rtab[r_], in_=rope_d[qk, c].rearrange("a p t -> p a t")[:, :, ti * TT:(ti + 1) * TT]),
                               "rt%d" % r_, writes=[("rtab", r_)])
                        bA = psbank()
                        bB = psbank()
                        for ab, b in ((0, bA), (1, bB)):
                            for k in range(NFC):
                                TR.op("pe", lambda e, k=k, b=b, ab=ab, s_=s_, xn=xn: e.matmul(ps[:, b, :], lhsT=wR[s_][:, ab, k, :], rhs=xn[:, k, :],
                                                                                        start=(k == 0), stop=(k == NFC - 1)),
                                      reads=[("wRs", s_, ab), (xk, k)], writes=[("ps", b)])
                        TR.op("dve", lambda e, bA=bA, r_=r_: e.tensor_tensor(out=t1, in0=ps[:, bA, :], in1=rtab[r_][:, 0, :], op=ALU.mult),
                              reads=[("ps", bA), ("rtab", r_)], writes=["t1"])
                        TR.op("dve", lambda e, bB=bB, r_=r_: e.tensor_tensor(out=t2, in0=ps[:, bB, :], in1=rtab[r_][:, 1, :], op=ALU.mult),
                              reads=[("ps", bB), ("rtab", r_)], writes=["t2"])
                        sl = cnt["stg"] % 4
                        cnt["stg"] += 1
                        sb = stg[sl]
                        TR.op("dve", lambda e, sb=sb: e.tensor_tensor(out=sb, in0=t1, in1=t2, op=ALU.add), reads=["t1", "t2"], writes=[("stg", sl)])
                        dst = (rqT_d, rkT_d)[qk]
                        TR.dma("act", lambda e, sb=sb, dst=dst, c=c, ti=ti: e.dma_start(out=dst[c * 128:(c + 1) * 128, ti * TT:(ti + 1) * TT], in_=sb),
                               "stg%d" % sl, reads=[("stg", sl)], writes=[("rqk", qk, c, ti)])
                for g in range(4):
                    s_ = cnt["T"] % 2
                    cnt["T"] += 1
                    TR.dma("sp", lambda e, g=g, s_=s_: e.dma_start(out=wT[s_], in_=cdT_b[j, g]), "wT%d" % s_,
                           reads=[("cdT", j, g)], writes=[("wTs", s_)])
                    dst = (vc_d, vr_d)[g // 2]
                    for tb in range(4):
                        b = psbank()
                        for k in range(NFC):
                            TR.op("pe", lambda e, k=k, b=b, tb=tb, s_=s_, xn=xn: e.matmul(ps[:, b, :], lhsT=xn[:, k, tb * 128:(tb + 1) * 128], rhs=wT[s_][:, k, :],
                                                                                    start=(k == 0), stop=(k == NFC - 1)),
                                  reads=[("wTs", s_), (xk, k)], writes=[("ps", b)])
                        sl = cnt["stg"] % 4
                        cnt["stg"] += 1
                        sb = stg[sl]
                        if tb % 2 == 0:
                            TR.op("act", lambda e, sb=sb, b=b: e.activation(out=sb, in_=ps[:, b, :], func=AF.Copy), reads=[("ps", b)], writes=[("stg", sl)])
                        else:
                            TR.op("dve", lambda e, sb=sb, b=b: e.tensor_copy(out=sb, in_=ps[:, b, :]), reads=[("ps", b)], writes=[("stg", sl)])
                        r0 = ti * TT + tb * 128
                        c0 = (g % 2) * 512
                        TR.dma("act", lambda e, sb=sb, dst=dst, r0=r0, c0=c0: e.dma_start(out=dst[r0:r0 + 128, c0:c0 + 512], in_=sb),
                               "stg%d" % sl, reads=[("stg", sl)], writes=[("v_d", g, r0)])
                for tb in range(4):
                    b = psbank()
                    for k in range(NFC):
                        TR.op("pe", lambda e, k=k, b=b, tb=tb, xn=xn: e.matmul(ps[:, b, 0:16], lhsT=xn[:, k, tb * 128:(tb + 1) * 128], rhs=wW[:, k, :],
                                                                         start=(k == 0), stop=(k == NFC - 1)),
                              reads=["wW", (xk, k)], writes=[("ps", b)])
                    TR.op("act", lambda e, b=b: e.activation(out=iwt, in_=ps[:, b, 0:16], func=AF.Copy, scale=0.25 * 0.125),
                          reads=[("ps", b)], writes=["iwt"])
                    r0 = ti * TT + tb * 128
                    TR.dma("act", lambda e, r0=r0: e.dma_start(out=iw_d[r0:r0 + 128, :], in_=iwt), "iwt", reads=["iwt"], writes=[("iw_d", r0)])

        def odd_stage_b(layer):
            j = layer // 2
            qh = [A.alloc(T, BF16) for _ in range(2)]
            kh = [A.alloc(T, BF16) for _ in range(2)]
            vh = [A.alloc(NQB * 128, BF16).rearrange("p (j e) -> p j e", e=128) for _ in range(2)]
            dd = A.alloc(8 * 128, F32).rearrange("p (h i) -> p h i", i=128)
            rgain = A.alloc(8, F32)
            NAT = 6
            at = [A.alloc(128, BF16) for _ in range(NAT)]
            dtmp = [A.alloc(128, F32) for _ in range(2)]
            oT = [A.alloc(TT, F32) for _ in range(2)]
            cen = A.alloc(TT, F32)
            sqv = A.alloc(TT, F32)
            rs = A.alloc(TT, F32)
            sgt = [A.alloc(TT, F32) for _ in range(2)]
            yb = [A.alloc(TT, BF16) for _ in range(2)]
            for s0_ in range(2):
                TR.op("pool", lambda e, s0_=s0_: e.memset(qh[s0_], 0.0), writes=[("qh", s0_)])
                TR.op("pool", lambda e, s0_=s0_: e.memset(kh[s0_], 0.0), writes=[("kh", s0_)])
            TR.dma("sp", lambda e: e.dma_start(out=dd.rearrange("p h i -> p (h i)"), in_=dd_d), "oc3", writes=["dd"])
            TR.dma("sp", lambda e: e.dma_start(out=rgain, in_=ret_norm_gain[j].rearrange("(h p) -> p h", p=128), allow_slow_non_contiguous=True),
                   "oc4", writes=["rgain"])
            n_at = 0
            n_o = 0
            n_dt = 0
            npo = [0]
            for h in range(8):
                s_ = h % 2
                TR.dma("sp", lambda e, h=h, s_=s_: e.dma_start(out=qh[s_][0:64, :], in_=rqT_d[h * 64:(h + 1) * 64, :]), "rq%d" % s_, writes=[("qh", s_)])
                TR.dma("sp", lambda e, h=h, s_=s_: e.dma_start(out=kh[s_][0:64, :], in_=rkT_d[h * 64:(h + 1) * 64, :]), "rk%d" % s_, writes=[("kh", s_)])
                for q4 in range(4):
                    TR.dma("sp", lambda e, h=h, s_=s_, q4=q4: e.dma_start(out=vh[s_][:, q4 * 8:(q4 + 1) * 8, :],
                                                                        in_=vr_d.rearrange("(j p) c -> p j c", p=128)[:, q4 * 8:(q4 + 1) * 8, h * 128:(h + 1) * 128]),
                           "vv%d_%d" % (s_, q4), writes=[("vh", s_, q4)])
                for tg in range(NT):
                    os_ = n_o % 2
                    n_o += 1
                    for ib in range(4):
                        I = tg * 4 + ib
                        po = 6 + npo[0] % 2
                        npo[0] += 1
                        pend = None
                        for J in range(I + 1):
                            b = psbank()
                            TR.op("pe", lambda e, b=b, J=J, I=I, s_=s_: e.matmul(
                                ps[:, b, 0:128], lhsT=kh[s_][:, J * 128:(J + 1) * 128], rhs=qh[s_][:, I * 128:(I + 1) * 128],
                                start=True, stop=True), reads=[("kh", s_), ("qh", s_)], writes=[("ps", b)])
                            a_ = n_at % NAT
                            n_at += 1
                            ab = at[a_]
                            if J < I:
                                sc = float(GAM[h] ** (128 * (I - J)))
                                TR.op("act", lambda e, ab=ab, b=b, sc=sc: e.activation(out=ab, in_=ps[:, b, 0:128], func=AF.Copy, scale=sc),
                                      reads=[("ps", b)], writes=[("at", a_)])
                            else:
                                d_ = n_dt % 2
                                n_dt += 1
                                dtm = dtmp[d_]
                                TR.op("act", lambda e, dtm=dtm, b=b: e.activation(out=dtm, in_=ps[:, b, 0:128], func=AF.Copy),
                                      reads=[("ps", b)], writes=[("dtmp", d_)])
                                TR.op("pool", lambda e, ab=ab, dtm=dtm, h=h: e.tensor_tensor(out=ab, in0=dtm, in1=dd[:, h, :], op=ALU.mult),
                                      reads=[("dtmp", d_), "dd"], writes=[("at", a_)])
                            if pend is not None:
                                pend()
                            pend = (lambda ab=ab, a_=a_, J=J, I=I, po=po, s_=s_: TR.op(
                                "pe", lambda e: e.matmul(ps[:, po, 0:128], lhsT=vh[s_][:, J, :], rhs=ab, start=(J == 0), stop=(J == I)),
                                reads=[("vh", s_, J // 8), ("at", a_)], writes=[("ps", po)]))
                        pend()
                        TR.op("act", lambda e, po=po, ib=ib, os_=os_: e.activation(out=oT[os_][:, ib * 128:(ib + 1) * 128], in_=ps[:, po, 0:128], func=AF.Copy),
                              reads=[("ps", po)], writes=[("oT", os_)])
                        yield I + 1
                    o_ = oT[os_]
                    b1 = psbank()
                    TR.op("pe", lambda e, b1=b1, o_=o_: e.matmul(ps[:, b1, :], lhsT=ones, rhs=o_, start=True, stop=True),
                          reads=[("oT", os_), "ones"], writes=[("ps", b1)])
                    TR.op("dve", lambda e, b1=b1, o_=o_: e.scalar_tensor_tensor(out=cen, in0=ps[:, b1, :], scalar=-1.0 / 128, in1=o_, op0=ALU.mult, op1=ALU.add),
                          reads=[("ps", b1), ("oT", os_)], writes=["cen"])
                    TR.op("act", lambda e: e.activation(out=sqv, in_=cen, func=AF.Square), reads=["cen"], writes=["sqv"])
                    b2 = psbank()
                    TR.op("pe", lambda e, b2=b2: e.matmul(ps[:, b2, :], lhsT=ones, rhs=sqv, start=True, stop=True),
                          reads=["sqv", "ones"], writes=[("ps", b2)])
                    TR.op("dve", lambda e, b2=b2: e.tensor_scalar(out=rs, in0=ps[:, b2, :], scalar1=1.0 / 128, scalar2=LN_EPS, op0=ALU.mult, op1=ALU.add),
                          reads=[("ps", b2)], writes=["rs"])
                    TR.op("act", lambda e: e.activation(out=rs, in_=rs, func=AF.Sqrt), reads=["rs"], writes=["rs"])
                    TR.op("dve", lambda e: e.reciprocal(out=rs, in_=rs), reads=["rs"], writes=["rs"])
                    TR.op("pool", lambda e: e.tensor_tensor(out=cen, in0=cen, in1=rs, op=ALU.mult), reads=["cen", "rs"], writes=["cen"])
                    g_ = sgt[os_]
                    TR.dma("sp", lambda e, g_=g_, h=h, tg=tg: e.dma_start(out=g_, in_=sg_d[h * 128:(h + 1) * 128, tg * TT:(tg + 1) * TT]), "sgt%d" % os_,
                           writes=[("sgt", os_)])
                    y_ = yb[os_]
                    TR.op("dve", lambda e, g_=g_, y_=y_, h=h: e.scalar_tensor_tensor(out=y_, in0=cen, scalar=rgain[:, h:h + 1], in1=g_, op0=ALU.mult, op1=ALU.mult),
                          reads=["cen", "rgain", ("sgt", os_)], writes=[("yb", os_)])
                    TR.dma("sp", lambda e, y_=y_, h=h, tg=tg: e.dma_start(out=catT_d[1024 + h * 128:1024 + (h + 1) * 128, tg * TT:(tg + 1) * TT], in_=y_),
                           "yb%d" % os_, reads=[("yb", os_)], writes=[("catT", 8 + h, tg)])
                    yield 1

        def odd_stage_c1(layer):
            ikE = A.alloc(T, BF16)
            ikO = A.alloc(T, BF16)
            iq = [A.alloc(8 * 128, BF16).rearrange("p (c t) -> p c t", t=128) for _ in range(2)]
            iw = [A.alloc(16, F32) for _ in range(2)]
            wabs = [A.alloc(16, F32) for _ in range(2)]
            wsg = [A.alloc(16, F32) for _ in range(2)]
            dg = [A.alloc(16 * 128, BF16).rearrange("p (h t) -> p h t", t=128) for _ in range(2)]
            acc = A.alloc(T, F32)
            work = A.alloc(T, F32)
            rl = [A.alloc(512, BF16) for _ in range(4)]
            m8 = A.alloc(8, F32)
            thrc = A.alloc(1, F32)
            nm = A.alloc(T, BF16)
            nmT = [A.alloc(NQB * 128, BF16).rearrange("p (j t) -> p j t", t=128) for _ in range(2)]
            identb = A.alloc(128, BF16)
            TR.op("pool", lambda e: e.tensor_copy(out=identb, in_=ident), reads=["ident"], writes=["identb1"])
            TR.op("pool", lambda e: e.memset(thrc, -1e29), writes=["thrc"])
            TR.op("pool", lambda e: e.memset(ikE, 0.0), writes=["ik"])
            TR.op("pool", lambda e: e.memset(ikO, 0.0), writes=["ik"])
            TR.dma("sp", lambda e: e.dma_start(out=ikE[0:64, :], in_=ikT_d[0:64, :]), "oc5", writes=["ik"])
            TR.dma("sp", lambda e: e.dma_start(out=ikO[64:128, :], in_=ikT_d[64:128, :]), "oc0", writes=["ik"])
            nrl = 0
            nacc = 0
            for I in range(NQB):
                S = 128 * (I + 1)
                s_ = I % 2
                TR.dma("sp", lambda e, I=I, s_=s_: e.dma_start(out=iq[s_], in_=iqT_d.rearrange("(c p) t -> p c t", p=128)[:, :, I * 128:(I + 1) * 128]),
                       "iq%d" % s_, writes=[("iq", s_)])
                TR.dma("sp", lambda e, I=I, s_=s_: e.dma_start(out=iw[s_], in_=iw_d[I * 128:(I + 1) * 128, :]), "iw%d" % s_, writes=[("iw", s_)])
                TR.op("dve", lambda e, s_=s_: e.tensor_scalar(out=wsg[s_], in0=iw[s_], scalar1=0.0, scalar2=2.0, op0=ALU.is_ge, op1=ALU.mult),
                      reads=[("iw", s_)], writes=[("wsg", s_)])
                TR.op("dve", lambda e, s_=s_: e.tensor_scalar(out=wsg[s_], in0=wsg[s_], scalar1=-1.0, scalar2=None, op0=ALU.add),
                      reads=[("wsg", s_)], writes=[("wsg", s_)])
                TR.op("dve", lambda e, s_=s_: e.tensor_tensor(out=wabs[s_], in0=iw[s_], in1=wsg[s_], op=ALU.mult), reads=[("iw", s_), ("wsg", s_)], writes=[("wabs", s_)])
                for hh in range(16):
                    TR.op("dve", lambda e, s_=s_, hh=hh: e.tensor_scalar(out=dg[s_][:, hh, :], in0=ident, scalar1=wsg[s_][:, hh:hh + 1], scalar2=None, op0=ALU.mult),
                          reads=["ident", ("wsg", s_)], writes=[("dg", s_)])
                for c0 in range(0, S, 512):
                    n = min(512, S - c0)
                    ck = ("acc", c0 // 512)
                    ba = 4 + nacc % 2
                    nacc += 1
                    pend = None
                    for hh in range(16):
                        b = psbank()
                        TR.op("pe", lambda e, b=b, hh=hh, c0=c0, n=n, s_=s_: e.matmul(
                            ps[:, b, 0:n], lhsT=iq[s_][:, hh // 2, :], rhs=(ikE if hh % 2 == 0 else ikO)[:, c0:c0 + n], start=True, stop=True),
                            reads=[("iq", s_), "ik"], writes=[("ps", b)])
                        r_ = nrl % 4
                        nrl += 1
                        rb = rl[r_]
                        TR.op("act", lambda e, rb=rb, b=b, n=n, s_=s_, hh=hh: e.activation(out=rb[:, 0:n], in_=ps[:, b, 0:n], func=AF.Relu, scale=wabs[s_][:, hh:hh + 1]),
                              reads=[("ps", b), ("wabs", s_)], writes=[("rl", r_)])
                        if pend is not None:
                            pend()
                        pend = (lambda rb=rb, r_=r_, hh=hh, n=n, ba=ba, s_=s_: TR.op(
                            "pe", lambda e: e.matmul(ps[:, ba, 0:n], lhsT=dg[s_][:, hh, :], rhs=rb[:, 0:n], start=(hh == 0), stop=(hh == 15)),
                            reads=[("dg", s_), ("rl", r_)], writes=[("ps", ba)]))
                    pend()
                    TR.op("act", lambda e, ba=ba, c0=c0, n=n: e.activation(out=acc[:, c0:c0 + n], in_=ps[:, ba, 0:n], func=AF.Copy),
                          reads=[("ps", ba)], writes=[ck])
                    yield 2
                acck = [("acc", c) for c in range((S + 511) // 512)]
                TR.op("pool", lambda e, S=S: e.memset(acc[0:64, S - 64:S], -1e30), reads=acck, writes=acck)
                if I >= 2:
                    for r in range(32):
                        src_ = acc if r == 0 else work
                        TR.op("dve", lambda e, src_=src_, S=S: e.max(out=m8, in_=src_[:, 0:S]), reads=acck + ["work"], writes=["m8"])
                        if r < 31:
                            TR.op("dve", lambda e, src_=src_, S=S: e.match_replace(out=work[:, 0:S], in_to_replace=m8, in_values=src_[:, 0:S], imm_value=-1e30),
                                  reads=acck + ["m8", "work"], writes=["work"])
                        if r % 2 == 1:
                            yield (I + 1) * 0.5
                    thr = m8[:, 7:8]
                    thrk = "m8"
                else:
                    thr = thrc[:, 0:1]
                    thrk = "thrc"
                TR.op("dve", lambda e, S=S, thr=thr: e.tensor_scalar(out=nm[:, 0:S], in0=acc[:, 0:S], scalar1=thr, scalar2=-30000.0, op0=ALU.is_lt, op1=ALU.mult),
                      reads=acck + [thrk], writes=["nm"])
                ns = I % 2
                for J0 in range(0, I + 1, 8):
                    js = list(range(J0, min(J0 + 8, I + 1)))
                    b = psbank()
                    pbf = ps[:, b, :].bitcast(BF16)
                    for jj, J in enumerate(js):
                        TR.op("pe", lambda e, pbf=pbf, jj=jj, J=J: e.transpose(out=pbf[:, jj * 128:(jj + 1) * 128], in_=nm[:, J * 128:(J + 1) * 128], identity=identb),
                              reads=["nm", "identb1"], writes=[("ps", b)])
                    nj = len(js)
                    TR.op("act", lambda e, pbf=pbf, J0=J0, nj=nj, ns=ns: e.activation(out=nmT[ns][:, J0:J0 + nj, :].rearrange("p j t -> p (j t)"), in_=pbf[:, 0:nj * 128], func=AF.Copy),
                          reads=[("ps", b)], writes=[("nmT", ns)])
                TR.dma("sp", lambda e, I=I, ns=ns: e.dma_start(out=nmT_d[I, :, 0:(I + 1) * 128], in_=nmT[ns][:, 0:I + 1, :].rearrange("p j t -> p (j t)")),
                       "nmT%d" % ns, reads=[("nmT", ns)], writes=[("nmT_d", I)])
                yield 1

        def odd_stage_c2(layer):
            A.reset()
            qh = [A.alloc(T, BF16) for _ in range(2)]
            kh = [A.alloc(T, BF16) for _ in range(2)]
            vh = [A.alloc(NQB * 130, BF16).rearrange("p (j e) -> p j e", e=130) for _ in range(2)]
            nmb = [A.alloc(NQB * 128, BF16).rearrange("p (j t) -> p j t", t=128) for _ in range(2)]
            bg = A.alloc(8 * 256, F32).rearrange("p (h k t) -> p h k t", h=8, k=2)
            t15 = A.alloc(8, F32)
            tab = A.alloc(256, F32)
            mb = A.alloc(8, F32)
            rc = A.alloc(8, F32)
            identb = A.alloc(128, BF16)
            mx = [A.alloc(8, F32) for _ in range(2)]
            mcol = [A.alloc(1, F32) for _ in range(2)]
            rcol = [A.alloc(1, F32) for _ in range(2)]
            rdiag = [A.alloc(128, BF16) for _ in range(2)]
            onesb = A.alloc(128, BF16)
            pt = [A.alloc(512, BF16) for _ in range(4)]
            rcp = A.alloc(1, F32)
            yo = A.alloc(128, BF16)
            yT = [A.alloc(TT, BF16) for _ in range(2)]
            TR.op("dve", lambda e: e.tensor_copy(out=identb, in_=ident), reads=["ident"], writes=["identb"])
            TR.op("dve", lambda e: e.memset(onesb, 1.0), writes=["onesb"])
            for s_ in range(2):
                TR.op("dve", lambda e, s_=s_: e.memset(vh[s_].rearrange("p j e -> p (j e)"), 1.0), writes=[("vh", s_, q4) for q4 in range(4)])
            TR.dma("sp", lambda e: e.dma_start(out=bg.rearrange("p h k t -> p (h k t)"), in_=biasg_d), "oc1", writes=["bg"])
            TR.dma("sp", lambda e: e.dma_start(out=t15, in_=t15_d), "oc2", writes=["t15"])
            TR.dma("sp", lambda e: e.dma_start(out=tab, in_=tab_d), "oc3", writes=["tab"])
            for h in range(8):
                TR.op("dve", lambda e, h=h: e.tensor_scalar(out=bg[:, h].rearrange("p k t -> p (k t)"), in0=bg[:, h].rearrange("p k t -> p (k t)"),
                                                            scalar1=t15[:, h:h + 1], scalar2=1.0 / ASCALE, op0=ALU.subtract, op1=ALU.mult),
                      reads=["bg", "t15"], writes=["bg"])
            TR.op("dve", lambda e: e.tensor_reduce(out=mb, in_=tab.rearrange("p (b h) -> p h b", h=8), axis=AX.X, op=ALU.max),
                  reads=["tab"], writes=["mb"])
            TR.op("dve", lambda e: e.tensor_tensor(out=rc, in0=tab[:, 120:128], in1=mb, op=ALU.subtract), reads=["tab", "mb"], writes=["rc"])
            TR.op("dve", lambda e: e.tensor_scalar(out=rc, in0=rc, scalar1=1.0 / ASCALE, scalar2=None, op0=ALU.mult), reads=["rc"], writes=["rc"])
            C.rot = [0, 1, 2, 3, 4, 5]
            st = {"npt": 0, "npo": 0}

            def stage1(n, h, I):
                s_ = h % 2
                S = 128 * (I + 1)
                ms = n % 2
                if I == 0:
                    TR.dma("sp", lambda e: e.dma_start(out=qh[s_], in_=qT_d[h * 128:(h + 1) * 128, :]), "cq%d" % s_, writes=[("qh", s_)])
                    TR.dma("sp", lambda e: e.dma_start(out=kh[s_], in_=kT_d[h * 128:(h + 1) * 128, :]), "ck%d" % s_, writes=[("kh", s_)])
                    for q4 in range(4):
                        TR.dma("sp", lambda e, q4=q4: e.dma_start(out=vh[s_][:, q4 * 8:(q4 + 1) * 8, 0:128],
                                                                    in_=vc_d.rearrange("(j p) c -> p j c", p=128)[:, q4 * 8:(q4 + 1) * 8, h * 128:(h + 1) * 128]),
                               "vv%d_%d" % (s_, q4), writes=[("vh", s_, q4)])
                TR.dma("sp", lambda e: e.dma_start(out=nmb[ms][:, 0:I + 1, :].rearrange("p j t -> p (j t)"), in_=nmT_d[I, :, 0:(I + 1) * 128]),
                       "nmb%d" % ms, reads=[("nmT_d", I)], writes=[("nmb", ms)])
                nch = (S + 511) // 512
                for c in range(nch):
                    nn = min(512, S - c * 512)
                    b = psbank()
                    TR.op("pe", lambda e, b=b, c=c, nn=nn: e.matmul(ps[:, b, 0:nn], lhsT=qh[s_][:, I * 128:(I + 1) * 128], rhs=kh[s_][:, c * 512:c * 512 + nn],
                                                                     start=True, stop=True), reads=[("qh", s_), ("kh", s_)], writes=[("ps", b)])
                    TR.op("dve", lambda e, b=b, c=c, nn=nn: e.tensor_reduce(out=mx[ms][:, c:c + 1], in_=ps[:, b, 0:nn], axis=AX.X, op=ALU.max),
                          reads=[("ps", b)], writes=[("mx", ms)])
                TR.op("dve", lambda e: e.tensor_reduce(out=mcol[ms], in_=mx[ms][:, 0:nch], axis=AX.X, op=ALU.max), reads=[("mx", ms)], writes=[("mcol", ms)])
                TR.op("dve", lambda e: e.tensor_scalar(out=rcol[ms], in0=mcol[ms], scalar1=-1.0, scalar2=rc[:, h:h + 1], op0=ALU.mult, op1=ALU.add),
                      reads=[("mcol", ms), "rc"], writes=[("rcol", ms)])
                TR.op("dve", lambda e: e.tensor_scalar(out=rdiag[ms], in0=ident, scalar1=rcol[ms][:, 0:1], scalar2=None, op0=ALU.mult),
                      reads=["ident", ("rcol", ms)], writes=[("rdiag", ms)])

            def stage2(n, h, I):
                s_ = h % 2
                ms = n % 2
                rdg = rdiag[ms]
                po = 6 + st["npo"] % 2
                st["npo"] += 1
                pend = []
                for J0 in range(0, I + 1, 4):
                    js = list(range(J0, min(J0 + 4, I + 1)))
                    b = psbank()
                    for jj, J in enumerate(js):
                        o_ = ps[:, b, jj * 128:(jj + 1) * 128]
                        near = J >= I - 1
                        TR.op("pe", lambda e, o_=o_, J=J: e.matmul(o_, lhsT=kh[s_][:, J * 128:(J + 1) * 128], rhs=qh[s_][:, I * 128:(I + 1) * 128], start=True, stop=False),
                              reads=[("kh", s_), ("qh", s_)], writes=[("ps", b)])
                        TR.op("pe", lambda e, o_=o_: e.matmul(o_, lhsT=onesb, rhs=rdg, start=False, stop=False),
                              reads=["onesb", ("rdiag", ms)], writes=[("ps", b)])
                        TR.op("pe", lambda e, o_=o_, J=J, near=near: e.matmul(o_, lhsT=identb, rhs=nmb[ms][:, J, :], start=False, stop=(not near)),
                              reads=["identb", ("nmb", ms)], writes=[("ps", b)])
                        if near:
                            kb = J - (I - 1)
                            TR.op("pe", lambda e, o_=o_, kb=kb: e.matmul(o_, lhsT=ident, rhs=bg[:, h, kb, :], start=False, stop=True),
                                  reads=["ident", "bg"], writes=[("ps", b)])
                    p_ = st["npt"] % 4
                    st["npt"] += 1
                    ptb = pt[p_]
                    nj = len(js)
                    TR.op("act", lambda e, ptb=ptb, b=b, nj=nj: e.activation(out=ptb[:, 0:nj * 128], in_=ps[:, b, 0:nj * 128], func=AF.Exp, scale=ASCALE),
                          reads=[("ps", b)], writes=[("pt", p_)])
                    for f in pend:
                        f()
                    pend = []
                    for jj, J in enumerate(js):
                        pend.append(lambda ptb=ptb, p_=p_, jj=jj, J=J: TR.op(
                            "pe", lambda e: e.matmul(ps[:, po, 0:129], lhsT=ptb[:, jj * 128:(jj + 1) * 128], rhs=vh[s_][:, J, 0:129], start=(J == 0), stop=(J == I)),
                            reads=[("pt", p_), ("vh", s_, J // 8)], writes=[("ps", po)]))
                for f in pend:
                    f()
                TR.op("dve", lambda e: e.reciprocal(out=rcp, in_=ps[:, po, 128:129]), reads=[("ps", po)], writes=["rcp"])
                TR.op("act", lambda e: e.activation(out=yo, in_=ps[:, po, 0:128], func=AF.Copy, scale=rcp[:, 0:1]), reads=[("ps", po), "rcp"], writes=["yo"])
                bt = psbank()
                pbf = ps[:, bt, :].bitcast(BF16)
                TR.op("pe", lambda e: e.transpose(out=pbf[:, 0:128], in_=yo, identity=identb), reads=["yo", "identb"], writes=[("ps", bt)])
                ys = (I // 4) % 2
                TR.op("dve", lambda e: e.tensor_copy(out=yT[ys][:, (I % 4) * 128:(I % 4 + 1) * 128], in_=pbf[:, 0:128]),
                      reads=[("ps", bt)], writes=[("yT", ys)])
                if I % 4 == 3:
                    tg = I // 4
                    TR.dma("sp", lambda e: e.dma_start(out=catT_d[h * 128:(h + 1) * 128, tg * TT:(tg + 1) * TT], in_=yT[ys]),
                           "yT%d" % ys, reads=[("yT", ys)], writes=[("catT", h, tg)])

            items = [(h, I) for h in range(8) for I in range(NQB)]
            stage1(0, *items[0])
            for n in range(len(items)):
                if n + 1 < len(items):
                    stage1(n + 1, *items[n + 1])
                stage2(n, *items[n])


        def odd_stage_d(layer, hsrc, hdst):
            j = layer // 2
            A.reset()
            cat = [A.alloc(NFC * TT, BF16).rearrange("p (f t) -> p f t", t=TT) for _ in range(2)]
            wo = [A.alloc(NFC * 256, BF16).rearrange("p (k n) -> p k n", n=256) for _ in range(2)]
            epi = [A.alloc(TT, F32) for _ in range(3)]
            epi_n = [0]
            nw = 0
            cv = catT_d.rearrange("(f p) t -> p f t", p=128)
            for ti in range(NT):
                cs = ti % 2
                for q in range(4):
                    TR.dma("sp", lambda e, q=q, ti=ti, cs=cs: e.dma_start(out=cat[cs][:, q * 4:(q + 1) * 4, :], in_=cv[:, q * 4:(q + 1) * 4, ti * TT:(ti + 1) * TT]),
                           "cat%d_%d" % (cs, q), writes=[("cat", cs, q)])
                for c in range(8):
                    s_ = nw % 2
                    nw += 1
                    TR.dma("sp", lambda e, c=c, s_=s_: e.dma_start(out=wo[s_], in_=cdo_b[j, c]), "wo%d" % s_, reads=[("cdo", j, c)], writes=[("wos", s_)])
                    for hf in range(2):
                        b = psbank()
                        for k in range(NFC):
                            TR.op("pe", lambda e, s_=s_, hf=hf, k=k, b=b, cs=cs: e.matmul(ps[:, b, :], lhsT=wo[s_][:, k, hf * 128:(hf + 1) * 128], rhs=cat[cs][:, k, :],
                                                                                          start=(k == 0), stop=(k == NFC - 1)),
                                  reads=[("wos", s_), ("cat", cs, k // 4)], writes=[("ps", b)])
                        residual_epilogue(hsrc, hdst, ti, c * 2 + hf, b, 1.0, epi, epi_n)

        def run_merged(gens, totals):
            prog = [0.0] * len(gens)
            alive = [True] * len(gens)
            while any(alive):
                cand = [i for i in range(len(gens)) if alive[i]]
                i = min(cand, key=lambda k: prog[k] / totals[k])
                try:
                    prog[i] += next(gens[i])
                except StopIteration:
                    alive[i] = False

        def phase_mix_odd(layer, hsrc, hdst, next_cast=None, stages="abcd"):
            if next_cast is not None:
                next_cast()
            odd_stage_a(layer, hsrc)
            TR.barrier()
            if "b" not in stages:
                A.reset()
                zt = A.alloc(T, BF16)
                TR.op("dve", lambda e: e.memset(zt, 0.0), writes=["zt"])
                for hh_ in range(8):
                    TR.dma("sp", lambda e, hh_=hh_: e.dma_start(out=catT_d[1024 + hh_ * 128:1024 + (hh_ + 1) * 128, :], in_=zt), "zt", reads=["zt"], writes=[("catz", 8 + hh_)])
                TR.barrier()
            if "c" not in stages:
                A.reset()
                zt = A.alloc(T, BF16)
                TR.op("dve", lambda e: e.memset(zt, 0.0), writes=["zt"])
                for hh_ in range(8):
                    TR.dma("sp", lambda e, hh_=hh_: e.dma_start(out=catT_d[hh_ * 128:(hh_ + 1) * 128, :], in_=zt), "zt", reads=["zt"], writes=[("catz", hh_)])
                TR.barrier()
            A.reset()
            C.rot = [0, 1, 2, 3]
            gens, totals = [], []
            if "b" in stages:
                gens.append(odd_stage_b(layer))
                totals.append(8 * 528 + 64.0)
            if "c" in stages:
                gens.append(odd_stage_c1(layer))
                totals.append(2 * 148 + 16 * 525 * 0.5 + 32.0)
            run_merged(gens, totals)
            C.rot = list(range(8))
            TR.barrier()
            if "c" in stages:
                odd_stage_c2(layer)
                C.rot = list(range(8))
                TR.barrier()
            odd_stage_d(layer, hsrc, hdst)


        def phase_final(hsrc):
            A.reset()
            hT = A.alloc(NFC * TT, F32).rearrange("p (f t) -> p f t", t=TT)
            y = A.alloc(NFC * TT, F32).rearrange("p (f t) -> p f t", t=TT)
            sq = [A.alloc(TT, F32) for _ in range(2)]
            rstd = A.alloc(TT, F32)
            ot = [A.alloc(D, F32) for _ in range(2)]
            n = 0
            for ti in range(NT):
                rms_norm_tile(hsrc, ti, 12, hT, y, sq, rstd)
                for tb in range(TT // 128):
                    o = ot[n % 2]
                    okey = ("ot", n % 2)
                    n += 1
                    for f4 in range(NFC // 4):
                        b = psbank()
                        for k in range(4):
                            f = f4 * 4 + k
                            TR.op("pe", lambda e, f=f, k=k, b=b, tb=tb: e.transpose(
                                out=ps[:, b, k * 128:(k + 1) * 128], in_=y[:, f, tb * 128:(tb + 1) * 128], identity=ident),
                                reads=[("xn", f), "ident"], writes=[("ps", b)])
                        if f4 % 2 == 0:
                            TR.op("act", lambda e, o=o, b=b, f4=f4: e.activation(out=o[:, f4 * 512:(f4 + 1) * 512], in_=ps[:, b, :], func=AF.Copy),
                                  reads=[("ps", b)], writes=[okey])
                        else:
                            TR.op("dve", lambda e, o=o, b=b, f4=f4: e.tensor_copy(out=o[:, f4 * 512:(f4 + 1) * 512], in_=ps[:, b, :]),
                                  reads=[("ps", b)], writes=[okey])
                    r0 = ti * TT + tb * 128
                    TR.dma("sp", lambda e, o=o, r0=r0: e.dma_start(out=out[r0:r0 + 128, :], in_=o), "ot%d" % ((n - 1) % 2),
                           reads=[okey], writes=[("out", r0)])

        plan = []
        plan.append(("prologue",))
        for layer in range(DEPTH):
            plan.append(("ffn", layer * 2, layer * 3 + 0))
            plan.append(("mix", layer))
            plan.append(("ffn", layer * 2 + 1, layer * 3 + 2))
        plan.append(("final",))
        if phases is not None:
            plan = [p for p in plan if p in phases or p[0] in ("prologue", "final")]

        def caster(p):
            if p[0] == "ffn":
                return lambda: cast_ffn(p[1])
            if p[0] == "mix" and p[1] % 2 == 0:
                return lambda: cast_even(p[1] // 2)
            if p[0] == "mix":
                return lambda: cast_odd(p[1] // 2)
            return None
        wplan = [p for p in plan if caster(p) is not None]
        cur = 0
        if wplan:
            caster(wplan[0])()
        for p in plan:
            nxt = None
            if p in wplan:
                i = wplan.index(p)
                if i + 1 < len(wplan):
                    nxt = caster(wplan[i + 1])
            if p[0] == "prologue":
                phase_prologue(hbuf[cur])
            elif p[0] == "ffn":
                phase_ffn(p[1], p[2], hbuf[cur], hbuf[1 - cur], nxt)
                cur = 1 - cur
            elif p[0] == "mix":
                if p[1] % 2 == 0:
                    phase_mix_even(p[1], hbuf[cur], hbuf[1 - cur], nxt)
                    cur = 1 - cur
                else:
                    phase_mix_odd(p[1], hbuf[cur], hbuf[1 - cur], nxt, stages=odd_stages)
                    cur = 1 - cur
            elif p[0] == "final":
                phase_final(hbuf[cur])
            TR.barrier()
        TR.emit(block)
        C.nops = TR.nops
    return nc


def _t5_bucket_np(rel):
    import jax
    import jax.numpy as jnp
    import math
    with jax.default_device(jax.devices("cpu")[0]):
        rel = jnp.asarray(rel, dtype=jnp.int32)
        nb = 16
        max_exact = 8
        ret = jnp.where(rel > 0, nb, 0)
        n = jnp.abs(rel)
        large = max_exact + (jnp.log(jnp.maximum(n, 1).astype(jnp.float32) / max_exact)
                             / math.log(128 / max_exact) * (nb - max_exact)).astype(jnp.int32)
        large = jnp.minimum(large, nb - 1)
        return np.asarray(ret + jnp.where(n < max_exact, n, large))


def host_constants(rel_bias_table=None):
    t = np.arange(TT)
    invc = np.stack([1.0 / np.minimum(t + 1, w) for w in (2, 4, 8, 16)]).astype(np.float32)
    invc0 = np.ascontiguousarray(np.broadcast_to(invc.reshape(1, 4 * TT), (128, 4 * TT)))
    out = {"ident": np.eye(128, dtype=np.float32), "invc0": invc0}
    pos = np.arange(T, dtype=np.float64)
    gam = np.array([1.0 - 2.0 ** (-5.0 - h) for h in range(8)], dtype=np.float64)
    p = np.arange(128)
    i = p % 64
    f = i % 32
    sign = np.where(i < 32, -1.0, 1.0)
    freqs = 10000.0 ** (-f.astype(np.float64) / 32.0)
    ang = (pos[None, :].astype(np.float32) * freqs[:, None].astype(np.float32)).astype(np.float64)
    cos, sin = np.cos(ang), np.sin(ang) * sign[:, None]
    tl = (np.arange(T) % 128).astype(np.float64)
    rope = np.zeros((2, 4, 2, 128, T), dtype=np.float32)
    for c in range(4):
        hh = 2 * c + p // 64
        dq = gam[hh][:, None] ** tl[None, :]
        dk = (64.0 ** -0.5) * gam[hh][:, None] ** (-tl[None, :])
        rope[0, c, 0], rope[0, c, 1] = cos * dq, sin * dq
        rope[1, c, 0], rope[1, c, 1] = cos * dk, sin * dk
    out["rope_tab"] = rope
    jl = np.arange(128)[:, None]
    il = np.arange(128)[None, :]
    vis = (jl < ((il // 64) + 1) * 64)
    dd = np.zeros((128, 8, 128), dtype=np.float32)
    for h in range(8):
        m = np.where(il >= jl, 1.0, gam[h] ** (2.0 * (jl - il)))
        dd[:, h, :] = m * vis
    out["ret_diag"] = dd.reshape(128, 8 * 128)
    if rel_bias_table is not None:
        tab = np.asarray(rel_bias_table, dtype=np.float32)
        sl = np.arange(128)[:, None, None]
        blk = np.arange(2)[None, :, None]
        tl_ = np.arange(128)[None, None, :]
        rel = blk * 128 + sl - 128 - tl_
        bidx = _t5_bucket_np(rel)
        g = tab[bidx]
        out["bias_g"] = np.ascontiguousarray(np.transpose(g, (0, 3, 1, 2))).reshape(128, 8 * 2 * 128)
        out["bias_t15"] = np.ascontiguousarray(np.broadcast_to(tab[15:16, :], (128, 8)))
        out["bias_tab"] = np.ascontiguousarray(np.broadcast_to(tab.reshape(1, 256), (128, 256)))
    return out


def make_in_maps(inputs, n_cores=N_CORES):
    consts = host_constants(inputs.get("rel_bias_table"))
    shared = {
        "norm_gains": np.ascontiguousarray(np.asarray(inputs["norm_gains"], dtype=np.float32).reshape(DEPTH * 3, D)),
        "ffn_w_gate_up": np.ascontiguousarray(np.asarray(inputs["ffn_w_gate_up"], dtype=np.float32).reshape(DEPTH * 2, D, 2 * DFF)),
        "ffn_w_down": np.ascontiguousarray(np.asarray(inputs["ffn_w_down"], dtype=np.float32).reshape(DEPTH * 2, DFF, D)),
        "final_norm": np.ascontiguousarray(np.asarray(inputs["final_norm"], dtype=np.float32).reshape(1, D)),
    }
    for k in ("ab_w_in", "ab_conv_w", "ab_pool_w", "ab_pool_scale", "ab_w_out", "cd_w_in", "ret_norm_gain", "cd_w_out"):
        shared[k] = np.ascontiguousarray(np.asarray(inputs[k], dtype=np.float32))
    shared.update(consts)
    xs = np.asarray(inputs["x"], dtype=np.float32)
    maps = []
    for c in range(n_cores):
        m = dict(shared)
        m["x"] = np.ascontiguousarray(xs[c])
        maps.append(m)
    return maps


def kernel(**inputs):
    nc = build_program()
    in_maps = make_in_maps(inputs)
    res = run_bass_kernel_spmd(nc, in_maps, core_ids=list(range(N_CORES)))
    return np.stack([np.asarray(r["out"], dtype=np.float32) for r in res.results], axis=0)
```
